# Optimizing a Trainium2 kernel written in Bass

```python
import jax, jax.numpy as jnp
from jax import lax
import numpy as np

D_MODEL = 1024
BATCH = 8
SEQ = 8192
DEPTH = 1

GDN_HEADS = 4
GDN_HEAD_DIM = 128
GDN_WIDTH = GDN_HEADS * GDN_HEAD_DIM
CONV_WIDTH = 4
CHUNK = 64
MLA_HEADS = 4
QK_NOPE_DIM = 128
QK_ROPE_DIM = 64
V_HEAD_DIM = 128
Q_LORA_RANK = 384
KV_LORA_RANK = 256
MLA_WIDTH = MLA_HEADS * V_HEAD_DIM
ROPE_THETA = 10000.0
Q_BLOCK = 128
MIX_WIDTH = GDN_WIDTH + MLA_WIDTH
IN_DIM = 4 * GDN_WIDTH + 2 * GDN_HEADS + Q_LORA_RANK + KV_LORA_RANK + QK_ROPE_DIM
PEER_HEADS = 8
N_KEYS = 128
N_EXPERTS = N_KEYS * N_KEYS
PEER_KEY_DIM = 256
PEER_HALF = PEER_KEY_DIM // 2
PEER_TOPK = 16
TOKEN_BLOCK = 128
N_MOD = 6
EPS = 1e-6

kernel_name = "hybrid_gdn_mla_peer_adaln_block"


def rms_norm(x, g):
    xf = x.astype(jnp.float32)
    y = xf * lax.rsqrt(jnp.mean(xf * xf, axis=-1, keepdims=True) + EPS)
    return (y * g.astype(jnp.float32)).astype(x.dtype)


def l2_norm(x):
    return x * lax.rsqrt(jnp.sum(x * x, axis=-1, keepdims=True) + EPS)


def modulate(h, shift, scale):
    return h * (1.0 + scale) + shift


def apply_rotary(x, cos, sin):
    xf = x.astype(jnp.float32)
    x1, x2 = jnp.split(xf, 2, axis=-1)
    out = jnp.concatenate([x1 * cos - x2 * sin, x2 * cos + x1 * sin], axis=-1)
    return out.astype(x.dtype)


def causal_short_conv(x, w):
    ch = x.shape[-1]
    return lax.conv_general_dilated(
        x, w.astype(x.dtype)[:, None, :], window_strides=(1,),
        padding=[(CONV_WIDTH - 1, 0)], dimension_numbers=("NWC", "WIO", "NWC"),
        feature_group_count=ch)


def gated_delta_rule(q, k, v, g, beta):
    b, h, s, dk = q.shape
    dv = v.shape[-1]
    n = s // CHUNK
    q = q * (dk ** -0.5)
    q = q.reshape(b, h, n, CHUNK, dk)
    k = k.reshape(b, h, n, CHUNK, dk)
    v = v.reshape(b, h, n, CHUNK, dv)
    g = g.reshape(b, h, n, CHUNK)
    beta = beta.reshape(b, h, n, CHUNK)
    gc = jnp.cumsum(g, axis=-1)
    incl = jnp.tril(jnp.ones((CHUNK, CHUNK), dtype=bool))
    strict = jnp.tril(jnp.ones((CHUNK, CHUNK), dtype=bool), -1)
    decay = jnp.exp(jnp.where(incl, gc[..., :, None] - gc[..., None, :], -jnp.inf))
    kb = k * beta[..., None]
    m = jnp.where(strict, jnp.einsum('bhnid,bhnjd->bhnij', kb, k) * decay, 0.0)
    a = m + jnp.eye(CHUNK, dtype=m.dtype)
    rhs = jnp.concatenate([v * beta[..., None], kb * jnp.exp(gc)[..., None]], axis=-1)
    sol = lax.linalg.triangular_solve(a, rhs, left_side=True, lower=True, unit_diagonal=True)
    u, w = sol[..., :dv], sol[..., dv:]
    attn = jnp.einsum('bhnid,bhnjd->bhnij', q, k) * decay
    q_dec = q * jnp.exp(gc)[..., None]
    k_dec = k * jnp.exp(gc[..., -1:] - gc)[..., None]
    g_last = jnp.exp(gc[..., -1])
    xs = (jnp.moveaxis(q_dec, 2, 0), jnp.moveaxis(k_dec, 2, 0), jnp.moveaxis(u, 2, 0),
          jnp.moveaxis(w, 2, 0), jnp.moveaxis(attn, 2, 0), jnp.moveaxis(g_last, 2, 0))

    def step(state, inp):
        qd, kd, ui, wi, ai, gl = inp
        v_new = ui - jnp.einsum('bhcd,bhde->bhce', wi, state)
        o = jnp.einsum('bhcd,bhde->bhce', qd, state) + jnp.einsum('bhij,bhje->bhie', ai, v_new)
        state = state * gl[..., None, None] + jnp.einsum('bhcd,bhce->bhde', kd, v_new)
        return state, o

    state0 = jnp.zeros((b, h, dk, dv), dtype=q.dtype)
    _, o = lax.scan(step, state0, xs)
    return jnp.moveaxis(o, 0, 2).reshape(b, h, s, dv)


def gdn_mixer(q, k, v, z, a, bgate, conv_w, a_log, dt_bias, norm_g):
    bsz, s, _ = q.shape
    qkv = jax.nn.silu(causal_short_conv(jnp.concatenate([q, k, v], axis=-1), conv_w))
    qc, kc, vc = jnp.split(qkv.astype(jnp.float32), 3, axis=-1)

    def heads(t):
        return t.reshape(bsz, s, GDN_HEADS, GDN_HEAD_DIM).transpose(0, 2, 1, 3)

    qh, kh, vh = l2_norm(heads(qc)), l2_norm(heads(kc)), heads(vc)
    g = -jnp.exp(a_log.astype(jnp.float32)) * jax.nn.softplus(
        a.astype(jnp.float32) + dt_bias.astype(jnp.float32))
    beta = jax.nn.sigmoid(bgate.astype(jnp.float32))
    o = gated_delta_rule(qh, kh, vh, g.transpose(0, 2, 1), beta.transpose(0, 2, 1))
    o = o.transpose(0, 2, 1, 3)
    zh = z.astype(jnp.float32).reshape(bsz, s, GDN_HEADS, GDN_HEAD_DIM)
    o = rms_norm(o, norm_g) * jax.nn.silu(zh)
    return o.reshape(bsz, s, GDN_WIDTH).astype(q.dtype)


def mla_mixer(c_q, c_kv, k_rope, cos, sin, q_norm_g, w_uq, kv_norm_g, w_ukv):
    bsz, s, _ = c_q.shape
    q = (rms_norm(c_q, q_norm_g) @ w_uq).reshape(bsz, s, MLA_HEADS, QK_NOPE_DIM + QK_ROPE_DIM)
    kv = (rms_norm(c_kv, kv_norm_g) @ w_ukv).reshape(bsz, s, MLA_HEADS, QK_NOPE_DIM + V_HEAD_DIM)
    q_nope, q_rope = q[..., :QK_NOPE_DIM], q[..., QK_NOPE_DIM:]
    k_nope, v = kv[..., :QK_NOPE_DIM], kv[..., QK_NOPE_DIM:]
    q_rope = apply_rotary(q_rope, cos[:, :, None, :], sin[:, :, None, :])
    k_rope = apply_rotary(k_rope, cos, sin)
    k_rope = jnp.broadcast_to(k_rope[:, :, None, :], (bsz, s, MLA_HEADS, QK_ROPE_DIM))
    scale = (QK_NOPE_DIM + QK_ROPE_DIM) ** -0.5
    qf = (jnp.concatenate([q_nope, q_rope], axis=-1) * scale).transpose(0, 2, 1, 3)
    kf = jnp.concatenate([k_nope, k_rope], axis=-1).transpose(0, 2, 1, 3)
    vf = v.transpose(0, 2, 1, 3)
    nb = s // Q_BLOCK
    qb = qf.reshape(bsz, MLA_HEADS, nb, Q_BLOCK, -1).transpose(2, 0, 1, 3, 4)
    key_pos = jnp.arange(s)

    def attend(args):
        q_blk, start = args
        sc = jnp.einsum('bhqd,bhkd->bhqk', q_blk, kf).astype(jnp.float32)
        q_pos = start + jnp.arange(Q_BLOCK)
        sc = jnp.where(key_pos[None, :] <= q_pos[:, None], sc, -jnp.inf)
        p = jax.nn.softmax(sc, axis=-1).astype(vf.dtype)
        return jnp.einsum('bhqk,bhkd->bhqd', p, vf)

    o = lax.map(attend, (qb, jnp.arange(nb, dtype=jnp.int32) * Q_BLOCK))
    return o.transpose(1, 0, 3, 2, 4).reshape(bsz, s, MLA_WIDTH)


def peer_ffn(h, w_pq, sub_keys, expert_u, expert_v):
    bsz, s, d = h.shape
    ht = h.reshape(bsz * s // TOKEN_BLOCK, TOKEN_BLOCK, d)

    def block(xb):
        q = (xb @ w_pq).reshape(TOKEN_BLOCK, PEER_HEADS, 2, PEER_HALF)
        sc = jnp.einsum('thpd,phkd->thpk', q, sub_keys).astype(jnp.float32)
        s_top, i_top = lax.top_k(sc, PEER_TOPK)
        cand = (s_top[:, :, 0, :, None] + s_top[:, :, 1, None, :]).reshape(
            TOKEN_BLOCK, PEER_HEADS, PEER_TOPK * PEER_TOPK)
        cand_idx = (i_top[:, :, 0, :, None] * N_KEYS + i_top[:, :, 1, None, :]).reshape(
            TOKEN_BLOCK, PEER_HEADS, PEER_TOPK * PEER_TOPK)
        best, pos = lax.top_k(cand, PEER_TOPK)
        idx = jnp.take_along_axis(cand_idx, pos, axis=-1)
        gate = jax.nn.softmax(best, axis=-1)
        u = expert_u[idx]
        act = jax.nn.gelu(jnp.einsum('td,thkd->thk', xb, u).astype(jnp.float32))
        v = expert_v[idx]
        return jnp.einsum('thk,thkd->td', (gate * act).astype(xb.dtype), v)

    return lax.map(block, ht).reshape(bsz, s, d)


def setup_inputs(seed: int = 0) -> dict:
    key = jax.random.key(seed)
    ks = jax.random.split(key, 24)
    f32 = jnp.float32
    D = D_MODEL
    nrm = lambda k, shp, sc: jax.random.normal(k, shp, f32) * sc
    x = nrm(ks[0], (BATCH, SEQ, D), 1.0)
    c = nrm(ks[1], (BATCH, D), 1.0)
    start = jax.random.randint(ks[2], (BATCH, 1), 0, 4096, dtype=jnp.int32)
    positions = (start + jnp.arange(SEQ, dtype=jnp.int32)[None, :]).astype(jnp.int32)
    ln_mix_g = 1.0 + nrm(ks[3], (DEPTH, D), 0.02)
    w_in = nrm(ks[4], (DEPTH, D, IN_DIM), D ** -0.5)
    conv_w = nrm(ks[5], (DEPTH, CONV_WIDTH, 3 * GDN_WIDTH), CONV_WIDTH ** -0.5)
    a_log = jnp.log(jax.random.uniform(ks[6], (DEPTH, GDN_HEADS), f32, 1.0, 16.0))
    dt = jnp.exp(jax.random.uniform(ks[7], (DEPTH, GDN_HEADS), f32, np.log(1e-3), np.log(1e-1)))
    dt_bias = dt + jnp.log(-jnp.expm1(-dt))
    gdn_norm_g = 1.0 + nrm(ks[8], (DEPTH, GDN_HEAD_DIM), 0.02)
    q_norm_g = 1.0 + nrm(ks[9], (DEPTH, Q_LORA_RANK), 0.02)
    w_uq = nrm(ks[10], (DEPTH, Q_LORA_RANK, MLA_HEADS * (QK_NOPE_DIM + QK_ROPE_DIM)), Q_LORA_RANK ** -0.5)
    kv_norm_g = 1.0 + nrm(ks[11], (DEPTH, KV_LORA_RANK), 0.02)
    w_ukv = nrm(ks[12], (DEPTH, KV_LORA_RANK, MLA_HEADS * (QK_NOPE_DIM + V_HEAD_DIM)), KV_LORA_RANK ** -0.5)
    w_out = nrm(ks[13], (DEPTH, MIX_WIDTH, D), MIX_WIDTH ** -0.5)
    ln_ffn_g = 1.0 + nrm(ks[14], (DEPTH, D), 0.02)
    w_pq = nrm(ks[15], (DEPTH, D, PEER_HEADS * PEER_KEY_DIM), D ** -0.5)
    sub_keys = nrm(ks[16], (DEPTH, 2, PEER_HEADS, N_KEYS, PEER_HALF), PEER_HALF ** -0.5)
    expert_u = nrm(ks[17], (DEPTH, N_EXPERTS, D), D ** -0.5)
    expert_v = nrm(ks[18], (DEPTH, N_EXPERTS, D), PEER_HEADS ** -0.5)
    w_ada = nrm(ks[19], (DEPTH, D, N_MOD * D), D ** -0.5)
    b_ada = nrm(ks[20], (DEPTH, N_MOD * D), 0.02)
    final_norm_g = 1.0 + nrm(ks[21], (D,), 0.02)
    return {"x": x, "c": c, "positions": positions, "ln_mix_g": ln_mix_g, "w_in": w_in,
            "conv_w": conv_w, "a_log": a_log, "dt_bias": dt_bias, "gdn_norm_g": gdn_norm_g,
            "q_norm_g": q_norm_g, "w_uq": w_uq, "kv_norm_g": kv_norm_g, "w_ukv": w_ukv,
            "w_out": w_out, "ln_ffn_g": ln_ffn_g, "w_pq": w_pq, "sub_keys": sub_keys,
            "expert_u": expert_u, "expert_v": expert_v, "w_ada": w_ada, "b_ada": b_ada,
            "final_norm_g": final_norm_g}


def reference(x, c, positions, ln_mix_g, w_in, conv_w, a_log, dt_bias, gdn_norm_g,
              q_norm_g, w_uq, kv_norm_g, w_ukv, w_out, ln_ffn_g, w_pq, sub_keys,
              expert_u, expert_v, w_ada, b_ada, final_norm_g):
    half = QK_ROPE_DIM // 2
    inv_freq = ROPE_THETA ** (-jnp.arange(half, dtype=jnp.float32) / half)
    ang = positions.astype(jnp.float32)[..., None] * inv_freq
    cos, sin = jnp.cos(ang), jnp.sin(ang)
    sizes = [GDN_WIDTH] * 4 + [GDN_HEADS] * 2 + [Q_LORA_RANK, KV_LORA_RANK, QK_ROPE_DIM]
    split_at = [int(v) for v in np.cumsum(sizes)[:-1]]
    c_act = jax.nn.silu(c)
    for layer in range(DEPTH):
        mod = (c_act @ w_ada[layer] + b_ada[layer])[:, None, :]
        sh1, sc1, gt1, sh2, sc2, gt2 = jnp.split(mod, N_MOD, axis=-1)
        h = modulate(rms_norm(x, ln_mix_g[layer]), sh1, sc1)
        proj = h @ w_in[layer]
        gq, gk, gv, gz, ga, gb, cq, ckv, kr = jnp.split(proj, split_at, axis=-1)
        o_gdn = gdn_mixer(gq, gk, gv, gz, ga, gb, conv_w[layer], a_log[layer],
                          dt_bias[layer], gdn_norm_g[layer])
        o_mla = mla_mixer(cq, ckv, kr, cos, sin, q_norm_g[layer], w_uq[layer],
                          kv_norm_g[layer], w_ukv[layer])
        mixed = jnp.concatenate([o_gdn, o_mla.astype(o_gdn.dtype)], axis=-1) @ w_out[layer]
        x = x + gt1 * mixed
        h = modulate(rms_norm(x, ln_ffn_g[layer]), sh2, sc2)
        x = x + gt2 * peer_ffn(h, w_pq[layer], sub_keys[layer], expert_u[layer], expert_v[layer])
    return rms_norm(x, final_norm_g)
```

```python
import math
import numpy as np
from contextlib import ExitStack
import concourse.bass as bass
import concourse.mybir as mybir
from concourse.bass_utils import run_bass_kernel_spmd

F32 = mybir.dt.float32
BF16 = mybir.dt.bfloat16
I32 = mybir.dt.int32
U32 = mybir.dt.uint32
AF = mybir.ActivationFunctionType
ALU = mybir.AluOpType
AX = mybir.AxisListType

SEM_CH = 30000
EPS = 1e-6
D = 1024
IN_DIM = 2760
NEG = -30000.0


class Res:
    __slots__ = ("name", "w", "r", "acc", "n_in", "n_out", "excl")

    def __init__(self, name, acc=False, excl=False):
        self.name = name
        self.excl = excl
        self.w = {}
        self.r = {}
        self.acc = acc
        self.n_in = 0
        self.n_out = 0


class KB:
    ENGS = ("pe", "act", "dve", "pool", "sp")

    def __init__(self, nc, es):
        self.nc = nc
        self.es = es
        self.lists = {e: [] for e in self.ENGS}
        self.count = {e: 0 for e in self.ENGS}
        self.seen = {e: {} for e in self.ENGS}
        self.sems = {}
        self.dma_keys = {}
        self.nsem = 0
        self.ninst = 0

    def sb(self, name, shape, dt, es=None):
        t = (es or self.es).enter_context(self.nc.sbuf_tensor("sb_" + name, list(shape), dt))
        return t, Res(name)

    def ps(self, name, shape, dt, es=None):
        t = (es or self.es).enter_context(self.nc.psum_tensor("ps_" + name, list(shape), dt))
        return t, Res(name, excl=True)

    def _sem(self, key):
        s = self.sems.get(key)
        if s is None:
            self.nsem += 1
            s = self.es.enter_context(self.nc.semaphore("s%d" % self.nsem))
            self.sems[key] = s
        return s

    def _engsem(self, eng, g):
        ch = (g - 1) // SEM_CH
        return self._sem(("eng", eng, ch)), (g - 1) % SEM_CH + 1

    def _deps(self, eng, reads, writes):
        deps = {}
        for r in reads:
            for k, v in r.w.items():
                if deps.get(k, 0) < v:
                    deps[k] = v
        for w in writes:
            if w.acc:
                continue
            for d in (w.w, w.r):
                for k, v in d.items():
                    if deps.get(k, 0) < v:
                        deps[k] = v
        waits = []
        seen = self.seen[eng]
        for k, v in deps.items():
            if eng == "pe" and k == ("e", "pe"):
                continue
            if seen.get(k, 0) >= v:
                continue
            seen[k] = v
            waits.append((k, v))
        return waits

    def _emit_waits(self, eng, waits):
        L = self.lists[eng]
        for k, v in waits:
            if k[0] == "e":
                L.append(("we", k[1], v))
            else:
                L.append(("w", self._sem(k), v))
            self.ninst += 1

    def _mark(self, key, val, reads, writes):
        for r in reads:
            r.r[key] = val
        for w in writes:
            if w.acc:
                w.w[key] = val
            else:
                w.w = {key: val}
                w.r = {}

    def op(self, eng, reads, writes, meth, *args, **kw):
        if any(r.excl for r in reads):
            writes = list(writes) + [r for r in reads if r.excl]
            reads = [r for r in reads if not r.excl]
        waits = self._deps(eng, reads, writes)
        self._emit_waits(eng, waits)
        self.count[eng] += 1
        g = self.count[eng]
        self.lists[eng].append(("ie", meth, args, kw, eng, g))
        self.ninst += 1
        self._mark(("e", eng), g, reads, writes)

    def dma(self, eng, reads, writes, slot, store, meth, *args, **kw):
        waits = self._deps(eng, reads, writes)
        self._emit_waits(eng, waits)
        if store:
            key = ("do", id(slot))
            slot.n_out += 16
            val = slot.n_out
        else:
            key = ("di", id(slot))
            slot.n_in += 16
            val = slot.n_in
        sem = self._sem(key)
        self.dma_keys[key] = val
        self.lists[eng].append(("i", meth, args, kw, sem, 16))
        self.ninst += 1
        self._mark(key, val, reads, writes)

    def load(self, eng, dst_ap, src_ap, slot, reads=()):
        self.dma(eng, list(reads), [slot], slot, False, "dma_start", out=dst_ap, in_=src_ap)

    def store(self, eng, dst_ap, src_ap, slot, dres=None):
        self.dma(eng, [slot], [dres] if dres is not None else [], slot, True, "dma_start", out=dst_ap, in_=src_ap)

    def barrier(self):
        toks = {}
        for e in self.ENGS:
            if self.count[e] > 0:
                toks[("e", e)] = self.count[e]
        toks.update(self.dma_keys)
        for e in self.ENGS:
            waits = []
            for k, v in toks.items():
                if k == ("e", e) and e == "pe":
                    continue
                if self.seen[e].get(k, 0) >= v:
                    continue
                self.seen[e][k] = v
                waits.append((k, v))
            self._emit_waits(e, waits)

    def finish(self):
        nc = self.nc
        lists = self.lists

        waited = {e: set() for e in self.ENGS}
        for L in lists.values():
            for it in L:
                if it[0] == "we":
                    waited[it[1]].add(it[2])
        rank = {e: {g: i + 1 for i, g in enumerate(sorted(waited[e]))} for e in self.ENGS}

        def replay(engine, L):
            for it in L:
                if it[0] == "w":
                    engine.wait_ge(it[1], it[2])
                elif it[0] == "we":
                    sem, lv = self._engsem(it[1], rank[it[1]][it[2]])
                    engine.wait_ge(sem, lv)
                elif it[0] == "ie":
                    ins = getattr(engine, it[1])(*it[2], **it[3])
                    r = rank[it[4]].get(it[5])
                    if r is not None:
                        sem, lv = self._engsem(it[4], r)
                        ins.then_inc(sem, 1)
                else:
                    ins = getattr(engine, it[1])(*it[2], **it[3])
                    ins.then_inc(it[4], it[5])

        with nc.Block() as block:
            @block.tensor
            def _(e):
                replay(e, lists["pe"])

            @block.scalar
            def _(e):
                replay(e, lists["act"])

            @block.vector
            def _(e):
                replay(e, lists["dve"])

            @block.gpsimd
            def _(e):
                replay(e, lists["pool"])

            @block.sync
            def _(e):
                replay(e, lists["sp"])


C_ID, C_U, C_SL0, C_SL1, C_LS, C_PM, C_NA, C_CN, C_ONE = [i * 128 for i in range(9)]
C_IND0 = 9 * 128
C_IND1 = C_IND0 + 1
C_INVF = C_IND0 + 2
C_IOTA = C_IND0 + 3
NCONST = C_IOTA + 16


def make_consts():
    c = np.zeros((128, NCONST), np.float32)
    i = np.arange(128)
    ch = i // 64
    c[:, C_ID:C_ID + 128] = np.eye(128)
    c[:, C_U:C_U + 128] = ((ch[:, None] == ch[None, :]) & (i[:, None] <= i[None, :]))
    c[63, C_SL0:C_SL0 + 128] = 1.0
    c[127, C_SL1:C_SL1 + 128] = 1.0
    c[:, C_LS:C_LS + 128] = (i[:, None] == (ch[None, :] * 64 + 63))
    same = ch[:, None] == ch[None, :]
    c[:, C_PM:C_PM + 128] = np.where(same & (i[None, :] < i[:, None]), 0.0, -NEG)
    c[:, C_NA:C_NA + 128] = np.where(same & (i[:, None] <= i[None, :]), 0.0, NEG)
    c[:, C_CN:C_CN + 128] = np.where(i[:, None] <= i[None, :], 0.0, NEG)
    c[:, C_ONE:C_ONE + 128] = 1.0
    c[:, C_IND0] = (i < 64)
    c[:, C_IND1] = (i >= 64)
    half = 32
    c[:, C_INVF] = (10000.0 ** (-(np.arange(128) % half).astype(np.float64) / half)).astype(np.float32)
    c[:, C_IOTA:C_IOTA + 16] = np.arange(16)[None, :]
    return c


V_C, V_LNM, V_LNF, V_QG, V_KVG, V_CW = 0, 8, 16, 24, 27, 29
NVEC = V_CW + 48
R_ALOG, R_DTB, R_GG = 0, 4, 8
NROW = 136

_CACHE = {}
GDN_NST = 99
GDN_GATES = True


def build(S, dbg=False):
    NT = S // 128
    nc = bass.Bass("TRN2", target_bir_lowering=False)
    okind = "ExternalOutput" if dbg else "Internal"

    def din(name, shape, dt=F32):
        return nc.dram_tensor(name, list(shape), dt, kind="ExternalInput")

    def dsc(name, shape, dt):
        return nc.dram_tensor(name, list(shape), dt, kind=okind)

    x_d = din("x", [S, D]); pos_d = din("pos", [S], I32)
    consts_d = din("consts", [128, NCONST]); vecs_d = din("vecs", [128, NVEC]); rowv_d = din("rowv", [NROW])
    bada_d = din("b_ada", [1, 6144]); fng_d = din("fng", [D]); lnf_d = din("lnf", [D])
    win_d = din("w_in", [D, IN_DIM]); wuq_d = din("w_uq", [384, 768]); wukv_d = din("w_ukv", [256, 1024])
    wout_d = din("w_out", [D, D]); wpq_d = din("w_pq", [D, 2048]); skT_d = din("skT", [128, 2048])
    eu_d = din("expert_u", [16384, D]); ev_d = din("expert_v", [16384, D]); wada_d = din("w_ada", [D, 6144])
    out_d = nc.dram_tensor("out", [S, D], F32, kind="ExternalOutput")
    mod_d = dsc("mod_s", [6144], F32)
    qkvz_d = dsc("qkvz_s", [S, 2048], BF16)
    gab_d = dsc("gab_s", [S, 8], F32)
    qnT_d = dsc("qnT_s", [4, 128, S], BF16)
    qrT_d = dsc("qrT_s", [4, 65, S], BF16)
    knT_d = dsc("knT_s", [4, 128, S], BF16)
    krT_d = dsc("krT_s", [65, S], BF16)
    v_d = dsc("v_s", [4, S, 129], BF16)
    omixT_d = dsc("omixT_s", [D, S], BF16)
    uvb_d = nc.dram_tensor("uvb_s", [16384, 2 * D], BF16)

    with ExitStack() as es:
        kb = KB(nc, es)
        R = lambda n: Res(n, acc=True)
        mod_r, qkvz_r, gab_r, qnT_r, qrT_r, knT_r, krT_r, v_r, omix_r, uvb_r = [R(n) for n in
            ("mod", "qkvz", "gab", "qnT", "qrT", "knT", "krT", "v", "omix", "uvb")]
        cst, cst_r = kb.sb("cst", [128, NCONST], F32)
        vecs, vecs_r = kb.sb("vecs", [128, NVEC], F32)
        rowv, rowv_r = kb.sb("rowv", [128, NROW], F32)
        modT, modT_r = kb.sb("modT", [128, 48], F32)
        AB1, AB1_r = kb.sb("AB1", [128, 32], F32)
        idb, idb_r = kb.sb("idb", [128, 128], BF16)
        oneb, oneb_r = kb.sb("oneb", [128, 128], BF16)
        kmax, kmax_r = kb.sb("kmax", [128, 8], F32)
        kb.load("sp", cst[:], consts_d.ap(), cst_r)
        kb.load("sp", vecs[:], vecs_d.ap(), vecs_r)
        kb.load("sp", rowv[:], rowv_d.ap().partition_broadcast(128), rowv_r)
        kb.op("dve", [cst_r], [idb_r], "tensor_copy", idb[:], cst[:, C_ID:C_ID + 128])
        kb.op("dve", [cst_r], [oneb_r], "tensor_copy", oneb[:], cst[:, C_ONE:C_ONE + 128])
        kb.op("dve", [], [kmax_r], "memset", kmax[:], 0.0)
        ident = cst[:, C_ID:C_ID + 128]
        ones = cst[:, C_ONE:C_ONE + 128]

        with ExitStack() as p0:
            cact, cact_r = kb.sb("cact", [128, 8], F32, p0)
            wst = [kb.sb("wst%d" % i, [128, 6144], F32, p0) for i in range(2)]
            mrow, mrow_r = kb.sb("mrow", [1, 6144], F32, p0)
            brow, brow_r = kb.sb("brow", [1, 6144], F32, p0)
            pm = [kb.ps("pm%d" % i, [1, 512], F32, p0) for i in range(4)]
            kb.op("act", [vecs_r], [cact_r], "activation", cact[:], vecs[:, V_C:V_C + 8], AF.Silu)
            kb.load("sp", brow[:], bada_d.ap(), brow_r)
            for half in range(3):
                for kt in range(8):
                    w, wr = wst[kt % 2]
                    kb.load("sp" if kt % 2 == 0 else "pool", w[:, 0:2048], wada_d.ap()[kt * 128:(kt + 1) * 128, half * 2048:(half + 1) * 2048], wr)
                    for j in range(4):
                        kb.op("pe", [cact_r, wr], [pm[j][1]], "matmul", pm[j][0][:], cact[:, kt:kt + 1], w[:, j * 512:(j + 1) * 512], start=(kt == 0), stop=(kt == 7))
                for j in range(4):
                    c0 = half * 2048 + j * 512
                    kb.op("dve", [pm[j][1], brow_r], [mrow_r], "tensor_tensor", mrow[:, c0:c0 + 512], pm[j][0][:], brow[:, c0:c0 + 512], ALU.add)
            kb.store("sp", mod_d.ap().rearrange("(o n) -> o n", o=1), mrow[:], mrow_r, mod_r)
            kb.barrier()
            kb.dma("sp", [mod_r], [modT_r], modT_r, False, "dma_start", out=modT[:], in_=mod_d.ap().rearrange("(j p) -> p j", p=128), allow_slow_non_contiguous=True)
            kb.op("dve", [modT_r, vecs_r], [AB1_r], "scalar_tensor_tensor", AB1[:, 0:8], modT[:, 8:16], 1.0, vecs[:, V_LNM:V_LNM + 8], ALU.add, ALU.mult)
            kb.op("dve", [modT_r], [AB1_r], "tensor_copy", AB1[:, 8:16], modT[:, 0:8])
            kb.op("dve", [modT_r, vecs_r], [AB1_r], "scalar_tensor_tensor", AB1[:, 16:24], modT[:, 32:40], 1.0, vecs[:, V_LNF:V_LNF + 8], ALU.add, ALU.mult)
            kb.op("dve", [modT_r], [AB1_r], "tensor_copy", AB1[:, 24:32], modT[:, 24:32])
            kb.barrier()

        with ExitStack() as pa:
            win, win_r = kb.sb("win", [128, 8, 2824], BF16, pa)
            wuq, wuq_r = kb.sb("wuq", [128, 3, 1024], BF16, pa)
            wukv, wukv_r = kb.sb("wukv", [128, 2, 1024], BF16, pa)
            stg = [kb.sb("stg%d" % i, [128, 2760], F32, pa) for i in range(2)]
            for kt in range(8):
                s_, sr = stg[kt % 2]
                kb.load("sp", s_[:], win_d.ap()[kt * 128:(kt + 1) * 128, :], sr)
                kb.op("act" if kt % 2 else "dve", [sr], [win_r], "activation" if kt % 2 else "tensor_copy", *((win[:, kt, 0:2760], s_[:], AF.Copy) if kt % 2 else (win[:, kt, 0:2760], s_[:])))
                kb.op("dve", [sr], [win_r], "tensor_scalar", win[:, kt, 2760:2792], s_[:, 2728:2760], -1.0, None, ALU.mult)
                kb.op("dve", [sr], [win_r], "tensor_copy", win[:, kt, 2792:2824], s_[:, 2696:2728])
            for kt in range(3):
                s_, sr = stg[kt % 2]
                kb.load("sp", s_[:, 0:768], wuq_d.ap()[kt * 128:(kt + 1) * 128, :], sr)
                kb.op("dve", [sr], [wuq_r], "tensor_copy", wuq[:, kt, 0:768], s_[:, 0:768])
                for h in range(4):
                    b0 = h * 192 + 128
                    kb.op("dve", [sr], [wuq_r], "tensor_scalar", wuq[:, kt, 768 + h * 64:768 + h * 64 + 32], s_[:, b0 + 32:b0 + 64], -1.0, None, ALU.mult)
                    kb.op("dve", [sr], [wuq_r], "tensor_copy", wuq[:, kt, 768 + h * 64 + 32:768 + h * 64 + 64], s_[:, b0:b0 + 32])
            for kt in range(2):
                s_, sr = stg[(kt + 1) % 2]
                kb.load("sp", s_[:, 0:1024], wukv_d.ap()[kt * 128:(kt + 1) * 128, :], sr)
                kb.op("dve", [sr], [wukv_r], "tensor_copy", wukv[:, kt, :], s_[:, 0:1024])

            xt = [kb.sb("xt%d" % i, [128, D], F32, pa) for i in range(2)]
            junk, junk_r = kb.sb("junkA", [128, D], F32, pa)
            st1, st1_r = kb.sb("st1", [128, 16], F32, pa)
            xn, xn_r = kb.sb("xnA", [128, D], BF16, pa)
            hT, hT_r = kb.sb("hTA", [128, 8, 128], BF16, pa)
            cin, cin_r = kb.sb("cin", [128, 12, 131], F32, pa)
            cv, cv_r = kb.sb("cv", [128, 12, 128], F32, pa)
            cs, cs_r = kb.sb("cs", [128, 12, 128], BF16, pa)
            stA = [kb.sb("stA%d" % i, [128, 2048], BF16, pa) for i in range(2)]
            gst = [kb.sb("gst%d" % i, [128, 8], F32, pa) for i in range(2)]
            cn, cn_r = kb.sb("cn", [128, 640], BF16, pa)
            cnT, cnT_r = kb.sb("cnT", [128, 5, 128], BF16, pa)
            qst = [kb.sb("qst%d" % i, [128, 4, 128], BF16, pa) for i in range(2)]
            kst = [kb.sb("kst%d" % i, [128, 4, 128], BF16, pa) for i in range(2)]
            rst = [kb.sb("rst%d" % i, [65, 4, 128], BF16, pa) for i in range(2)]
            krst = [kb.sb("krst%d" % i, [65, 128], BF16, pa) for i in range(2)]
            vst = [kb.sb("vst%d" % i, [128, 4, 129], BF16, pa) for i in range(2)]
            sq1, sq1_r = kb.sb("sq1", [128, 4, 128], BF16, pa)
            sq2, sq2_r = kb.sb("sq2", [64, 4, 128], BF16, pa)
            posi, posi_r = kb.sb("posi", [64, 128], I32, pa)
            tr, tr_r = kb.sb("trig", [64, 6, 128], F32, pa)
            t12, t12_r = kb.sb("t12", [64, 2, 4, 128], F32, pa)
            tmx, tmx_r = kb.sb("tmx", [128, 4], F32, pa)
            pT, pT_r = kb.ps("pTA", [128, 8, 128], BF16, pa)
            pF = [kb.ps("pFA%d" % i, [128, 4, 128], F32, pa) for i in range(2)]
            pK, pK_r = kb.ps("pKA", [64, 2, 128], F32, pa)
            pZ, pZ_r = kb.ps("pZA", [128, 512], F32, pa)
            pC2, pC2_r = kb.ps("pC2A", [128, 392], F32, pa)
            pC3, pC3_r = kb.ps("pC3A", [128, 256], F32, pa)
            pR, pR_r = kb.ps("pRA", [64, 4, 128], F32, pa)
            kb.op("pool", [], [cin_r], "memset", cin[:], 0.0)
            for i in range(2):
                kb.op("pool", [], [vst[i][1]], "memset", vst[i][0][:], 1.0)
                kb.op("pool", [], [krst[i][1]], "memset", krst[i][0][:], 1.0)
                kb.op("pool", [], [rst[i][1]], "memset", rst[i][0][:], 0.0)
            cwv = vecs[:, V_CW:V_CW + 48]
            QS = 192.0 ** -0.5
            for ti in range(NT):
                t0 = ti * 128
                b = ti % 2
                x_, xr = xt[b]
                if ti == 0:
                    kb.load("sp", x_[:], x_d.ap()[t0:t0 + 128, :], xr)
                if ti + 1 < NT:
                    kb.load("sp", xt[(ti + 1) % 2][0][:], x_d.ap()[t0 + 128:t0 + 256, :], xt[(ti + 1) % 2][1])
                kb.load("pool", posi[:], pos_d.ap()[t0:t0 + 128].partition_broadcast(64), posi_r)
                kb.op("act", [xr], [junk_r, st1_r], "activation", junk[:], x_[:], AF.Square, accum_out=st1[:, 0:1])
                kb.op("act", [st1_r], [st1_r], "activation", st1[:, 1:2], st1[:, 0:1], AF.Sqrt, bias=EPS, scale=1.0 / D)
                kb.op("dve", [st1_r], [st1_r], "reciprocal", st1[:, 2:3], st1[:, 1:2])
                kb.op("act", [xr, st1_r], [xn_r], "activation", xn[:], x_[:], AF.Copy, scale=st1[:, 2:3])
                for kt in range(8):
                    kb.op("pe", [xn_r, idb_r], [pT_r], "transpose", pT[:, kt, :], xn[:, kt * 128:(kt + 1) * 128], idb[:])
                kb.op("dve", [pT_r, AB1_r], [hT_r], "tensor_tensor", hT[:], pT[:], AB1[:, 0:8].unsqueeze(2).to_broadcast([128, 8, 128]), ALU.mult)
                kb.op("dve", [hT_r, AB1_r], [hT_r], "tensor_tensor", hT[:], hT[:], AB1[:, 8:16].unsqueeze(2).to_broadcast([128, 8, 128]), ALU.add)
                for grp in range(3):
                    pf, pfr = pF[grp % 2]
                    for j in range(4):
                        ct = grp * 4 + j
                        for kt in range(8):
                            kb.op("pe", [hT_r, win_r], [pfr], "matmul", pf[:, j, :], win[:, kt, ct * 128:(ct + 1) * 128], hT[:, kt, :], start=(kt == 0), stop=(kt == 7))
                    kb.op("act", [pfr], [cin_r], "activation", cin[:, grp * 4:(grp + 1) * 4, 3:131], pf[:], AF.Copy)
                for j in range(2):
                    for kt in range(8):
                        kb.op("pe", [hT_r, win_r], [pK_r], "matmul", pK[:, j, :], win[:, kt, 2696 + 64 * j + (0 if j == 0 else 0):2696 + 64 * j + 64], hT[:, kt, :], start=(kt == 0), stop=(kt == 7))
                for kt in range(8):
                    kb.op("pe", [hT_r, win_r], [pZ_r], "matmul", pZ[:], hT[:, kt, :], win[:, kt, 1536:2048], start=(kt == 0), stop=(kt == 7))
                for kt in range(8):
                    kb.op("pe", [hT_r, win_r], [pC2_r], "matmul", pC2[:], hT[:, kt, :], win[:, kt, 2048:2440], start=(kt == 0), stop=(kt == 7))
                for kt in range(8):
                    kb.op("pe", [hT_r, win_r], [pC3_r], "matmul", pC3[:], hT[:, kt, :], win[:, kt, 2440:2696], start=(kt == 0), stop=(kt == 7))
                sa, sar = stA[b]
                kb.op("act", [pZ_r], [sar], "activation", sa[:, 1536:2048], pZ[:], AF.Silu)
                g_, gr_ = gst[b]
                kb.op("dve", [pC2_r], [gr_], "tensor_copy", g_[:], pC2[:, 0:8])
                kb.store("sp", gab_d.ap()[t0:t0 + 128, :], g_[:], gr_, gab_r)
                kb.op("act", [pC2_r], [junk_r, st1_r], "activation", junk[:, 0:384], pC2[:, 8:392], AF.Square, accum_out=st1[:, 4:5])
                kb.op("act", [pC3_r], [junk_r, st1_r], "activation", junk[:, 384:640], pC3[:], AF.Square, accum_out=st1[:, 5:6])
                kb.op("act", [st1_r], [st1_r], "activation", st1[:, 6:7], st1[:, 4:5], AF.Sqrt, bias=EPS, scale=1.0 / 384)
                kb.op("act", [st1_r], [st1_r], "activation", st1[:, 7:8], st1[:, 5:6], AF.Sqrt, bias=EPS, scale=1.0 / 256)
                kb.op("dve", [st1_r], [st1_r], "reciprocal", st1[:, 8:10], st1[:, 6:8])
                kb.op("act", [pC2_r, st1_r], [cn_r], "activation", cn[:, 0:384], pC2[:, 8:392], AF.Copy, scale=st1[:, 8:9])
                kb.op("act", [pC3_r, st1_r], [cn_r], "activation", cn[:, 384:640], pC3[:], AF.Copy, scale=st1[:, 9:10])
                for j in range(5):
                    kb.op("pe", [cn_r, idb_r], [pT_r], "transpose", pT[:, j, :], cn[:, j * 128:(j + 1) * 128], idb[:])
                kb.op("dve", [pT_r, vecs_r], [cnT_r], "tensor_tensor", cnT[:], pT[:, 0:5, :], vecs[:, V_QG:V_QG + 5].unsqueeze(2).to_broadcast([128, 5, 128]), ALU.mult)
                kb.op("dve", [posi_r], [tr_r], "tensor_copy", tr[:, 0, :], posi[:])
                kb.op("dve", [tr_r, cst_r], [tr_r], "tensor_scalar", tr[:, 0, :], tr[:, 0, :], cst[0:64, C_INVF:C_INVF + 1], None, ALU.mult)
                for which, off in ((4, 0.0), (5, math.pi / 2)):
                    kb.op("dve", [tr_r], [tr_r], "tensor_scalar", tr[:, 1, :], tr[:, 0, :], off, 1.0 / (2 * math.pi), ALU.add, ALU.mult)
                    kb.op("dve", [tr_r], [tr_r], "tensor_scalar", tr[:, 2, :], tr[:, 1, :], 12582912.0, None, ALU.add)
                    kb.op("dve", [tr_r], [tr_r], "tensor_scalar", tr[:, 2, :], tr[:, 2, :], -12582912.0, None, ALU.add)
                    kb.op("dve", [tr_r], [tr_r], "tensor_tensor", tr[:, 3, :], tr[:, 1, :], tr[:, 2, :], ALU.subtract)
                    kb.op("dve", [tr_r], [tr_r], "tensor_scalar", tr[:, 3, :], tr[:, 3, :], -0.4999, 0.4999, ALU.max, ALU.min)
                    kb.op("act", [tr_r], [tr_r], "activation", tr[:, which, :], tr[:, 3, :], AF.Sin, scale=2 * math.pi)
                sinT = tr[:, 4, :]
                cosT = tr[:, 5, :]
                pq, pqr = pF[0]
                for h in range(4):
                    for kt in range(3):
                        kb.op("pe", [cnT_r, wuq_r], [pqr], "matmul", pq[:, h, :], wuq[:, kt, h * 192:h * 192 + 128], cnT[:, kt, :], start=(kt == 0), stop=(kt == 2))
                for h in range(4):
                    for kt in range(3):
                        kb.op("pe", [cnT_r, wuq_r], [pR_r], "matmul", pR[:, h, :], wuq[:, kt, h * 192 + 128:h * 192 + 192], cnT[:, kt, :], start=(kt == 0), stop=(kt == 2))
                    for kt in range(3):
                        kb.op("pe", [cnT_r, wuq_r], [pZ_r], "matmul", pZ[0:64, h * 128:(h + 1) * 128], wuq[:, kt, 768 + h * 64:768 + h * 64 + 64], cnT[:, kt, :], start=(kt == 0), stop=(kt == 2))
                q_, qr_ = qst[b]
                kb.op("act", [pqr], [qr_], "activation", q_[:], pq[:], AF.Copy, scale=QS)
                kb.op("act", [pqr], [sq1_r], "activation", sq1[:], pq[:], AF.Square, scale=QS)
                kb.store("pool", qnT_d.ap()[:, :, t0:t0 + 128].rearrange("h p t -> p h t"), q_[:], qr_, qnT_r)
                r_, rr_ = rst[b]
                kb.op("dve", [pR_r, tr_r], [t12_r], "tensor_tensor", t12[:, 0], pR[:], cosT.unsqueeze(1).to_broadcast([64, 4, 128]), ALU.mult)
                kb.op("dve", [pZ_r, tr_r], [t12_r], "tensor_tensor", t12[:, 1], pZ[0:64, :].rearrange("p (h c) -> p h c", h=4), sinT.unsqueeze(1).to_broadcast([64, 4, 128]), ALU.mult)
                kb.op("dve", [t12_r], [t12_r], "tensor_tensor", t12[:, 0], t12[:, 0], t12[:, 1], ALU.add)
                kb.op("act", [t12_r], [rr_], "activation", r_[0:64], t12[:, 0], AF.Copy, scale=QS)
                kb.op("act", [t12_r], [sq2_r], "activation", sq2[:], t12[:, 0], AF.Square, scale=QS)
                pn, pnr = pF[1]
                kb.op("pe", [sq1_r, oneb_r], [pnr], "matmul", pn[:].rearrange("p a b -> p (a b)"), oneb[:], sq1[:].rearrange("p a b -> p (a b)"), start=True, stop=False)
                kb.op("pe", [sq2_r, oneb_r], [pnr], "matmul", pn[:].rearrange("p a b -> p (a b)"), oneb[0:64, :], sq2[:].rearrange("p a b -> p (a b)"), start=False, stop=True)
                kb.op("act", [pnr], [rr_], "activation", r_[64:65], pn[64:65], AF.Sqrt, scale=1.0609)
                kb.store("pool", qrT_d.ap()[:, :, t0:t0 + 128].rearrange("h p t -> p h t"), r_[:], rr_, qrT_r)
                pk2, pk2r = pF[0]
                for h in range(4):
                    for kt in range(2):
                        kb.op("pe", [cnT_r, wukv_r], [pk2r], "matmul", pk2[:, h, :], wukv[:, kt, h * 256:h * 256 + 128], cnT[:, 3 + kt, :], start=(kt == 0), stop=(kt == 1))
                k_, kr_ = kst[b]
                kb.op("act", [pk2r], [kr_], "activation", k_[:], pk2[:], AF.Copy)
                kb.op("act", [pk2r], [sq1_r], "activation", sq1[:], pk2[:], AF.Square)
                kb.store("pool", knT_d.ap()[:, :, t0:t0 + 128].rearrange("h p t -> p h t"), k_[:], kr_, knT_r)
                kr2, kr2r = krst[b]
                kb.op("dve", [pK_r, tr_r], [t12_r], "tensor_tensor", t12[:, 0, 0], pK[:, 0, :], cosT, ALU.mult)
                kb.op("dve", [pK_r, tr_r], [t12_r], "tensor_tensor", t12[:, 1, 0], pK[:, 1, :], sinT, ALU.mult)
                kb.op("dve", [t12_r], [t12_r], "tensor_tensor", t12[:, 0, 0], t12[:, 0, 0], t12[:, 1, 0], ALU.add)
                kb.op("act", [t12_r], [kr2r], "activation", kr2[0:64], t12[:, 0, 0], AF.Copy)
                kb.op("act", [t12_r], [sq2_r], "activation", sq2[:, 0, :], t12[:, 0, 0], AF.Square)
                kb.store("pool", krT_d.ap()[:, t0:t0 + 128], kr2[:], kr2r, krT_r)
                pn2, pn2r = pF[1]
                kb.op("pe", [sq1_r, oneb_r], [pn2r], "matmul", pn2[:].rearrange("p a b -> p (a b)"), oneb[:], sq1[:].rearrange("p a b -> p (a b)"), start=True, stop=True)
                kb.op("pe", [sq2_r, oneb_r], [pC3_r], "matmul", pC3[:, 0:128], oneb[0:64, :], sq2[:, 0, :], start=True, stop=True)
                kb.op("dve", [pn2r], [tmx_r], "tensor_reduce", tmx[:], pn2[:], AX.X, ALU.max)
                kb.op("dve", [tmx_r, kmax_r], [kmax_r], "tensor_tensor", kmax[:, 0:4], kmax[:, 0:4], tmx[:], ALU.max)
                kb.op("dve", [pC3_r], [tmx_r], "tensor_reduce", tmx[:, 0:1], pC3[:, 0:128], AX.X, ALU.max)
                kb.op("dve", [tmx_r, kmax_r], [kmax_r], "tensor_tensor", kmax[:, 4:5], kmax[:, 4:5], tmx[:, 0:1], ALU.max)
                for kt in range(2):
                    kb.op("pe", [cnT_r, wukv_r], [pZ_r], "matmul", pZ[:].rearrange("p (h c) -> p h c", h=4), cnT[:, 3 + kt, :], wukv[:, kt, :].rearrange("p (h c) -> p h c", h=4)[:, :, 128:256], start=(kt == 0), stop=(kt == 1))
                v_, vr_ = vst[b]
                kb.op("dve", [pZ_r], [vr_], "tensor_copy", v_[:, :, 0:128], pZ[:].rearrange("p (h c) -> p h c", h=4))
                kb.store("pool", v_d.ap()[:, t0:t0 + 128, :].rearrange("h p c -> p h c"), v_[:], vr_, v_r)
                for ct in range(12):
                    kb.op("dve", [cin_r, vecs_r], [cv_r], "tensor_scalar", cv[:, ct, :], cin[:, ct, 0:128], cwv[:, ct * 4:ct * 4 + 1], None, ALU.mult)
                    for i in range(1, 4):
                        kb.op("dve", [cin_r, vecs_r, cv_r], [cv_r], "scalar_tensor_tensor", cv[:, ct, :], cin[:, ct, i:i + 128], cwv[:, ct * 4 + i:ct * 4 + i + 1], cv[:, ct, :], ALU.mult, ALU.add)
                kb.op("pool", [cin_r], [cin_r], "tensor_copy", cin[:, :, 0:3], cin[:, :, 128:131])
                kb.op("act", [cv_r], [cs_r], "activation", cs[:], cv[:], AF.Silu)
                sa, sar = stA[b]
                for grp in range(2):
                    n = 8 if grp == 0 else 4
                    for j in range(n):
                        ct = grp * 8 + j
                        kb.op("pe", [cs_r, idb_r], [pT_r], "transpose", pT[:, j, :], cs[:, ct, :], idb[:])
                    kb.op("dve", [pT_r], [sar], "tensor_copy", sa[:, grp * 1024:grp * 1024 + n * 128], pT[:, 0:n, :])
                kb.store("sp", qkvz_d.ap()[t0:t0 + 128, :], sa[:], sar, qkvz_r)
            kb.barrier()

        with ExitStack() as pb:
            f32t = lambda n, shp=(128, 128): kb.sb(n, list(shp), F32, pb)
            bft = lambda n, shp=(128, 128): kb.sb(n, list(shp), BF16, pb)
            qz = [bft("qz%d" % i, (128, 2048)) for i in range(3)]
            gabt = [f32t("gabt%d" % i, (128, 8)) for i in range(3)]
            gts = [f32t("gt%d" % i, (128, 64)) for i in range(3)]
            nega, nega_r = f32t("nega", (128, 4))
            o_sb, o_r = f32t("o_sb", (128, 4, 128))
            osq, osq_r = f32t("osq", (128, 4, 128))
            ost, ost_r = f32t("ost", (128, 12))
            omg, omg_r = bft("omg", (128, 4, 128))
            oT = [bft("oT%d" % i, (128, 4, 128)) for i in range(2)]
            pTb = [kb.ps("pTbB%d" % i, [128, 8, 128], BF16, pb) for i in range(2)]
            pG, pG_r = kb.ps("pGB", [128, 512], F32, pb)
            pW = [kb.ps("pWB%d" % h, [128, 4, 128], F32, pb)[0] for h in range(4)]
            pWr = [[Res("pW%d" % h, excl=True)] * 4 for h in range(4)]
            HH = [[], []]
            for h in range(4):
                for par in range(2):
                    Td = {}
                    for n in ("stq",):
                        Td[n] = f32t("%s%d_%d" % (n, h, par), (128, 16))
                    for n in ("kbg", "vb", "dg", "decM", "decAT", "Ma", "Mb", "Na", "Nb", "Pa", "Pb", "MI", "u"):
                        Td[n] = f32t("%s%d_%d" % (n, h, par))
                    for n in ("junk", "kn", "qs", "kd0", "kd1", "dq", "kT", "qT", "qdT", "attnT", "wT"):
                        Td[n] = bft("%s%d_%d" % (n, h, par))
                    if par == 0:
                        Td["S"] = f32t("S%d" % h)
                        Td["vnew"] = bft("vnew%d" % h)
                        Td["Sb"] = bft("Sb%d" % h)
                        kb.op("pool", [], [Td["vnew"][1]], "memset", Td["vnew"][0][:], 0.0)
                        kb.op("pool", [], [Td["S"][1]], "memset", Td["S"][0][:], 0.0)
                    else:
                        for n in ("S", "vnew", "Sb"):
                            Td[n] = HH[0][h][n]
                    HH[par].append(Td)
            kb.op("act", [rowv_r], [nega_r], "activation", nega[:], rowv[:, R_ALOG:R_ALOG + 4], AF.Exp)
            kb.op("dve", [nega_r], [nega_r], "tensor_scalar", nega[:], nega[:], -1.0, None, ALU.mult)
            identf = cst[:, C_ID:C_ID + 128]
            def gdn_tile(ti):
                t0 = ti * 128
                b = ti % 2
                q_, qr_ = qz[ti % 3]
                ga_, gar_ = gabt[ti % 3]
                gt, gtr = gts[ti % 3]
                H = HH[b]
                V = lambda r, w, m, *a, **k: kb.op("dve", r, w, m, *a, **k)
                A = lambda r, w, *a, **k: kb.op("act", r, w, "activation", *a, **k)

                gsteps = []

                def g0():
                    kb.load("sp", q_[:], qkvz_d.ap()[t0:t0 + 128, :], qr_, reads=[qkvz_r])
                    kb.load("sp", ga_[:], gab_d.ap()[t0:t0 + 128, :], gar_, reads=[gab_r])
                    V([gar_, rowv_r], [gtr], "tensor_tensor", gt[:, 0:4], ga_[:, 0:4], rowv[:, R_DTB:R_DTB + 4], ALU.add)
                    V([gtr], [gtr], "tensor_scalar", gt[:, 4:8], gt[:, 0:4], -1.0, None, ALU.mult)
                    V([gtr], [gtr], "tensor_tensor", gt[:, 4:8], gt[:, 4:8], gt[:, 0:4], ALU.max)
                gsteps.append(g0)

                def g1():
                    A([gtr], [gtr], gt[:, 8:12], gt[:, 4:8], AF.Exp, scale=-1.0)
                gsteps.append(g1)

                def g2():
                    V([gtr], [gtr], "tensor_scalar", gt[:, 12:16], gt[:, 8:12], 2.0, None, ALU.add)
                    V([gtr], [gtr], "reciprocal", gt[:, 12:16], gt[:, 12:16])
                    V([gtr], [gtr], "tensor_tensor", gt[:, 12:16], gt[:, 12:16], gt[:, 8:12], ALU.mult)
                    V([gtr], [gtr], "tensor_tensor", gt[:, 52:56], gt[:, 12:16], gt[:, 12:16], ALU.mult)
                    V([gtr], [gtr], "tensor_scalar", gt[:, 56:60], gt[:, 52:56], 1.0 / 11, 1.0 / 9, ALU.mult, ALU.add)
                    for cf in (1.0 / 7, 1.0 / 5, 1.0 / 3, 1.0):
                        V([gtr], [gtr], "tensor_tensor", gt[:, 56:60], gt[:, 56:60], gt[:, 52:56], ALU.mult)
                        V([gtr], [gtr], "tensor_scalar", gt[:, 56:60], gt[:, 56:60], cf, None, ALU.add)
                    V([gtr], [gtr], "scalar_tensor_tensor", gt[:, 12:16], gt[:, 12:16], 2.0, gt[:, 56:60], ALU.mult, ALU.mult)
                    V([gtr], [gtr], "tensor_scalar", gt[:, 16:20], gt[:, 0:4], 0.0, None, ALU.max)
                    V([gtr], [gtr], "tensor_tensor", gt[:, 16:20], gt[:, 16:20], gt[:, 12:16], ALU.add)
                    V([gtr, nega_r], [gtr], "tensor_tensor", gt[:, 20:24], gt[:, 16:20], nega[:], ALU.mult)
                gsteps.append(g2)

                def g3():
                    A([gar_], [gtr], gt[:, 24:28], ga_[:, 4:8], AF.Sigmoid)
                gsteps.append(g3)

                def g4():
                    kb.op("pe", [gtr, cst_r], [pG_r], "matmul", pG[:, 0:4], cst[:, C_U:C_U + 128], gt[:, 20:24], start=True, stop=True)
                gsteps.append(g4)

                def g5():
                    V([pG_r], [gtr], "tensor_copy", gt[:, 28:32], pG[:, 0:4])
                    V([gtr], [gtr], "tensor_scalar", gt[:, 32:36], gt[:, 28:32], -1.0, None, ALU.mult)
                gsteps.append(g5)

                def g6():
                    A([gtr], [gtr], gt[:, 36:40], gt[:, 28:32], AF.Exp)
                gsteps.append(g6)

                def g7():
                    kb.op("pe", [gtr, cst_r], [pG_r], "matmul", pG[:, 4:8], cst[:, C_LS:C_LS + 128], gt[:, 28:32], start=True, stop=True)
                    kb.op("pe", [gtr, cst_r], [pG_r], "matmul", pG[:, 8:12], cst[:, C_SL0:C_SL0 + 128], gt[:, 28:32], start=True, stop=True)
                    kb.op("pe", [gtr, cst_r], [pG_r], "matmul", pG[:, 12:16], cst[:, C_SL1:C_SL1 + 128], gt[:, 28:32], start=True, stop=True)
                gsteps.append(g7)

                def g8():
                    V([pG_r, gtr], [gtr], "tensor_tensor", gt[:, 52:56], pG[:, 4:8], gt[:, 28:32], ALU.subtract)
                gsteps.append(g8)

                def g9():
                    A([gtr], [gtr], gt[:, 40:44], gt[:, 52:56], AF.Exp)
                    A([pG_r], [gtr], gt[:, 44:52], pG[:, 8:16], AF.Exp)
                gsteps.append(g9)

                def g10():
                    V([gtr], [gtr], "tensor_tensor", gt[:, 56:60], gt[:, 24:28], gt[:, 36:40], ALU.mult)
                gsteps.append(g10)


                def s1(h):
                    T = H[h]
                    stq, sr = T["stq"]
                    qh = q_[:, h * 128:(h + 1) * 128]; kh = q_[:, 512 + h * 128:512 + (h + 1) * 128]; vh = q_[:, 1024 + h * 128:1024 + (h + 1) * 128]
                    A([qr_], [T["junk"][1], sr], T["junk"][0][:], qh, AF.Square, accum_out=stq[:, 0:1])
                    A([qr_], [T["junk"][1], sr], T["junk"][0][:], kh, AF.Square, accum_out=stq[:, 1:2])
                    A([sr], [sr], stq[:, 2:4], stq[:, 0:2], AF.Sqrt, bias=EPS)
                    V([sr], [sr], "reciprocal", stq[:, 4:6], stq[:, 2:4])
                    V([sr], [sr], "tensor_scalar", stq[:, 6:7], stq[:, 4:5], 128.0 ** -0.5, None, ALU.mult)
                    V([sr, gtr], [sr], "tensor_tensor", stq[:, 7:8], stq[:, 5:6], gt[:, 40 + h:41 + h], ALU.mult)
                    V([sr, cst_r], [sr], "tensor_scalar", stq[:, 8:10], cst[:, C_IND0:C_IND0 + 2], stq[:, 7:8], None, ALU.mult)
                    V([sr, gtr], [sr], "tensor_tensor", stq[:, 10:11], stq[:, 5:6], gt[:, 56 + h:57 + h], ALU.mult)

                def s1b(h):
                    T = H[h]
                    stq, sr = T["stq"]
                    qh = q_[:, h * 128:(h + 1) * 128]; kh = q_[:, 512 + h * 128:512 + (h + 1) * 128]; vh = q_[:, 1024 + h * 128:1024 + (h + 1) * 128]
                    A([qr_, sr], [T["kn"][1]], T["kn"][0][:], kh, AF.Copy, scale=stq[:, 5:6])
                    A([qr_, sr], [T["qs"][1]], T["qs"][0][:], qh, AF.Copy, scale=stq[:, 6:7])
                    V([qr_, sr], [T["kd0"][1]], "tensor_scalar", T["kd0"][0][:], kh, stq[:, 8:9], None, ALU.mult)
                    V([qr_, sr], [T["kd1"][1]], "tensor_scalar", T["kd1"][0][:], kh, stq[:, 9:10], None, ALU.mult)
                    V([qr_, sr], [T["kbg"][1]], "tensor_scalar", T["kbg"][0][:], kh, stq[:, 10:11], None, ALU.mult)
                    V([qr_, gtr], [T["vb"][1]], "tensor_scalar", T["vb"][0][:], vh, gt[:, 24 + h:25 + h], None, ALU.mult)
                    V([cst_r, gtr], [T["dg"][1]], "tensor_scalar", T["dg"][0][:], identf, gt[:, 28 + h:29 + h], None, ALU.mult)
                    V([idb_r, gtr], [T["dq"][1]], "tensor_scalar", T["dq"][0][:], idb[:], gt[:, 36 + h:37 + h], None, ALU.mult)

                def s2(h):
                    T = H[h]
                    pt, ptr = pTb[h // 2]
                    s0 = (h % 2) * 4
                    kb.op("pe", [T["kn"][1], idb_r], [ptr], "transpose", pt[:, s0, :], T["kn"][0][:], idb[:])
                    kb.op("pe", [T["qs"][1], idb_r], [ptr], "transpose", pt[:, s0 + 1, :], T["qs"][0][:], idb[:])
                    kb.op("pe", [T["qs"][1], T["dq"][1]], [pWr[h][3]], "matmul", pW[h][:, 3, :], T["qs"][0][:], T["dq"][0][:], start=True, stop=True)
                    V([ptr], [T["kT"][1]], "tensor_copy", T["kT"][0][:], pt[:, s0, :])
                    V([ptr], [T["qT"][1]], "tensor_copy", T["qT"][0][:], pt[:, s0 + 1, :])
                    A([pWr[h][3]], [T["qdT"][1]], T["qdT"][0][:], pW[h][:, 3, :], AF.Copy)

                def s3(h):
                    T = H[h]
                    kb.op("pe", [T["kT"][1]], [pWr[h][0]], "matmul", pW[h][:, 0, :], T["kT"][0][:], T["kT"][0][:], start=True, stop=True)
                    kb.op("pe", [T["kT"][1], T["qT"][1]], [pWr[h][1]], "matmul", pW[h][:, 1, :], T["kT"][0][:], T["qT"][0][:], start=True, stop=True)
                    kb.op("pe", [T["dg"][1], cst_r], [pWr[h][2]], "matmul", pW[h][:, 2, :], ones, T["dg"][0][:], start=True, stop=False)
                    kb.op("pe", [cst_r], [pWr[h][2]], "matmul", pW[h][:, 2, :], identf, cst[:, C_PM:C_PM + 128], start=False, stop=True)
                    kb.op("pe", [T["dg"][1], cst_r], [pWr[h][3]], "matmul", pW[h][:, 3, :], ones, T["dg"][0][:], start=True, stop=False)
                    kb.op("pe", [cst_r], [pWr[h][3]], "matmul", pW[h][:, 3, :], identf, cst[:, C_NA:C_NA + 128], start=False, stop=True)

                def s3b(h):
                    T = H[h]
                    A([pWr[h][2], gtr], [T["decM"][1]], T["decM"][0][:], pW[h][:, 2, :], AF.Exp, bias=gt[:, 28 + h:29 + h], scale=-1.0)
                    A([pWr[h][3], gtr], [T["decAT"][1]], T["decAT"][0][:], pW[h][:, 3, :], AF.Exp, bias=gt[:, 32 + h:33 + h], scale=1.0)
                    V([pWr[h][0], gtr, T["decM"][1]], [T["Ma"][1]], "scalar_tensor_tensor", T["Ma"][0][:], pW[h][:, 0, :], gt[:, 24 + h:25 + h], T["decM"][0][:], ALU.mult, ALU.mult)
                    V([pWr[h][1], T["decAT"][1]], [T["attnT"][1]], "tensor_tensor", T["attnT"][0][:], pW[h][:, 1, :], T["decAT"][0][:], ALU.mult)

                def s4(h):
                    T = H[h]
                    kb.op("pe", [T["Ma"][1], cst_r], [pWr[h][0]], "transpose", pW[h][:, 0, :], T["Ma"][0][:], identf)
                    A([pWr[h][0]], [T["Na"][1]], T["Na"][0][:], pW[h][:, 0, :], AF.Copy)
                    V([pWr[h][0], cst_r], [T["Pa"][1]], "scalar_tensor_tensor", T["Pa"][0][:], pW[h][:, 0, :], -1.0, identf, ALU.mult, ALU.add)

                def chain(L):
                    def f(h):
                        T = H[h]
                        cur, nxt = ("a", "b") if L % 2 == 1 else ("b", "a")
                        Mc, Nc, Pc = T["M" + cur], T["N" + cur], T["P" + cur]
                        Mn, Nn, Pn = T["M" + nxt], T["N" + nxt], T["P" + nxt]
                        kb.op("pe", [Mc[1], Nc[1]], [pWr[h][0]], "matmul", pW[h][:, 0, :], Nc[0][:], Mc[0][:], start=True, stop=True)
                        if L < 5:
                            kb.op("pe", [Mc[1], Nc[1]], [pWr[h][1]], "matmul", pW[h][:, 1, :], Mc[0][:], Nc[0][:], start=True, stop=True)
                        V([pWr[h][0], cst_r], [T["MI"][1]], "tensor_tensor", T["MI"][0][:], pW[h][:, 0, :], identf, ALU.add)
                        if L < 5:
                            A([pWr[h][0]], [Mn[1]], Mn[0][:], pW[h][:, 0, :], AF.Copy)
                            A([pWr[h][1]], [Nn[1]], Nn[0][:], pW[h][:, 1, :], AF.Copy)

                    def f2(h):
                        T = H[h]
                        cur, nxt = ("a", "b") if L % 2 == 1 else ("b", "a")
                        Pc, Pn = T["P" + cur], T["P" + nxt]
                        kb.op("pe", [T["MI"][1], Pc[1]], [pWr[h][2]], "matmul", pW[h][:, 2, :], T["MI"][0][:], Pc[0][:], start=True, stop=True)
                        V([pWr[h][2]], [Pn[1]], "tensor_copy", Pn[0][:], pW[h][:, 2, :])
                    return [f, f2]

                def s10(h):
                    T = H[h]
                    TT = T["Pb"]
                    kb.op("pe", [TT[1], T["vb"][1]], [pWr[h][0]], "matmul", pW[h][:, 0, :], TT[0][:], T["vb"][0][:], start=True, stop=True)
                    kb.op("pe", [TT[1], T["kbg"][1]], [pWr[h][1]], "matmul", pW[h][:, 1, :], T["kbg"][0][:], TT[0][:], start=True, stop=True)
                    A([pWr[h][0]], [T["u"][1]], T["u"][0][:], pW[h][:, 0, :], AF.Copy)
                    V([pWr[h][1]], [T["wT"][1]], "tensor_copy", T["wT"][0][:], pW[h][:, 1, :])

                def scan(c):
                    rows = slice(c * 64, (c + 1) * 64)

                    def fa(h):
                        T = H[h]
                        A([T["S"][1]], [T["Sb"][1]], T["Sb"][0][:], T["S"][0][:], AF.Copy)
                        kb.op("pe", [T["wT"][1], T["Sb"][1]], [pWr[h][2]], "matmul", pW[h][:, 2, :], T["wT"][0][:], T["Sb"][0][:], start=True, stop=True)

                    def fb(h):
                        T = H[h]
                        V([T["u"][1], pWr[h][2]], [T["vnew"][1]], "tensor_tensor", T["vnew"][0][rows, :], T["u"][0][rows, :], pW[h][rows, 2, :], ALU.subtract)
                        kb.op("pe", [T["qdT"][1], T["Sb"][1]], [pWr[h][c]], "matmul", pW[h][:, c, :], T["qdT"][0][:], T["Sb"][0][:], start=True, stop=False)
                        kb.op("pe", [T["attnT"][1], T["vnew"][1]], [pWr[h][c]], "matmul", pW[h][:, c, :], T["attnT"][0][:], T["vnew"][0][:], start=False, stop=True)
                        kd = T["kd%d" % c]
                        kb.op("pe", [kd[1], T["vnew"][1]], [pWr[h][3]], "matmul", pW[h][:, 3, :], kd[0][:], T["vnew"][0][:], start=True, stop=True)

                    def fc(h):
                        T = H[h]
                        A([pWr[h][c]], [o_r], o_sb[rows, h, :], pW[h][rows, c, :], AF.Copy)
                        V([T["S"][1], gtr, pWr[h][3]], [T["S"][1]], "scalar_tensor_tensor", T["S"][0][:], T["S"][0][:], gt[:, 44 + c * 4 + h:45 + c * 4 + h], pW[h][:, 3, :], ALU.mult, ALU.add)
                    return [fa, fb, fc]

                def _final():
                    V([o_r], [osq_r], "tensor_tensor", osq[:], o_sb[:], o_sb[:], ALU.mult)
                    V([osq_r], [ost_r], "tensor_reduce", ost[:, 0:4], osq[:], AX.X, ALU.add)
                    A([ost_r], [ost_r], ost[:, 4:8], ost[:, 0:4], AF.Sqrt, bias=EPS, scale=1.0 / 128)
                    V([ost_r], [ost_r], "reciprocal", ost[:, 8:12], ost[:, 4:8])
                    V([o_r, ost_r], [osq_r], "tensor_tensor", osq[:], o_sb[:], ost[:, 8:12].unsqueeze(2).to_broadcast([128, 4, 128]), ALU.mult)
                    V([osq_r, rowv_r], [osq_r], "tensor_tensor", osq[:], osq[:], rowv[:, R_GG:R_GG + 128].unsqueeze(1).to_broadcast([128, 4, 128]), ALU.mult)
                    V([osq_r, qr_], [omg_r], "tensor_tensor", omg[:], osq[:], q_[:, 1536:2048].rearrange("p (h c) -> p h c", h=4), ALU.mult)
                    pt, ptr = pTb[ti % 2]
                    for h in range(4):
                        kb.op("pe", [omg_r, idb_r], [ptr], "transpose", pt[:, (3 if h % 2 == 0 else 7) - (h // 2) * 0 - (0), :] if False else pt[:, [2, 3, 6, 7][h], :], omg[:, h, :], idb[:])
                    ot, otr = oT[b]
                    V([ptr], [otr], "tensor_copy", ot[:, 0:2, :], pt[:, 2:4, :])
                    V([ptr], [otr], "tensor_copy", ot[:, 2:4, :], pt[:, 6:8, :])
                    kb.store("sp", omixT_d.ap()[0:512, t0:t0 + 128].rearrange("(h p) t -> p h t", p=128), ot[:], otr, omix_r)

                pre = []
                stages = [s1, s1b, s2, s3, s3b, s4] + chain(1) + chain(2) + chain(3) + chain(4) + chain(5) + [s10]
                for step in range(len(stages) + 3):
                    for h in range(4):
                        k = step - h
                        if 0 <= k < len(stages):
                            pre.append(lambda st=stages[k], h=h: st(h))
                scans = []
                for c in range(2):
                    subs = scan(c)
                    L = []
                    for step in range(len(subs) + 3):
                        for h in range(4):
                            k = step - h
                            if 0 <= k < len(subs):
                                L.append(lambda f=subs[k], h=h: f(h))
                    scans.append(L)

                return pre, scans, _final, gsteps

            GT2, GT2_r = kb.sb("GT2", [128, D], F32, pb)
            kb.load("sp", GT2[:], mod_d.ap()[5120:6144].partition_broadcast(128), GT2_r, reads=[mod_r])
            stw = [kb.sb("cvw%d" % i, [128, 4096], F32, pb) for i in range(2)]
            stb = [kb.sb("cvb%d" % i, [128, 4096], BF16, pb) for i in range(2)]
            NCH = 16384 // 512
            conv_steps = []
            for ci in range(NCH):
                for which in range(2):
                    def f_cv(ci=ci, which=which):
                        s_, sr = stw[which]
                        b_, br = stb[which]
                        src = (eu_d, ev_d)[which].ap()[ci * 512:(ci + 1) * 512, :].rearrange("(p r) c -> p r c", r=4)
                        dst = uvb_d.ap()[ci * 512:(ci + 1) * 512, which * 1024:(which + 1) * 1024].rearrange("(p r) c -> p r c", r=4)
                        kb.load("sp", s_[:].rearrange("p (r c) -> p r c", r=4), src, sr)
                        if which == 0:
                            kb.op("act", [sr], [br], "activation", b_[:], s_[:], AF.Copy)
                        else:
                            kb.op("dve", [sr, GT2_r], [br], "tensor_tensor", b_[:].rearrange("p (r c) -> p r c", r=4), s_[:].rearrange("p (r c) -> p r c", r=4), GT2[:].unsqueeze(1).to_broadcast([128, 4, 1024]), ALU.mult)
                        kb.store("sp", dst, b_[:].rearrange("p (r c) -> p r c", r=4), br, uvb_r)
                    conv_steps.append(f_cv)
            cvi = [0]

            def emit_conv(n):
                while n > 0 and cvi[0] < len(conv_steps):
                    conv_steps[cvi[0]]()
                    cvi[0] += 1
                    n -= 1
            GT_ = {}

            def gtile(ti):
                if ti >= NT:
                    return [], None, None, []
                if ti not in GT_:
                    GT_[ti] = gdn_tile(ti)
                return GT_[ti]

            def merge2(A_, B_):
                out, ia, ib = [], 0, 0
                na, nb = len(A_), len(B_)
                for k in range(na + nb):
                    if ib >= nb or (ia < na and (ia + 1) * (nb + 1) <= (ib + 1) * (na + 1)):
                        out.append(A_[ia]); ia += 1
                    else:
                        out.append(B_[ib]); ib += 1
                return out
            for f in gtile(0)[3] + gtile(1)[3] + gtile(0)[0]:
                f()
            cur = gtile(0)
            for ti in range(NT):
                emit_conv(1)
                nxt = gtile(ti + 1)
                npre = merge2(nxt[0], gtile(ti + 2)[3])
                h1 = len(npre) // 2
                for f in cur[1][0]:
                    f()
                for f in npre[:h1]:
                    f()
                for f in cur[1][1]:
                    f()
                for f in npre[h1:]:
                    f()
                cur[2]()
                cur = nxt
                GT_.pop(ti, None)
            emit_conv(len(conv_steps))
            kb.barrier()

        with ExitStack() as pd:
            wout, wout_r = kb.sb("wout", [128, 8, 1024], BF16, pd)
            wpq, wpq_r = kb.sb("wpq", [128, 8, 2048], BF16, pd)
            skT, skT_r = kb.sb("skTb", [128, 16, 128], BF16, pd)
            A2b, A2b_r = kb.sb("A2b", [128, D], F32, pd)
            B2b, B2b_r = kb.sb("B2b", [128, D], F32, pd)
            FNG, FNG_r = kb.sb("FNG", [128, D], F32, pd)
            bc = lambda a, n: mod_d.ap()[a:a + n].partition_broadcast(128)
            kb.load("sp", A2b[:], bc(4096, 1024), A2b_r, reads=[mod_r])
            kb.load("sp", B2b[:], bc(3072, 1024), B2b_r, reads=[mod_r])
            kb.load("sp", FNG[:], lnf_d.ap().partition_broadcast(128), FNG_r)
            kb.op("dve", [A2b_r, FNG_r], [A2b_r], "scalar_tensor_tensor", A2b[:], A2b[:], 1.0, FNG[:], ALU.add, ALU.mult)
            kb.load("sp", FNG[:], fng_d.ap().partition_broadcast(128), FNG_r)
            with ExitStack() as pw:
                GT1, GT1_r = kb.sb("GT1", [128, D], F32, pw)
                kb.load("sp", GT1[:], bc(2048, 1024), GT1_r, reads=[mod_r])
                stw = [kb.sb("stw%d" % i, [128, 4096], F32, pw) for i in range(2)]
                stb = [kb.sb("stb%d" % i, [128, 4096], BF16, pw) for i in range(2)]
                for kt in range(8):
                    s_, sr = stw[kt % 2]
                    kb.load("sp", s_[:, 0:1024], wout_d.ap()[kt * 128:(kt + 1) * 128, :], sr)
                    kb.op("dve", [sr, GT1_r], [wout_r], "tensor_tensor", wout[:, kt, :], s_[:, 0:1024], GT1[:], ALU.mult)
                    s2, sr2 = stw[(kt + 1) % 2]
                    kb.load("sp", s2[:, 0:2048], wpq_d.ap()[kt * 128:(kt + 1) * 128, :], sr2)
                    kb.op("act", [sr2], [wpq_r], "activation", wpq[:, kt, :], s2[:, 0:2048], AF.Copy)
                s_, sr = stw[0]
                kb.load("sp", s_[:, 0:2048], skT_d.ap(), sr)
                kb.op("dve", [sr], [skT_r], "tensor_copy", skT[:].rearrange("p a b -> p (a b)"), s_[:, 0:2048])
                kb.barrier()
            GS = 4
            NG = 128 // GS
            NBUF = 3
            uvg = [(kb.sb("uvg%d" % i, [128, GS, 2048], BF16, pd)[0], [Res("uvg%d_%d" % (i, j)) for j in range(GS)]) for i in range(NBUF)]
            dg = [kb.sb("dgp%d" % i, [128, 128], BF16, pd) for i in range(4)]
            two = lambda n, shp, dt: [kb.sb("%s%d" % (n, i), shp, dt, pd) for i in range(2)]
            om = [kb.sb("om0", [128, 8, 128], BF16, pd)] * 2
            xt = [kb.sb("xtD0", [128, D], F32, pd)] * 2
            x1s = [kb.sb("x1_%d" % i, [128, D], F32, pd) for i in range(3)]
            h2s = [kb.sb("h2_%d" % i, [128, D], BF16, pd) for i in range(3)]
            eids = two("eid", [128, 128], I32)
            gates = two("gate", [128, 8, 16], F32)
            ob = xt
            junkb, junkb_r = kb.sb("junkDb", [128, D], BF16, pd)
            junkv, junkv_r = kb.sb("junkDv", [128, D], BF16, pd)
            h2T, h2T_r = kb.sb("h2T", [128, 8, 128], BF16, pd)
            qTb, qTb_r = kb.sb("qTb", [128, 16, 128], BF16, pd)
            scs = two("scD", [128, 16, 128], F32)
            iu1, iu1_r = kb.sb("iu1", [128, 16, 16], U32, pd)
            iu2, iu2_r = kb.sb("iu2", [128, 8, 16], U32, pd)
            wk, wk_r = kb.sb("wkD", [128, 256], F32, pd)
            stp, stp_r = kb.sb("stop", [128, 16, 16], F32, pd)
            itp, itp_r = kb.sb("itop", [128, 16, 16], F32, pd)
            best, best_r = kb.sb("best", [128, 8, 16], F32, pd)
            posf, posf_r = kb.sb("posf", [128, 8, 16], F32, pd)
            ab, ab_r = kb.sb("abD", [128, 4, 8, 16], F32, pd)
            isel, isel_r = kb.sb("isel", [128, 2, 8, 16], F32, pd)
            eidf, eidf_r = kb.sb("eidf", [128, 128], F32, pd)
            gs, gs_r = kb.sb("gsD", [128, 16], F32, pd)
            dots, dots_r = kb.sb("dots", [128, 128], F32, pd)
            ges = [kb.sb("geD%d" % i, [128, 4, GS], F32, pd) for i in range(3)]
            hgs = [kb.sb("hgD%d" % i, [128, GS], F32, pd) for i in range(3)]
            coef, coef_r = kb.sb("coef", [128, 128], F32, pd)
            stD, stD_r = kb.sb("stD", [128, 8], F32, pd)
            stE, stE_r = kb.sb("stE", [128, 8], F32, pd)
            pM, pM_r = kb.ps("pMD", [128, 2, 512], F32, pd)
            pX, pX_r = kb.ps("pXD", [128, 512], F32, pd)
            pTd, pTd_r = kb.ps("pTD", [128, 8, 128], BF16, pd)
            pQ, pQ_r = kb.ps("pQD", [128, 4, 128], F32, pd)
            pS, pS_r = pQ, pQ_r
            STps = [kb.ps("STp%d" % i, [128, 4, 128], F32, pd) for i in range(2)]
            Op, Op_r = kb.ps("Op", [128, 129], F32, pd)
            nk, nk_r = kb.sb("nk", [128, 8], F32, pd)
            cnb, cnb_r = kb.sb("cnb", [128, 128], BF16, pd)
            Qn2 = [kb.sb("Qn%d" % i, [128, 128], BF16, pd) for i in range(2)]
            Qr2 = [kb.sb("Qr%d" % i, [65, 128], BF16, pd) for i in range(2)]
            Kg = [kb.sb("Kg%d" % i, [128, 512], BF16, pd) for i in range(4)]
            Krg = [kb.sb("Krg%d" % i, [65, 512], BF16, pd) for i in range(4)]
            Vg = [kb.sb("Vg%d" % i, [128, 4, 129], BF16, pd) for i in range(4)]
            PTs = [kb.sb("PT%d" % i, [128, 4, 128], BF16, pd) for i in range(2)]
            rc, rc_r = kb.sb("rc", [128, 2], F32, pd)
            onb, onb_r = kb.sb("onb", [128, 128], BF16, pd)
            kb.op("dve", [kmax_r], [nk_r], "tensor_scalar", nk[:, 0:4], kmax[:, 0:4], kmax[:, 4:5], None, ALU.add)
            kb.op("act", [nk_r], [nk_r], "activation", nk[:, 4:8], nk[:, 0:4], AF.Sqrt, scale=1.0609)
            kb.op("dve", [nk_r], [nk_r], "tensor_scalar", nk[:, 4:8], nk[:, 4:8], -1.0, None, ALU.mult)
            kb.op("dve", [cst_r], [cnb_r], "tensor_copy", cnb[:], cst[:, C_CN:C_CN + 128])
            mla_cnt = [0]
            V = lambda r, w, m, *a, **k: kb.op("dve", r, w, m, *a, **k)
            A = lambda r, w, *a, **k: kb.op("act", r, w, "activation", *a, **k)
            iota16 = cst[:, C_IOTA:C_IOTA + 16]

            def front(ti):
                t0 = ti * 128
                b = ti % 2
                o_, omr = om[b]
                x_, xr = xt[b]
                x1, x1_r = x1s[ti % 3]
                h2, h2_r = h2s[ti % 3]
                eid, eid_r = eids[b]
                gate, gate_r = gates[b]
                sc, sc_r = scs[b]
                cand, cand_r = sc[:].rearrange("p (h t) k -> p h (t k)", t=2), sc_r
                eq, eq_r = cand[:].rearrange("p h (a b) -> p h a b", a=16), sc_r
                steps = []
                add = steps.append

                qi = ti
                units = []
                for h in range(4):
                    units.append(("q", h, 0))
                    for g0 in range(0, qi + 1, 4):
                        units.append(("g", h, g0))
                    units.append(("fin", h, 0))
                gidx = {}
                for u in units:
                    if u[0] == "g":
                        gidx[u] = mla_cnt[0]
                        mla_cnt[0] += 1

                def u_load(u):
                    kind, h, g0 = u
                    if kind == "q":
                        qn, qnr = Qn2[h % 2]
                        qr, qrr = Qr2[h % 2]
                        kb.load("sp", qn[:], qnT_d.ap()[h, :, t0:t0 + 128], qnr, reads=[qnT_r])
                        kb.load("sp", qr[:], qrT_d.ap()[h, :, t0:t0 + 128], qrr, reads=[qrT_r])
                    elif kind == "g":
                        c = gidx[u] % 4
                        n = min(g0 + 4, qi + 1) - g0
                        kg, kgr = Kg[c]
                        krg, krgr = Krg[c]
                        vg_, vgr = Vg[c]
                        kb.load("sp", kg[:, 0:n * 128], knT_d.ap()[h, :, g0 * 128:(g0 + n) * 128], kgr, reads=[knT_r])
                        kb.load("sp", krg[:, 0:n * 128], krT_d.ap()[:, g0 * 128:(g0 + n) * 128], krgr, reads=[krT_r])
                        kb.load("sp", vg_[:, 0:n, :], v_d.ap()[h, g0 * 128:(g0 + n) * 128, :].rearrange("(n p) c -> p n c", p=128), vgr, reads=[v_r])

                def u_comp(u):
                    kind, h, g0 = u
                    qn, qnr = Qn2[h % 2]
                    qr, qrr = Qr2[h % 2]
                    if kind == "q":
                        V([qrr, nk_r], [qrr], "tensor_scalar", qr[64:65, :], qr[64:65, :], nk[64:65, 4 + h:5 + h], None, ALU.mult)
                    elif kind == "g":
                        js = list(range(g0, min(g0 + 4, qi + 1)))
                        n = len(js)
                        c = gidx[u] % 4
                        kg, kgr = Kg[c]
                        krg, krgr = Krg[c]
                        vg_, vgr = Vg[c]
                        pt, ptr = PTs[gidx[u] % 2]
                        STp, STp_r = STps[gidx[u] % 2]
                        for jj, j in enumerate(js):
                            kb.op("pe", [kgr, qnr], [STp_r], "matmul", STp[:, jj, :], kg[:, jj * 128:(jj + 1) * 128], qn[:], start=True, stop=False)
                            kb.op("pe", [krgr, qrr], [STp_r], "matmul", STp[:, jj, :], krg[:, jj * 128:(jj + 1) * 128], qr[:], start=False, stop=(j != qi))
                            if j == qi:
                                kb.op("pe", [idb_r, cnb_r], [STp_r], "matmul", STp[:, jj, :], idb[:], cnb[:], start=False, stop=True)
                        A([STp_r], [ptr], pt[:, 0:n, :], STp[:, 0:n, :], AF.Exp)
                        for jj, j in enumerate(js):
                            kb.op("pe", [ptr, vgr], [Op_r], "matmul", Op[:], pt[:, jj, :], vg_[:, jj, :], start=(j == 0), stop=(j == qi))
                    else:
                        V([Op_r], [rc_r], "reciprocal", rc[:, 0:1], Op[:, 128:129])
                        A([Op_r, rc_r], [onb_r], onb[:], Op[:, 0:128], AF.Copy, scale=rc[:, 0:1])
                        kb.op("pe", [onb_r, idb_r], [pTd_r], "transpose", pTd[:, 0, :], onb[:], idb[:])
                        A([pTd_r], [omr], o_[:, 4 + h, :], pTd[:, 0, :], AF.Copy)

                LOOK = 2
                for k, u in enumerate(units):
                    def f_u(k=k, u=u):
                        if k == 0:
                            for kk in range(min(LOOK, len(units))):
                                u_load(units[kk])
                        if k + LOOK < len(units):
                            u_load(units[k + LOOK])
                        u_comp(u)
                    add(f_u)

                def f_load():
                    kb.load("sp", o_[:, 0:4, :], omixT_d.ap()[0:512, t0:t0 + 128].rearrange("(kt p) t -> p kt t", p=128), omr, reads=[omix_r])
                    kb.load("sp", x_[:], x_d.ap()[t0:t0 + 128, :], xr)
                add(f_load)

                def f_mix():
                    for half in range(2):
                        for kt in range(8):
                            kb.op("pe", [omr, wout_r], [pX_r], "matmul", pX[:], o_[:, kt, :], wout[:, kt, half * 512:(half + 1) * 512], start=(kt == 0), stop=(kt == 7))
                        V([xr, pX_r], [x1_r], "tensor_tensor", x1[:, half * 512:(half + 1) * 512], x_[:, half * 512:(half + 1) * 512], pX[:], ALU.add)
                add(f_mix)

                def f_norm():
                    A([x1_r], [xr, stD_r], x_[:], x1[:], AF.Square, accum_out=stD[:, 0:1])
                    A([stD_r], [stD_r], stD[:, 1:2], stD[:, 0:1], AF.Sqrt, bias=EPS, scale=1.0 / D)
                    V([stD_r], [stD_r], "reciprocal", stD[:, 2:3], stD[:, 1:2])
                    V([x1_r, stD_r, A2b_r], [xr], "scalar_tensor_tensor", x_[:], x1[:], stD[:, 2:3], A2b[:], ALU.mult, ALU.mult)
                add(f_norm)

                def f_h2():
                    V([xr, B2b_r], [h2_r], "tensor_tensor", h2[:], x_[:], B2b[:], ALU.add)
                    for kt in range(8):
                        kb.op("pe", [h2_r, idb_r], [pTd_r], "transpose", pTd[:, kt, :], h2[:, kt * 128:(kt + 1) * 128], idb[:])
                    A([pTd_r], [h2T_r], h2T[:], pTd[:], AF.Copy)
                add(f_h2)
                for g4 in range(4):
                    def f_q(g4=g4):
                        for j in range(4):
                            hp = g4 * 4 + j
                            for kt in range(8):
                                kb.op("pe", [h2T_r, wpq_r], [pQ_r], "matmul", pQ[:, j, :], wpq[:, kt, hp * 128:(hp + 1) * 128], h2T[:, kt, :], start=(kt == 0), stop=(kt == 7))
                        A([pQ_r], [qTb_r], qTb[:, g4 * 4:(g4 + 1) * 4, :], pQ[:], AF.Copy)
                    add(f_q)
                for g4 in range(4):
                    def f_s(g4=g4):
                        for j in range(4):
                            hp = g4 * 4 + j
                            kb.op("pe", [qTb_r, skT_r], [pS_r], "matmul", pS[:, j, :], qTb[:, hp, :], skT[:, hp, :], start=True, stop=True)
                        A([pS_r], [sc_r], sc[:, g4 * 4:(g4 + 1) * 4, :], pS[:], AF.Copy)
                    add(f_s)

                split = len(steps)

                def top16(src, n, vout, vres, iu, iures):
                    V([sc_r], [vres], "max", vout[:, 0:8], src)
                    V([sc_r, vres], [iures], "max_index", iu[:, 0:8], vout[:, 0:8], src)
                    V([sc_r, vres], [wk_r], "match_replace", wk[:, 0:n], vout[:, 0:8], src, -1e30)
                    V([wk_r], [vres], "max", vout[:, 8:16], wk[:, 0:n])
                    V([wk_r, vres], [iures], "max_index", iu[:, 8:16], vout[:, 8:16], wk[:, 0:n])
                for hp in range(16):
                    add(lambda hp=hp: top16(sc[:, hp, :], 128, stp[:, hp, :], stp_r, iu1[:, hp, :], iu1_r))
                add(lambda: V([iu1_r], [itp_r], "tensor_copy", itp[:], iu1[:]))
                st4 = stp[:].rearrange("p (h t) k -> p h t k", t=2)
                it4 = itp[:].rearrange("p (h t) k -> p h t k", t=2)
                add(lambda: V([stp_r], [cand_r], "tensor_tensor", cand[:].rearrange("p h (a b) -> p h a b", a=16), st4[:, :, 0, :].unsqueeze(3).to_broadcast([128, 8, 16, 16]), st4[:, :, 1, :].unsqueeze(2).to_broadcast([128, 8, 16, 16]), ALU.add))
                for h in range(8):
                    add(lambda h=h: top16(cand[:, h, :], 256, best[:, h, :], best_r, iu2[:, h, :], iu2_r))
                add(lambda: V([iu2_r], [posf_r], "tensor_copy", posf[:], iu2[:]))

                def f_idx():
                    V([posf_r], [ab_r], "tensor_scalar", ab[:, 2], posf[:], 1.0 / 16, -0.46875, ALU.mult, ALU.add)
                    V([ab_r], [ab_r], "tensor_scalar", ab[:, 3], ab[:, 2], 12582912.0, None, ALU.add)
                    V([ab_r], [ab_r], "tensor_scalar", ab[:, 0], ab[:, 3], -12582912.0, None, ALU.add)
                    V([ab_r, posf_r], [ab_r], "scalar_tensor_tensor", ab[:, 1], ab[:, 0], -16.0, posf[:], ALU.mult, ALU.add)
                add(f_idx)
                for t in range(2):
                    def f_sel(t=t):
                        V([ab_r, cst_r], [eq_r], "tensor_tensor", eq[:], ab[:, t].unsqueeze(3).to_broadcast([128, 8, 16, 16]), iota16.unsqueeze(1).unsqueeze(1).to_broadcast([128, 8, 16, 16]), ALU.is_equal)
                        V([eq_r, itp_r], [eq_r], "tensor_tensor", eq[:], eq[:], it4[:, :, t, :].unsqueeze(2).to_broadcast([128, 8, 16, 16]), ALU.mult)
                        V([eq_r], [isel_r], "tensor_reduce", isel[:, t], eq[:], AX.X, ALU.add)
                    add(f_sel)

                def f_gate():
                    V([isel_r], [eidf_r], "scalar_tensor_tensor", eidf[:].rearrange("p (h k) -> p h k", h=8), isel[:, 0], 128.0, isel[:, 1], ALU.mult, ALU.add)
                    V([eidf_r], [eid_r], "tensor_copy", eid[:], eidf[:])
                    V([best_r], [gate_r], "tensor_tensor", gate[:], best[:], best[:, :, 0:1].to_broadcast([128, 8, 16]), ALU.subtract)
                    A([gate_r], [gate_r], gate[:], gate[:], AF.Exp)
                    V([gate_r], [gs_r], "tensor_reduce", gs[:, 0:8], gate[:], AX.X, ALU.add)
                    V([gs_r], [gs_r], "reciprocal", gs[:, 8:16], gs[:, 0:8])
                    V([gate_r, gs_r], [gate_r], "tensor_tensor", gate[:], gate[:], gs[:, 8:16].unsqueeze(2).to_broadcast([128, 8, 16]), ALU.mult)
                add(f_gate)
                return steps[:split], steps[split:]

            def back(ti, fsteps, deferred=None):
                t0 = ti * 128
                b = ti % 2
                x1, x1_r = x1s[ti % 3]
                h2, h2_r = h2s[ti % 3]
                eid, eid_r = eids[b]
                gate, gate_r = gates[b]
                gflat = gate[:].rearrange("p h k -> p (h k)")
                V([], [dots_r], "memset", dots[:], 0.0)
                nf = len(fsteps)
                fi = 0

                def bufof(g):
                    return uvg[(ti * NG + g) % NBUF]

                def st_gather(g):
                    buf, bufr = bufof(g)
                    for j in range(GS):
                        hk = g * GS + j
                        kb.dma("pool", [eid_r, uvb_r], [bufr[j]], bufr[j], False, "indirect_dma_start", out=buf[:, j, :], out_offset=None, in_=uvb_d.ap(), in_offset=bass.IndirectOffsetOnAxis(ap=eid[:, hk:hk + 1], axis=0))

                def st_dots(g):
                    buf, bufr = bufof(g)
                    ge, ge_r = ges[g % 3]
                    for j in range(GS):
                        hk = g * GS + j
                        V([bufr[j], h2_r], [junkv_r, dots_r], "scalar_tensor_tensor", junkv[:], buf[:, j, 0:1024], 1.0, h2[:], ALU.mult, ALU.mult, accum_out=dots[:, hk:hk + 1])

                def st_pre(g):
                    ge, ge_r = ges[g % 3]
                    dsl = dots[:, g * GS:(g + 1) * GS]
                    V([dots_r], [ge_r], "tensor_tensor", ge[:, 0], dsl, dsl, ALU.mult)
                    V([ge_r], [ge_r], "tensor_scalar", ge[:, 0], ge[:, 0], 0.044715, 1.0, ALU.mult, ALU.add)
                    V([ge_r, dots_r], [ge_r], "tensor_tensor", ge[:, 1], ge[:, 0], dsl, ALU.mult)
                    A([ge_r], [ge_r], ge[:, 2], ge[:, 1], AF.Tanh, scale=0.7978845608028654)
                    hg, hg_r = hgs[g % 3]
                    V([dots_r, gate_r], [hg_r], "scalar_tensor_tensor", hg[:], dsl, 0.5, gflat[:, g * GS:(g + 1) * GS], ALU.mult, ALU.mult)

                def st_fin(g):
                    buf, bufr = bufof(g)
                    ge, ge_r = ges[g % 3]
                    dsl = dots[:, g * GS:(g + 1) * GS]
                    hg, hg_r = hgs[g % 3]
                    V([ge_r, hg_r], [coef_r], "scalar_tensor_tensor", coef[:, g * GS:(g + 1) * GS], ge[:, 2], 1.0, hg[:], ALU.add, ALU.mult)
                    for j in range(GS):
                        hk = g * GS + j
                        d_, dr = dg[hk % 4]
                        A([idb_r, coef_r], [dr], d_[:], idb[:], AF.Copy, scale=coef[:, hk:hk + 1])
                        for half in range(2):
                            kb.op("pe", [dr, bufr[j]], [pM_r], "matmul", pM[:, half, :], d_[:], buf[:, j, 1024 + half * 512:1024 + (half + 1) * 512], start=(hk == 0), stop=(hk == 127))

                for s_ in range(NG + 2):
                    if s_ < NG:
                        st_gather(s_)
                    if 0 <= s_ - 1 < NG:
                        st_dots(s_ - 1)
                        st_pre(s_ - 1)
                    if s_ == 2 and deferred is not None:
                        deferred()
                    if 0 <= s_ - 2 < NG:
                        st_fin(s_ - 2)
                    tgt = min(nf, ((s_ + 1) * nf) // NG)
                    while fi < tgt:
                        fsteps[fi]()
                        fi += 1
                while fi < nf:
                    fsteps[fi]()
                    fi += 1
                def fin_tile():
                    V([x1_r, pM_r], [x1_r], "tensor_tensor", x1[:].rearrange("p (a b) -> p a b", a=2), x1[:].rearrange("p (a b) -> p a b", a=2), pM[:], ALU.add)
                    A([x1_r], [junkb_r, stE_r], junkb[:], x1[:], AF.Square, accum_out=stE[:, 4:5])
                    A([stE_r], [stE_r], stE[:, 5:6], stE[:, 4:5], AF.Sqrt, bias=EPS, scale=1.0 / D)
                    V([stE_r], [stE_r], "reciprocal", stE[:, 6:7], stE[:, 5:6])
                    ot, otr = ob[b]
                    V([x1_r, stE_r, FNG_r], [otr], "scalar_tensor_tensor", ot[:], x1[:], stE[:, 6:7], FNG[:], ALU.mult, ALU.mult)
                    kb.store("sp", out_d.ap()[t0:t0 + 128, :], ot[:], otr)
                return fin_tile

            def merge(A_, B_):
                out, ia, ib = [], 0, 0
                na, nb = len(A_), len(B_)
                for k in range(na + nb):
                    if ib >= nb or (ia < na and (ia + 1) * (nb + 1) <= (ib + 1) * (na + 1)):
                        out.append(A_[ia]); ia += 1
                    else:
                        out.append(B_[ib]); ib += 1
                return out

            FR = {}

            def fr(ti):
                if ti >= NT:
                    return [], []
                if ti not in FR:
                    FR[ti] = front(ti)
                return FR[ti]
            for f in fr(0)[0] + fr(0)[1] + fr(1)[0]:
                f()
            pend = None
            for ti in range(NT):
                pend = back(ti, merge(fr(ti + 1)[1], fr(ti + 2)[0]), pend)
                FR.pop(ti, None)
            pend()
        kb.barrier()
        kb.finish()
    return nc, kb


def _host_inputs(inp, b, S):
    f = lambda a: np.ascontiguousarray(np.asarray(a, np.float32))
    fm = lambda v: f(np.asarray(v).reshape(-1, 128).T)
    vecs = np.concatenate([fm(inp["c"][b]), fm(inp["ln_mix_g"][0]), fm(inp["ln_ffn_g"][0]), fm(inp["q_norm_g"][0]), fm(inp["kv_norm_g"][0]),
                           f(np.asarray(inp["conv_w"][0]).T.reshape(12, 128, 4).transpose(1, 0, 2).reshape(128, 48))], axis=1)
    rowv = np.concatenate([f(inp["a_log"][0]), f(inp["dt_bias"][0]), f(inp["gdn_norm_g"][0])])
    skT = f(np.asarray(inp["sub_keys"][0]).transpose(3, 1, 0, 2).reshape(128, 2048))
    return {"x": f(inp["x"][b, :S]), "pos": np.ascontiguousarray(np.asarray(inp["positions"][b, :S], np.int32)), "consts": make_consts(),
            "vecs": f(vecs), "rowv": f(rowv), "b_ada": f(inp["b_ada"][0]).reshape(1, 6144), "fng": f(inp["final_norm_g"]),
            "lnf": f(inp["ln_ffn_g"][0]),
            "w_in": f(inp["w_in"][0]), "w_uq": f(inp["w_uq"][0]), "w_ukv": f(inp["w_ukv"][0]), "w_out": f(inp["w_out"][0]),
            "w_pq": f(inp["w_pq"][0]), "skT": skT, "expert_u": f(inp["expert_u"][0]), "expert_v": f(inp["expert_v"][0]), "w_ada": f(inp["w_ada"][0])}


def kernel(**inputs):
    x = np.asarray(inputs["x"])
    B, S, _ = x.shape
    if S not in _CACHE:
        _CACHE[S] = build(S, dbg=False)[0]
    nc = _CACHE[S]
    in_maps = [_host_inputs(inputs, b, S) for b in range(B)]
    res = run_bass_kernel_spmd(nc, in_maps, core_ids=list(range(B)))
    return np.stack([np.asarray(r["out"], np.float32) for r in res.results], axis=0)
```

```python
import math
import numpy as np
from contextlib import ExitStack
import concourse.bass as bass
import concourse.mybir as mybir
from concourse.bass_utils import run_bass_kernel_spmd

F32 = mybir.dt.float32
BF16 = mybir.dt.bfloat16
I32 = mybir.dt.int32
U32 = mybir.dt.uint32
AF = mybir.ActivationFunctionType
ALU = mybir.AluOpType
AX = mybir.AxisListType

SEM_CH = 30000
EPS = 1e-6
D = 1024
IN_DIM = 2760
NEG = -30000.0


class Res:
    __slots__ = ("name", "w", "r", "acc", "n_in", "n_out", "excl")

    def __init__(self, name, acc=False, excl=False):
        self.name = name
        self.excl = excl
        self.w = {}
        self.r = {}
        self.acc = acc
        self.n_in = 0
        self.n_out = 0


class KB:
    ENGS = ("pe", "act", "dve", "pool", "sp")

    def __init__(self, nc, es):
        self.nc = nc
        self.es = es
        self.lists = {e: [] for e in self.ENGS}
        self.count = {e: 0 for e in self.ENGS}
        self.seen = {e: {} for e in self.ENGS}
        self.sems = {}
        self.dma_keys = {}
        self.nsem = 0
        self.ninst = 0

    def sb(self, name, shape, dt, es=None):
        t = (es or self.es).enter_context(self.nc.sbuf_tensor("sb_" + name, list(shape), dt))
        return t, Res(name)

    def ps(self, name, shape, dt, es=None):
        t = (es or self.es).enter_context(self.nc.psum_tensor("ps_" + name, list(shape), dt))
        return t, Res(name, excl=True)

    def _sem(self, key):
        s = self.sems.get(key)
        if s is None:
            self.nsem += 1
            s = self.es.enter_context(self.nc.semaphore("s%d" % self.nsem))
            self.sems[key] = s
        return s

    def _engsem(self, eng, g):
        ch = (g - 1) // SEM_CH
        return self._sem(("eng", eng, ch)), (g - 1) % SEM_CH + 1

    def _deps(self, eng, reads, writes):
        deps = {}
        for r in reads:
            for k, v in r.w.items():
                if deps.get(k, 0) < v:
                    deps[k] = v
        for w in writes:
            if w.acc:
                continue
            for d in (w.w, w.r):
                for k, v in d.items():
                    if deps.get(k, 0) < v:
                        deps[k] = v
        waits = []
        seen = self.seen[eng]
        for k, v in deps.items():
            if eng == "pe" and k == ("e", "pe"):
                continue
            if seen.get(k, 0) >= v:
                continue
            seen[k] = v
            waits.append((k, v))
        return waits

    def _emit_waits(self, eng, waits):
        L = self.lists[eng]
        for k, v in waits:
            if k[0] == "e":
                L.append(("we", k[1], v))
            else:
                L.append(("w", self._sem(k), v))
            self.ninst += 1

    def _mark(self, key, val, reads, writes):
        for r in reads:
            r.r[key] = val
        for w in writes:
            if w.acc:
                w.w[key] = val
            else:
                w.w = {key: val}
                w.r = {}

    def op(self, eng, reads, writes, meth, *args, **kw):
        if any(r.excl for r in reads):
            writes = list(writes) + [r for r in reads if r.excl]
            reads = [r for r in reads if not r.excl]
        waits = self._deps(eng, reads, writes)
        self._emit_waits(eng, waits)
        self.count[eng] += 1
        g = self.count[eng]
        self.lists[eng].append(("ie", meth, args, kw, eng, g))
        self.ninst += 1
        self._mark(("e", eng), g, reads, writes)

    def dma(self, eng, reads, writes, slot, store, meth, *args, **kw):
        waits = self._deps(eng, reads, writes)
        self._emit_waits(eng, waits)
        if store:
            key = ("do", id(slot))
            slot.n_out += 16
            val = slot.n_out
        else:
            key = ("di", id(slot))
            slot.n_in += 16
            val = slot.n_in
        sem = self._sem(key)
        self.dma_keys[key] = val
        self.lists[eng].append(("i", meth, args, kw, sem, 16))
        self.ninst += 1
        self._mark(key, val, reads, writes)

    def load(self, eng, dst_ap, src_ap, slot, reads=()):
        self.dma(eng, list(reads), [slot], slot, False, "dma_start", out=dst_ap, in_=src_ap)

    def store(self, eng, dst_ap, src_ap, slot, dres=None):
        self.dma(eng, [slot], [dres] if dres is not None else [], slot, True, "dma_start", out=dst_ap, in_=src_ap)

    def barrier(self):
        toks = {}
        for e in self.ENGS:
            if self.count[e] > 0:
                toks[("e", e)] = self.count[e]
        toks.update(self.dma_keys)
        for e in self.ENGS:
            waits = []
            for k, v in toks.items():
                if k == ("e", e) and e == "pe":
                    continue
                if self.seen[e].get(k, 0) >= v:
                    continue
                self.seen[e][k] = v
                waits.append((k, v))
            self._emit_waits(e, waits)

    def finish(self):
        nc = self.nc
        lists = self.lists

        waited = {e: set() for e in self.ENGS}
        for L in lists.values():
            for it in L:
                if it[0] == "we":
                    waited[it[1]].add(it[2])
        rank = {e: {g: i + 1 for i, g in enumerate(sorted(waited[e]))} for e in self.ENGS}

        def replay(engine, L):
            for it in L:
                if it[0] == "w":
                    engine.wait_ge(it[1], it[2])
                elif it[0] == "we":
                    sem, lv = self._engsem(it[1], rank[it[1]][it[2]])
                    engine.wait_ge(sem, lv)
                elif it[0] == "ie":
                    ins = getattr(engine, it[1])(*it[2], **it[3])
                    r = rank[it[4]].get(it[5])
                    if r is not None:
                        sem, lv = self._engsem(it[4], r)
                        ins.then_inc(sem, 1)
                else:
                    ins = getattr(engine, it[1])(*it[2], **it[3])
                    ins.then_inc(it[4], it[5])

        with nc.Block() as block:
            @block.tensor
            def _(e):
                replay(e, lists["pe"])

            @block.scalar
            def _(e):
                replay(e, lists["act"])

            @block.vector
            def _(e):
                replay(e, lists["dve"])

            @block.gpsimd
            def _(e):
                replay(e, lists["pool"])

            @block.sync
            def _(e):
                replay(e, lists["sp"])


C_ID, C_U, C_SL0, C_SL1, C_LS, C_PM, C_NA, C_CN, C_ONE = [i * 128 for i in range(9)]
C_IND0 = 9 * 128
C_IND1 = C_IND0 + 1
C_INVF = C_IND0 + 2
C_IOTA = C_IND0 + 3
NCONST = C_IOTA + 16


def make_consts():
    c = np.zeros((128, NCONST), np.float32)
    i = np.arange(128)
    ch = i // 64
    c[:, C_ID:C_ID + 128] = np.eye(128)
    c[:, C_U:C_U + 128] = ((ch[:, None] == ch[None, :]) & (i[:, None] <= i[None, :]))
    c[63, C_SL0:C_SL0 + 128] = 1.0
    c[127, C_SL1:C_SL1 + 128] = 1.0
    c[:, C_LS:C_LS + 128] = (i[:, None] == (ch[None, :] * 64 + 63))
    same = ch[:, None] == ch[None, :]
    c[:, C_PM:C_PM + 128] = np.where(same & (i[None, :] < i[:, None]), 0.0, -NEG)
    c[:, C_NA:C_NA + 128] = np.where(same & (i[:, None] <= i[None, :]), 0.0, NEG)
    c[:, C_CN:C_CN + 128] = np.where(i[:, None] <= i[None, :], 0.0, NEG)
    c[:, C_ONE:C_ONE + 128] = 1.0
    c[:, C_IND0] = (i < 64)
    c[:, C_IND1] = (i >= 64)
    half = 32
    c[:, C_INVF] = (10000.0 ** (-(np.arange(128) % half).astype(np.float64) / half)).astype(np.float32)
    c[:, C_IOTA:C_IOTA + 16] = np.arange(16)[None, :]
    return c


V_C, V_LNM, V_LNF, V_QG, V_KVG, V_CW = 0, 8, 16, 24, 27, 29
NVEC = V_CW + 48
R_ALOG, R_DTB, R_GG = 0, 4, 8
NROW = 136

_CACHE = {}
GDN_NST = 99
GDN_GATES = True


def build(S, dbg=False):
    NT = S // 128
    nc = bass.Bass("TRN2", target_bir_lowering=False)
    okind = "ExternalOutput" if dbg else "Internal"

    def din(name, shape, dt=F32):
        return nc.dram_tensor(name, list(shape), dt, kind="ExternalInput")

    def dsc(name, shape, dt):
        return nc.dram_tensor(name, list(shape), dt, kind=okind)

    x_d = din("x", [S, D]); pos_d = din("pos", [S], I32)
    consts_d = din("consts", [128, NCONST]); vecs_d = din("vecs", [128, NVEC]); rowv_d = din("rowv", [NROW])
    bada_d = din("b_ada", [1, 6144]); fng_d = din("fng", [D]); lnf_d = din("lnf", [D])
    win_d = din("w_in", [D, IN_DIM]); wuq_d = din("w_uq", [384, 768]); wukv_d = din("w_ukv", [256, 1024])
    wout_d = din("w_out", [D, D]); wpq_d = din("w_pq", [D, 2048]); skT_d = din("skT", [128, 2048])
    eu_d = din("expert_u", [16384, D]); ev_d = din("expert_v", [16384, D]); wada_d = din("w_ada", [D, 6144])
    out_d = nc.dram_tensor("out", [S, D], F32, kind="ExternalOutput")
    mod_d = dsc("mod_s", [6144], F32)
    qkvz_d = dsc("qkvz_s", [S, 2048], BF16)
    gab_d = dsc("gab_s", [S, 8], F32)
    qnT_d = dsc("qnT_s", [4, 128, S], BF16)
    qrT_d = dsc("qrT_s", [4, 65, S], BF16)
    knT_d = dsc("knT_s", [4, 128, S], BF16)
    krT_d = dsc("krT_s", [65, S], BF16)
    v_d = dsc("v_s", [4, S, 129], BF16)
    omixT_d = dsc("omixT_s", [D, S], BF16)
    uvb_d = nc.dram_tensor("uvb_s", [16384, 2 * D], BF16)

    with ExitStack() as es:
        kb = KB(nc, es)
        R = lambda n: Res(n, acc=True)
        mod_r, qkvz_r, gab_r, qnT_r, qrT_r, knT_r, krT_r, v_r, omix_r, uvb_r = [R(n) for n in
            ("mod", "qkvz", "gab", "qnT", "qrT", "knT", "krT", "v", "omix", "uvb")]
        cst, cst_r = kb.sb("cst", [128, NCONST], F32)
        vecs, vecs_r = kb.sb("vecs", [128, NVEC], F32)
        rowv, rowv_r = kb.sb("rowv", [128, NROW], F32)
        modT, modT_r = kb.sb("modT", [128, 48], F32)
        AB1, AB1_r = kb.sb("AB1", [128, 32], F32)
        idb, idb_r = kb.sb("idb", [128, 128], BF16)
        oneb, oneb_r = kb.sb("oneb", [128, 128], BF16)
        kmax, kmax_r = kb.sb("kmax", [128, 8], F32)
        kb.load("sp", cst[:], consts_d.ap(), cst_r)
        kb.load("sp", vecs[:], vecs_d.ap(), vecs_r)
        kb.load("sp", rowv[:], rowv_d.ap().partition_broadcast(128), rowv_r)
        kb.op("dve", [cst_r], [idb_r], "tensor_copy", idb[:], cst[:, C_ID:C_ID + 128])
        kb.op("dve", [cst_r], [oneb_r], "tensor_copy", oneb[:], cst[:, C_ONE:C_ONE + 128])
        kb.op("dve", [], [kmax_r], "memset", kmax[:], 0.0)
        ident = cst[:, C_ID:C_ID + 128]
        ones = cst[:, C_ONE:C_ONE + 128]

        with ExitStack() as p0:
            cact, cact_r = kb.sb("cact", [128, 8], F32, p0)
            wst = [kb.sb("wst%d" % i, [128, 6144], F32, p0) for i in range(2)]
            mrow, mrow_r = kb.sb("mrow", [1, 6144], F32, p0)
            brow, brow_r = kb.sb("brow", [1, 6144], F32, p0)
            pm = [kb.ps("pm%d" % i, [1, 512], F32, p0) for i in range(4)]
            kb.op("act", [vecs_r], [cact_r], "activation", cact[:], vecs[:, V_C:V_C + 8], AF.Silu)
            kb.load("sp", brow[:], bada_d.ap(), brow_r)
            for half in range(3):
                for kt in range(8):
                    w, wr = wst[kt % 2]
                    kb.load("sp" if kt % 2 == 0 else "pool", w[:, 0:2048], wada_d.ap()[kt * 128:(kt + 1) * 128, half * 2048:(half + 1) * 2048], wr)
                    for j in range(4):
                        kb.op("pe", [cact_r, wr], [pm[j][1]], "matmul", pm[j][0][:], cact[:, kt:kt + 1], w[:, j * 512:(j + 1) * 512], start=(kt == 0), stop=(kt == 7))
                for j in range(4):
                    c0 = half * 2048 + j * 512
                    kb.op("dve", [pm[j][1], brow_r], [mrow_r], "tensor_tensor", mrow[:, c0:c0 + 512], pm[j][0][:], brow[:, c0:c0 + 512], ALU.add)
            kb.store("sp", mod_d.ap().rearrange("(o n) -> o n", o=1), mrow[:], mrow_r, mod_r)
            kb.barrier()
            kb.dma("sp", [mod_r], [modT_r], modT_r, False, "dma_start", out=modT[:], in_=mod_d.ap().rearrange("(j p) -> p j", p=128), allow_slow_non_contiguous=True)
            kb.op("dve", [modT_r, vecs_r], [AB1_r], "scalar_tensor_tensor", AB1[:, 0:8], modT[:, 8:16], 1.0, vecs[:, V_LNM:V_LNM + 8], ALU.add, ALU.mult)
            kb.op("dve", [modT_r], [AB1_r], "tensor_copy", AB1[:, 8:16], modT[:, 0:8])
            kb.op("dve", [modT_r, vecs_r], [AB1_r], "scalar_tensor_tensor", AB1[:, 16:24], modT[:, 32:40], 1.0, vecs[:, V_LNF:V_LNF + 8], ALU.add, ALU.mult)
            kb.op("dve", [modT_r], [AB1_r], "tensor_copy", AB1[:, 24:32], modT[:, 24:32])
            kb.barrier()

        with ExitStack() as pa:
            win, win_r = kb.sb("win", [128, 8, 2824], BF16, pa)
            wuq, wuq_r = kb.sb("wuq", [128, 3, 1024], BF16, pa)
            wukv, wukv_r = kb.sb("wukv", [128, 2, 1024], BF16, pa)
            stg = [kb.sb("stg%d" % i, [128, 2760], F32, pa) for i in range(2)]
            for kt in range(8):
                s_, sr = stg[kt % 2]
                kb.load("sp", s_[:], win_d.ap()[kt * 128:(kt + 1) * 128, :], sr)
                kb.op("act" if kt % 2 else "dve", [sr], [win_r], "activation" if kt % 2 else "tensor_copy", *((win[:, kt, 0:2760], s_[:], AF.Copy) if kt % 2 else (win[:, kt, 0:2760], s_[:])))
                kb.op("dve", [sr], [win_r], "tensor_scalar", win[:, kt, 2760:2792], s_[:, 2728:2760], -1.0, None, ALU.mult)
                kb.op("dve", [sr], [win_r], "tensor_copy", win[:, kt, 2792:2824], s_[:, 2696:2728])
            for kt in range(3):
                s_, sr = stg[kt % 2]
                kb.load("sp", s_[:, 0:768], wuq_d.ap()[kt * 128:(kt + 1) * 128, :], sr)
                kb.op("dve", [sr], [wuq_r], "tensor_copy", wuq[:, kt, 0:768], s_[:, 0:768])
                for h in range(4):
                    b0 = h * 192 + 128
                    kb.op("dve", [sr], [wuq_r], "tensor_scalar", wuq[:, kt, 768 + h * 64:768 + h * 64 + 32], s_[:, b0 + 32:b0 + 64], -1.0, None, ALU.mult)
                    kb.op("dve", [sr], [wuq_r], "tensor_copy", wuq[:, kt, 768 + h * 64 + 32:768 + h * 64 + 64], s_[:, b0:b0 + 32])
            for kt in range(2):
                s_, sr = stg[(kt + 1) % 2]
                kb.load("sp", s_[:, 0:1024], wukv_d.ap()[kt * 128:(kt + 1) * 128, :], sr)
                kb.op("dve", [sr], [wukv_r], "tensor_copy", wukv[:, kt, :], s_[:, 0:1024])

            xt = [kb.sb("xt%d" % i, [128, D], F32, pa) for i in range(2)]
            junk, junk_r = kb.sb("junkA", [128, D], F32, pa)
            st1, st1_r = kb.sb("st1", [128, 16], F32, pa)
            xn, xn_r = kb.sb("xnA", [128, D], BF16, pa)
            hT, hT_r = kb.sb("hTA", [128, 8, 128], BF16, pa)
            cin, cin_r = kb.sb("cin", [128, 12, 131], F32, pa)
            cv, cv_r = kb.sb("cv", [128, 12, 128], F32, pa)
            cs, cs_r = kb.sb("cs", [128, 12, 128], BF16, pa)
            stA = [kb.sb("stA%d" % i, [128, 2048], BF16, pa) for i in range(2)]
            gst = [kb.sb("gst%d" % i, [128, 8], F32, pa) for i in range(2)]
            cn, cn_r = kb.sb("cn", [128, 640], BF16, pa)
            cnT, cnT_r = kb.sb("cnT", [128, 5, 128], BF16, pa)
            qst = [kb.sb("qst%d" % i, [128, 4, 128], BF16, pa) for i in range(2)]
            kst = [kb.sb("kst%d" % i, [128, 4, 128], BF16, pa) for i in range(2)]
            rst = [kb.sb("rst%d" % i, [65, 4, 128], BF16, pa) for i in range(2)]
            krst = [kb.sb("krst%d" % i, [65, 128], BF16, pa) for i in range(2)]
            vst = [kb.sb("vst%d" % i, [128, 4, 129], BF16, pa) for i in range(2)]
            sq1, sq1_r = kb.sb("sq1", [128, 4, 128], BF16, pa)
            sq2, sq2_r = kb.sb("sq2", [64, 4, 128], BF16, pa)
            posi, posi_r = kb.sb("posi", [64, 128], I32, pa)
            tr, tr_r = kb.sb("trig", [64, 6, 128], F32, pa)
            t12, t12_r = kb.sb("t12", [64, 2, 4, 128], F32, pa)
            tmx, tmx_r = kb.sb("tmx", [128, 4], F32, pa)
            pT, pT_r = kb.ps("pTA", [128, 8, 128], BF16, pa)
            pF = [kb.ps("pFA%d" % i, [128, 4, 128], F32, pa) for i in range(2)]
            pK, pK_r = kb.ps("pKA", [64, 2, 128], F32, pa)
            pZ, pZ_r = kb.ps("pZA", [128, 512], F32, pa)
            pC2, pC2_r = kb.ps("pC2A", [128, 392], F32, pa)
            pC3, pC3_r = kb.ps("pC3A", [128, 256], F32, pa)
            pR, pR_r = kb.ps("pRA", [64, 4, 128], F32, pa)
            kb.op("pool", [], [cin_r], "memset", cin[:], 0.0)
            for i in range(2):
                kb.op("pool", [], [vst[i][1]], "memset", vst[i][0][:], 1.0)
                kb.op("pool", [], [krst[i][1]], "memset", krst[i][0][:], 1.0)
                kb.op("pool", [], [rst[i][1]], "memset", rst[i][0][:], 0.0)
            cwv = vecs[:, V_CW:V_CW + 48]
            QS = 192.0 ** -0.5
            for ti in range(NT):
                t0 = ti * 128
                b = ti % 2
                x_, xr = xt[b]
                if ti == 0:
                    kb.load("sp", x_[:], x_d.ap()[t0:t0 + 128, :], xr)
                if ti + 1 < NT:
                    kb.load("sp", xt[(ti + 1) % 2][0][:], x_d.ap()[t0 + 128:t0 + 256, :], xt[(ti + 1) % 2][1])
                kb.load("pool", posi[:], pos_d.ap()[t0:t0 + 128].partition_broadcast(64), posi_r)
                kb.op("act", [xr], [junk_r, st1_r], "activation", junk[:], x_[:], AF.Square, accum_out=st1[:, 0:1])
                kb.op("act", [st1_r], [st1_r], "activation", st1[:, 1:2], st1[:, 0:1], AF.Sqrt, bias=EPS, scale=1.0 / D)
                kb.op("dve", [st1_r], [st1_r], "reciprocal", st1[:, 2:3], st1[:, 1:2])
                kb.op("act", [xr, st1_r], [xn_r], "activation", xn[:], x_[:], AF.Copy, scale=st1[:, 2:3])
                for kt in range(8):
                    kb.op("pe", [xn_r, idb_r], [pT_r], "transpose", pT[:, kt, :], xn[:, kt * 128:(kt + 1) * 128], idb[:])
                kb.op("dve", [pT_r, AB1_r], [hT_r], "tensor_tensor", hT[:], pT[:], AB1[:, 0:8].unsqueeze(2).to_broadcast([128, 8, 128]), ALU.mult)
                kb.op("dve", [hT_r, AB1_r], [hT_r], "tensor_tensor", hT[:], hT[:], AB1[:, 8:16].unsqueeze(2).to_broadcast([128, 8, 128]), ALU.add)
                for grp in range(3):
                    pf, pfr = pF[grp % 2]
                    for j in range(4):
                        ct = grp * 4 + j
                        for kt in range(8):
                            kb.op("pe", [hT_r, win_r], [pfr], "matmul", pf[:, j, :], win[:, kt, ct * 128:(ct + 1) * 128], hT[:, kt, :], start=(kt == 0), stop=(kt == 7))
                    kb.op("act", [pfr], [cin_r], "activation", cin[:, grp * 4:(grp + 1) * 4, 3:131], pf[:], AF.Copy)
                for j in range(2):
                    for kt in range(8):
                        kb.op("pe", [hT_r, win_r], [pK_r], "matmul", pK[:, j, :], win[:, kt, 2696 + 64 * j + (0 if j == 0 else 0):2696 + 64 * j + 64], hT[:, kt, :], start=(kt == 0), stop=(kt == 7))
                for kt in range(8):
                    kb.op("pe", [hT_r, win_r], [pZ_r], "matmul", pZ[:], hT[:, kt, :], win[:, kt, 1536:2048], start=(kt == 0), stop=(kt == 7))
                for kt in range(8):
                    kb.op("pe", [hT_r, win_r], [pC2_r], "matmul", pC2[:], hT[:, kt, :], win[:, kt, 2048:2440], start=(kt == 0), stop=(kt == 7))
                for kt in range(8):
                    kb.op("pe", [hT_r, win_r], [pC3_r], "matmul", pC3[:], hT[:, kt, :], win[:, kt, 2440:2696], start=(kt == 0), stop=(kt == 7))
                for ct in range(12):
                    kb.op("dve", [cin_r, vecs_r], [cv_r], "tensor_scalar", cv[:, ct, :], cin[:, ct, 0:128], cwv[:, ct * 4:ct * 4 + 1], None, ALU.mult)
                    for i in range(1, 4):
                        kb.op("dve", [cin_r, vecs_r, cv_r], [cv_r], "scalar_tensor_tensor", cv[:, ct, :], cin[:, ct, i:i + 128], cwv[:, ct * 4 + i:ct * 4 + i + 1], cv[:, ct, :], ALU.mult, ALU.add)
                kb.op("pool", [cin_r], [cin_r], "tensor_copy", cin[:, :, 0:3], cin[:, :, 128:131])
                kb.op("act", [cv_r], [cs_r], "activation", cs[:], cv[:], AF.Silu)
                sa, sar = stA[b]
                for grp in range(2):
                    n = 8 if grp == 0 else 4
                    for j in range(n):
                        ct = grp * 8 + j
                        kb.op("pe", [cs_r, idb_r], [pT_r], "transpose", pT[:, j, :], cs[:, ct, :], idb[:])
                    kb.op("dve", [pT_r], [sar], "tensor_copy", sa[:, grp * 1024:grp * 1024 + n * 128], pT[:, 0:n, :])
                kb.op("act", [pZ_r], [sar], "activation", sa[:, 1536:2048], pZ[:], AF.Silu)
                kb.store("sp", qkvz_d.ap()[t0:t0 + 128, :], sa[:], sar, qkvz_r)
                g_, gr_ = gst[b]
                kb.op("dve", [pC2_r], [gr_], "tensor_copy", g_[:], pC2[:, 0:8])
                kb.store("sp", gab_d.ap()[t0:t0 + 128, :], g_[:], gr_, gab_r)
                kb.op("act", [pC2_r], [junk_r, st1_r], "activation", junk[:, 0:384], pC2[:, 8:392], AF.Square, accum_out=st1[:, 4:5])
                kb.op("act", [pC3_r], [junk_r, st1_r], "activation", junk[:, 384:640], pC3[:], AF.Square, accum_out=st1[:, 5:6])
                kb.op("act", [st1_r], [st1_r], "activation", st1[:, 6:7], st1[:, 4:5], AF.Sqrt, bias=EPS, scale=1.0 / 384)
                kb.op("act", [st1_r], [st1_r], "activation", st1[:, 7:8], st1[:, 5:6], AF.Sqrt, bias=EPS, scale=1.0 / 256)
                kb.op("dve", [st1_r], [st1_r], "reciprocal", st1[:, 8:10], st1[:, 6:8])
                kb.op("act", [pC2_r, st1_r], [cn_r], "activation", cn[:, 0:384], pC2[:, 8:392], AF.Copy, scale=st1[:, 8:9])
                kb.op("act", [pC3_r, st1_r], [cn_r], "activation", cn[:, 384:640], pC3[:], AF.Copy, scale=st1[:, 9:10])
                for j in range(5):
                    kb.op("pe", [cn_r, idb_r], [pT_r], "transpose", pT[:, j, :], cn[:, j * 128:(j + 1) * 128], idb[:])
                kb.op("dve", [pT_r, vecs_r], [cnT_r], "tensor_tensor", cnT[:], pT[:, 0:5, :], vecs[:, V_QG:V_QG + 5].unsqueeze(2).to_broadcast([128, 5, 128]), ALU.mult)
                kb.op("dve", [posi_r], [tr_r], "tensor_copy", tr[:, 0, :], posi[:])
                kb.op("dve", [tr_r, cst_r], [tr_r], "tensor_scalar", tr[:, 0, :], tr[:, 0, :], cst[0:64, C_INVF:C_INVF + 1], None, ALU.mult)
                for which, off in ((4, 0.0), (5, math.pi / 2)):
                    kb.op("dve", [tr_r], [tr_r], "tensor_scalar", tr[:, 1, :], tr[:, 0, :], off, 1.0 / (2 * math.pi), ALU.add, ALU.mult)
                    kb.op("dve", [tr_r], [tr_r], "tensor_scalar", tr[:, 2, :], tr[:, 1, :], 12582912.0, None, ALU.add)
                    kb.op("dve", [tr_r], [tr_r], "tensor_scalar", tr[:, 2, :], tr[:, 2, :], -12582912.0, None, ALU.add)
                    kb.op("dve", [tr_r], [tr_r], "tensor_tensor", tr[:, 3, :], tr[:, 1, :], tr[:, 2, :], ALU.subtract)
                    kb.op("dve", [tr_r], [tr_r], "tensor_scalar", tr[:, 3, :], tr[:, 3, :], -0.4999, 0.4999, ALU.max, ALU.min)
                    kb.op("act", [tr_r], [tr_r], "activation", tr[:, which, :], tr[:, 3, :], AF.Sin, scale=2 * math.pi)
                sinT = tr[:, 4, :]
                cosT = tr[:, 5, :]
                pq, pqr = pF[0]
                for h in range(4):
                    for kt in range(3):
                        kb.op("pe", [cnT_r, wuq_r], [pqr], "matmul", pq[:, h, :], wuq[:, kt, h * 192:h * 192 + 128], cnT[:, kt, :], start=(kt == 0), stop=(kt == 2))
                for h in range(4):
                    for kt in range(3):
                        kb.op("pe", [cnT_r, wuq_r], [pR_r], "matmul", pR[:, h, :], wuq[:, kt, h * 192 + 128:h * 192 + 192], cnT[:, kt, :], start=(kt == 0), stop=(kt == 2))
                    for kt in range(3):
                        kb.op("pe", [cnT_r, wuq_r], [pZ_r], "matmul", pZ[0:64, h * 128:(h + 1) * 128], wuq[:, kt, 768 + h * 64:768 + h * 64 + 64], cnT[:, kt, :], start=(kt == 0), stop=(kt == 2))
                q_, qr_ = qst[b]
                kb.op("act", [pqr], [qr_], "activation", q_[:], pq[:], AF.Copy, scale=QS)
                kb.op("act", [pqr], [sq1_r], "activation", sq1[:], pq[:], AF.Square, scale=QS)
                kb.store("pool", qnT_d.ap()[:, :, t0:t0 + 128].rearrange("h p t -> p h t"), q_[:], qr_, qnT_r)
                r_, rr_ = rst[b]
                kb.op("dve", [pR_r, tr_r], [t12_r], "tensor_tensor", t12[:, 0], pR[:], cosT.unsqueeze(1).to_broadcast([64, 4, 128]), ALU.mult)
                kb.op("dve", [pZ_r, tr_r], [t12_r], "tensor_tensor", t12[:, 1], pZ[0:64, :].rearrange("p (h c) -> p h c", h=4), sinT.unsqueeze(1).to_broadcast([64, 4, 128]), ALU.mult)
                kb.op("dve", [t12_r], [t12_r], "tensor_tensor", t12[:, 0], t12[:, 0], t12[:, 1], ALU.add)
                kb.op("act", [t12_r], [rr_], "activation", r_[0:64], t12[:, 0], AF.Copy, scale=QS)
                kb.op("act", [t12_r], [sq2_r], "activation", sq2[:], t12[:, 0], AF.Square, scale=QS)
                pn, pnr = pF[1]
                kb.op("pe", [sq1_r, oneb_r], [pnr], "matmul", pn[:].rearrange("p a b -> p (a b)"), oneb[:], sq1[:].rearrange("p a b -> p (a b)"), start=True, stop=False)
                kb.op("pe", [sq2_r, oneb_r], [pnr], "matmul", pn[:].rearrange("p a b -> p (a b)"), oneb[0:64, :], sq2[:].rearrange("p a b -> p (a b)"), start=False, stop=True)
                kb.op("act", [pnr], [rr_], "activation", r_[64:65], pn[64:65], AF.Sqrt, scale=1.0609)
                kb.store("pool", qrT_d.ap()[:, :, t0:t0 + 128].rearrange("h p t -> p h t"), r_[:], rr_, qrT_r)
                pk2, pk2r = pF[0]
                for h in range(4):
                    for kt in range(2):
                        kb.op("pe", [cnT_r, wukv_r], [pk2r], "matmul", pk2[:, h, :], wukv[:, kt, h * 256:h * 256 + 128], cnT[:, 3 + kt, :], start=(kt == 0), stop=(kt == 1))
                k_, kr_ = kst[b]
                kb.op("act", [pk2r], [kr_], "activation", k_[:], pk2[:], AF.Copy)
                kb.op("act", [pk2r], [sq1_r], "activation", sq1[:], pk2[:], AF.Square)
                kb.store("pool", knT_d.ap()[:, :, t0:t0 + 128].rearrange("h p t -> p h t"), k_[:], kr_, knT_r)
                kr2, kr2r = krst[b]
                kb.op("dve", [pK_r, tr_r], [t12_r], "tensor_tensor", t12[:, 0, 0], pK[:, 0, :], cosT, ALU.mult)
                kb.op("dve", [pK_r, tr_r], [t12_r], "tensor_tensor", t12[:, 1, 0], pK[:, 1, :], sinT, ALU.mult)
                kb.op("dve", [t12_r], [t12_r], "tensor_tensor", t12[:, 0, 0], t12[:, 0, 0], t12[:, 1, 0], ALU.add)
                kb.op("act", [t12_r], [kr2r], "activation", kr2[0:64], t12[:, 0, 0], AF.Copy)
                kb.op("act", [t12_r], [sq2_r], "activation", sq2[:, 0, :], t12[:, 0, 0], AF.Square)
                kb.store("pool", krT_d.ap()[:, t0:t0 + 128], kr2[:], kr2r, krT_r)
                pn2, pn2r = pF[1]
                kb.op("pe", [sq1_r, oneb_r], [pn2r], "matmul", pn2[:].rearrange("p a b -> p (a b)"), oneb[:], sq1[:].rearrange("p a b -> p (a b)"), start=True, stop=True)
                kb.op("pe", [sq2_r, oneb_r], [pC3_r], "matmul", pC3[:, 0:128], oneb[0:64, :], sq2[:, 0, :], start=True, stop=True)
                kb.op("dve", [pn2r], [tmx_r], "tensor_reduce", tmx[:], pn2[:], AX.X, ALU.max)
                kb.op("dve", [tmx_r, kmax_r], [kmax_r], "tensor_tensor", kmax[:, 0:4], kmax[:, 0:4], tmx[:], ALU.max)
                kb.op("dve", [pC3_r], [tmx_r], "tensor_reduce", tmx[:, 0:1], pC3[:, 0:128], AX.X, ALU.max)
                kb.op("dve", [tmx_r, kmax_r], [kmax_r], "tensor_tensor", kmax[:, 4:5], kmax[:, 4:5], tmx[:, 0:1], ALU.max)
                for kt in range(2):
                    kb.op("pe", [cnT_r, wukv_r], [pZ_r], "matmul", pZ[:].rearrange("p (h c) -> p h c", h=4), cnT[:, 3 + kt, :], wukv[:, kt, :].rearrange("p (h c) -> p h c", h=4)[:, :, 128:256], start=(kt == 0), stop=(kt == 1))
                v_, vr_ = vst[b]
                kb.op("dve", [pZ_r], [vr_], "tensor_copy", v_[:, :, 0:128], pZ[:].rearrange("p (h c) -> p h c", h=4))
                kb.store("pool", v_d.ap()[:, t0:t0 + 128, :].rearrange("h p c -> p h c"), v_[:], vr_, v_r)
            kb.barrier()

        with ExitStack() as pb:
            f32t = lambda n, shp=(128, 128): kb.sb(n, list(shp), F32, pb)
            bft = lambda n, shp=(128, 128): kb.sb(n, list(shp), BF16, pb)
            qz = [bft("qz%d" % i, (128, 2048)) for i in range(3)]
            gabt = [f32t("gabt%d" % i, (128, 8)) for i in range(3)]
            gts = [f32t("gt%d" % i, (128, 64)) for i in range(3)]
            nega, nega_r = f32t("nega", (128, 4))
            o_sb, o_r = f32t("o_sb", (128, 4, 128))
            osq, osq_r = f32t("osq", (128, 4, 128))
            ost, ost_r = f32t("ost", (128, 12))
            omg, omg_r = bft("omg", (128, 4, 128))
            oT = [bft("oT%d" % i, (128, 4, 128)) for i in range(2)]
            pTb = [kb.ps("pTbB%d" % i, [128, 8, 128], BF16, pb) for i in range(2)]
            pG, pG_r = kb.ps("pGB", [128, 512], F32, pb)
            pW = [kb.ps("pWB%d" % h, [128, 4, 128], F32, pb)[0] for h in range(4)]
            pWr = [[Res("pW%d" % h, excl=True)] * 4 for h in range(4)]
            HH = [[], []]
            for h in range(4):
                for par in range(2):
                    Td = {}
                    for n in ("stq",):
                        Td[n] = f32t("%s%d_%d" % (n, h, par), (128, 16))
                    for n in ("kbg", "vb", "dg", "decM", "decAT", "Ma", "Mb", "Na", "Nb", "Pa", "Pb", "MI", "u"):
                        Td[n] = f32t("%s%d_%d" % (n, h, par))
                    for n in ("junk", "kn", "qs", "kd0", "kd1", "dq", "kT", "qT", "qdT", "attnT", "wT"):
                        Td[n] = bft("%s%d_%d" % (n, h, par))
                    if par == 0:
                        Td["S"] = f32t("S%d" % h)
                        Td["vnew"] = bft("vnew%d" % h)
                        Td["Sb"] = bft("Sb%d" % h)
                        kb.op("pool", [], [Td["vnew"][1]], "memset", Td["vnew"][0][:], 0.0)
                        kb.op("pool", [], [Td["S"][1]], "memset", Td["S"][0][:], 0.0)
                    else:
                        for n in ("S", "vnew", "Sb"):
                            Td[n] = HH[0][h][n]
                    HH[par].append(Td)
            kb.op("act", [rowv_r], [nega_r], "activation", nega[:], rowv[:, R_ALOG:R_ALOG + 4], AF.Exp)
            kb.op("dve", [nega_r], [nega_r], "tensor_scalar", nega[:], nega[:], -1.0, None, ALU.mult)
            identf = cst[:, C_ID:C_ID + 128]
            def gdn_tile(ti):
                t0 = ti * 128
                b = ti % 2
                q_, qr_ = qz[ti % 3]
                ga_, gar_ = gabt[ti % 3]
                gt, gtr = gts[ti % 3]
                H = HH[b]
                V = lambda r, w, m, *a, **k: kb.op("dve", r, w, m, *a, **k)
                A = lambda r, w, *a, **k: kb.op("act", r, w, "activation", *a, **k)

                gsteps = []

                def g0():
                    kb.load("sp", q_[:], qkvz_d.ap()[t0:t0 + 128, :], qr_, reads=[qkvz_r])
                    kb.load("sp", ga_[:], gab_d.ap()[t0:t0 + 128, :], gar_, reads=[gab_r])
                    V([gar_, rowv_r], [gtr], "tensor_tensor", gt[:, 0:4], ga_[:, 0:4], rowv[:, R_DTB:R_DTB + 4], ALU.add)
                    V([gtr], [gtr], "tensor_scalar", gt[:, 4:8], gt[:, 0:4], -1.0, None, ALU.mult)
                    V([gtr], [gtr], "tensor_tensor", gt[:, 4:8], gt[:, 4:8], gt[:, 0:4], ALU.max)
                gsteps.append(g0)

                def g1():
                    A([gtr], [gtr], gt[:, 8:12], gt[:, 4:8], AF.Exp, scale=-1.0)
                gsteps.append(g1)

                def g2():
                    V([gtr], [gtr], "tensor_scalar", gt[:, 12:16], gt[:, 8:12], 2.0, None, ALU.add)
                    V([gtr], [gtr], "reciprocal", gt[:, 12:16], gt[:, 12:16])
                    V([gtr], [gtr], "tensor_tensor", gt[:, 12:16], gt[:, 12:16], gt[:, 8:12], ALU.mult)
                    V([gtr], [gtr], "tensor_tensor", gt[:, 52:56], gt[:, 12:16], gt[:, 12:16], ALU.mult)
                    V([gtr], [gtr], "tensor_scalar", gt[:, 56:60], gt[:, 52:56], 1.0 / 11, 1.0 / 9, ALU.mult, ALU.add)
                    for cf in (1.0 / 7, 1.0 / 5, 1.0 / 3, 1.0):
                        V([gtr], [gtr], "tensor_tensor", gt[:, 56:60], gt[:, 56:60], gt[:, 52:56], ALU.mult)
                        V([gtr], [gtr], "tensor_scalar", gt[:, 56:60], gt[:, 56:60], cf, None, ALU.add)
                    V([gtr], [gtr], "scalar_tensor_tensor", gt[:, 12:16], gt[:, 12:16], 2.0, gt[:, 56:60], ALU.mult, ALU.mult)
                    V([gtr], [gtr], "tensor_scalar", gt[:, 16:20], gt[:, 0:4], 0.0, None, ALU.max)
                    V([gtr], [gtr], "tensor_tensor", gt[:, 16:20], gt[:, 16:20], gt[:, 12:16], ALU.add)
                    V([gtr, nega_r], [gtr], "tensor_tensor", gt[:, 20:24], gt[:, 16:20], nega[:], ALU.mult)
                gsteps.append(g2)

                def g3():
                    A([gar_], [gtr], gt[:, 24:28], ga_[:, 4:8], AF.Sigmoid)
                gsteps.append(g3)

                def g4():
                    kb.op("pe", [gtr, cst_r], [pG_r], "matmul", pG[:, 0:4], cst[:, C_U:C_U + 128], gt[:, 20:24], start=True, stop=True)
                gsteps.append(g4)

                def g5():
                    V([pG_r], [gtr], "tensor_copy", gt[:, 28:32], pG[:, 0:4])
                    V([gtr], [gtr], "tensor_scalar", gt[:, 32:36], gt[:, 28:32], -1.0, None, ALU.mult)
                gsteps.append(g5)

                def g6():
                    A([gtr], [gtr], gt[:, 36:40], gt[:, 28:32], AF.Exp)
                gsteps.append(g6)

                def g7():
                    kb.op("pe", [gtr, cst_r], [pG_r], "matmul", pG[:, 4:8], cst[:, C_LS:C_LS + 128], gt[:, 28:32], start=True, stop=True)
                    kb.op("pe", [gtr, cst_r], [pG_r], "matmul", pG[:, 8:12], cst[:, C_SL0:C_SL0 + 128], gt[:, 28:32], start=True, stop=True)
                    kb.op("pe", [gtr, cst_r], [pG_r], "matmul", pG[:, 12:16], cst[:, C_SL1:C_SL1 + 128], gt[:, 28:32], start=True, stop=True)
                gsteps.append(g7)

                def g8():
                    V([pG_r, gtr], [gtr], "tensor_tensor", gt[:, 52:56], pG[:, 4:8], gt[:, 28:32], ALU.subtract)
                gsteps.append(g8)

                def g9():
                    A([gtr], [gtr], gt[:, 40:44], gt[:, 52:56], AF.Exp)
                    A([pG_r], [gtr], gt[:, 44:52], pG[:, 8:16], AF.Exp)
                gsteps.append(g9)

                def g10():
                    V([gtr], [gtr], "tensor_tensor", gt[:, 56:60], gt[:, 24:28], gt[:, 36:40], ALU.mult)
                gsteps.append(g10)


                def s1(h):
                    T = H[h]
                    stq, sr = T["stq"]
                    qh = q_[:, h * 128:(h + 1) * 128]; kh = q_[:, 512 + h * 128:512 + (h + 1) * 128]; vh = q_[:, 1024 + h * 128:1024 + (h + 1) * 128]
                    A([qr_], [T["junk"][1], sr], T["junk"][0][:], qh, AF.Square, accum_out=stq[:, 0:1])
                    A([qr_], [T["junk"][1], sr], T["junk"][0][:], kh, AF.Square, accum_out=stq[:, 1:2])
                    A([sr], [sr], stq[:, 2:4], stq[:, 0:2], AF.Sqrt, bias=EPS)
                    V([sr], [sr], "reciprocal", stq[:, 4:6], stq[:, 2:4])
                    V([sr], [sr], "tensor_scalar", stq[:, 6:7], stq[:, 4:5], 128.0 ** -0.5, None, ALU.mult)
                    V([sr, gtr], [sr], "tensor_tensor", stq[:, 7:8], stq[:, 5:6], gt[:, 40 + h:41 + h], ALU.mult)
                    V([sr, cst_r], [sr], "tensor_scalar", stq[:, 8:10], cst[:, C_IND0:C_IND0 + 2], stq[:, 7:8], None, ALU.mult)
                    V([sr, gtr], [sr], "tensor_tensor", stq[:, 10:11], stq[:, 5:6], gt[:, 56 + h:57 + h], ALU.mult)

                def s1b(h):
                    T = H[h]
                    stq, sr = T["stq"]
                    qh = q_[:, h * 128:(h + 1) * 128]; kh = q_[:, 512 + h * 128:512 + (h + 1) * 128]; vh = q_[:, 1024 + h * 128:1024 + (h + 1) * 128]
                    A([qr_, sr], [T["kn"][1]], T["kn"][0][:], kh, AF.Copy, scale=stq[:, 5:6])
                    A([qr_, sr], [T["qs"][1]], T["qs"][0][:], qh, AF.Copy, scale=stq[:, 6:7])
                    V([qr_, sr], [T["kd0"][1]], "tensor_scalar", T["kd0"][0][:], kh, stq[:, 8:9], None, ALU.mult)
                    V([qr_, sr], [T["kd1"][1]], "tensor_scalar", T["kd1"][0][:], kh, stq[:, 9:10], None, ALU.mult)
                    V([qr_, sr], [T["kbg"][1]], "tensor_scalar", T["kbg"][0][:], kh, stq[:, 10:11], None, ALU.mult)
                    V([qr_, gtr], [T["vb"][1]], "tensor_scalar", T["vb"][0][:], vh, gt[:, 24 + h:25 + h], None, ALU.mult)
                    V([cst_r, gtr], [T["dg"][1]], "tensor_scalar", T["dg"][0][:], identf, gt[:, 28 + h:29 + h], None, ALU.mult)
                    V([idb_r, gtr], [T["dq"][1]], "tensor_scalar", T["dq"][0][:], idb[:], gt[:, 36 + h:37 + h], None, ALU.mult)

                def s2(h):
                    T = H[h]
                    pt, ptr = pTb[h // 2]
                    s0 = (h % 2) * 4
                    kb.op("pe", [T["kn"][1], idb_r], [ptr], "transpose", pt[:, s0, :], T["kn"][0][:], idb[:])
                    kb.op("pe", [T["qs"][1], idb_r], [ptr], "transpose", pt[:, s0 + 1, :], T["qs"][0][:], idb[:])
                    kb.op("pe", [T["qs"][1], T["dq"][1]], [pWr[h][3]], "matmul", pW[h][:, 3, :], T["qs"][0][:], T["dq"][0][:], start=True, stop=True)
                    V([ptr], [T["kT"][1]], "tensor_copy", T["kT"][0][:], pt[:, s0, :])
                    V([ptr], [T["qT"][1]], "tensor_copy", T["qT"][0][:], pt[:, s0 + 1, :])
                    A([pWr[h][3]], [T["qdT"][1]], T["qdT"][0][:], pW[h][:, 3, :], AF.Copy)

                def s3(h):
                    T = H[h]
                    kb.op("pe", [T["kT"][1]], [pWr[h][0]], "matmul", pW[h][:, 0, :], T["kT"][0][:], T["kT"][0][:], start=True, stop=True)
                    kb.op("pe", [T["kT"][1], T["qT"][1]], [pWr[h][1]], "matmul", pW[h][:, 1, :], T["kT"][0][:], T["qT"][0][:], start=True, stop=True)
                    kb.op("pe", [T["dg"][1], cst_r], [pWr[h][2]], "matmul", pW[h][:, 2, :], ones, T["dg"][0][:], start=True, stop=False)
                    kb.op("pe", [cst_r], [pWr[h][2]], "matmul", pW[h][:, 2, :], identf, cst[:, C_PM:C_PM + 128], start=False, stop=True)
                    kb.op("pe", [T["dg"][1], cst_r], [pWr[h][3]], "matmul", pW[h][:, 3, :], ones, T["dg"][0][:], start=True, stop=False)
                    kb.op("pe", [cst_r], [pWr[h][3]], "matmul", pW[h][:, 3, :], identf, cst[:, C_NA:C_NA + 128], start=False, stop=True)

                def s3b(h):
                    T = H[h]
                    A([pWr[h][2], gtr], [T["decM"][1]], T["decM"][0][:], pW[h][:, 2, :], AF.Exp, bias=gt[:, 28 + h:29 + h], scale=-1.0)
                    A([pWr[h][3], gtr], [T["decAT"][1]], T["decAT"][0][:], pW[h][:, 3, :], AF.Exp, bias=gt[:, 32 + h:33 + h], scale=1.0)
                    V([pWr[h][0], gtr, T["decM"][1]], [T["Ma"][1]], "scalar_tensor_tensor", T["Ma"][0][:], pW[h][:, 0, :], gt[:, 24 + h:25 + h], T["decM"][0][:], ALU.mult, ALU.mult)
                    V([pWr[h][1], T["decAT"][1]], [T["attnT"][1]], "tensor_tensor", T["attnT"][0][:], pW[h][:, 1, :], T["decAT"][0][:], ALU.mult)

                def s4(h):
                    T = H[h]
                    kb.op("pe", [T["Ma"][1], cst_r], [pWr[h][0]], "transpose", pW[h][:, 0, :], T["Ma"][0][:], identf)
                    A([pWr[h][0]], [T["Na"][1]], T["Na"][0][:], pW[h][:, 0, :], AF.Copy)
                    V([pWr[h][0], cst_r], [T["Pa"][1]], "scalar_tensor_tensor", T["Pa"][0][:], pW[h][:, 0, :], -1.0, identf, ALU.mult, ALU.add)

                def chain(L):
                    def f(h):
                        T = H[h]
                        cur, nxt = ("a", "b") if L % 2 == 1 else ("b", "a")
                        Mc, Nc, Pc = T["M" + cur], T["N" + cur], T["P" + cur]
                        Mn, Nn, Pn = T["M" + nxt], T["N" + nxt], T["P" + nxt]
                        kb.op("pe", [Mc[1], Nc[1]], [pWr[h][0]], "matmul", pW[h][:, 0, :], Nc[0][:], Mc[0][:], start=True, stop=True)
                        if L < 5:
                            kb.op("pe", [Mc[1], Nc[1]], [pWr[h][1]], "matmul", pW[h][:, 1, :], Mc[0][:], Nc[0][:], start=True, stop=True)
                        V([pWr[h][0], cst_r], [T["MI"][1]], "tensor_tensor", T["MI"][0][:], pW[h][:, 0, :], identf, ALU.add)
                        if L < 5:
                            A([pWr[h][0]], [Mn[1]], Mn[0][:], pW[h][:, 0, :], AF.Copy)
                            A([pWr[h][1]], [Nn[1]], Nn[0][:], pW[h][:, 1, :], AF.Copy)

                    def f2(h):
                        T = H[h]
                        cur, nxt = ("a", "b") if L % 2 == 1 else ("b", "a")
                        Pc, Pn = T["P" + cur], T["P" + nxt]
                        kb.op("pe", [T["MI"][1], Pc[1]], [pWr[h][2]], "matmul", pW[h][:, 2, :], T["MI"][0][:], Pc[0][:], start=True, stop=True)
                        V([pWr[h][2]], [Pn[1]], "tensor_copy", Pn[0][:], pW[h][:, 2, :])
                    return [f, f2]

                def s10(h):
                    T = H[h]
                    TT = T["Pb"]
                    kb.op("pe", [TT[1], T["vb"][1]], [pWr[h][0]], "matmul", pW[h][:, 0, :], TT[0][:], T["vb"][0][:], start=True, stop=True)
                    kb.op("pe", [TT[1], T["kbg"][1]], [pWr[h][1]], "matmul", pW[h][:, 1, :], T["kbg"][0][:], TT[0][:], start=True, stop=True)
                    A([pWr[h][0]], [T["u"][1]], T["u"][0][:], pW[h][:, 0, :], AF.Copy)
                    V([pWr[h][1]], [T["wT"][1]], "tensor_copy", T["wT"][0][:], pW[h][:, 1, :])

                def scan(c):
                    rows = slice(c * 64, (c + 1) * 64)

                    def fa(h):
                        T = H[h]
                        A([T["S"][1]], [T["Sb"][1]], T["Sb"][0][:], T["S"][0][:], AF.Copy)
                        kb.op("pe", [T["wT"][1], T["Sb"][1]], [pWr[h][2]], "matmul", pW[h][:, 2, :], T["wT"][0][:], T["Sb"][0][:], start=True, stop=True)

                    def fb(h):
                        T = H[h]
                        V([T["u"][1], pWr[h][2]], [T["vnew"][1]], "tensor_tensor", T["vnew"][0][rows, :], T["u"][0][rows, :], pW[h][rows, 2, :], ALU.subtract)
                        kb.op("pe", [T["qdT"][1], T["Sb"][1]], [pWr[h][c]], "matmul", pW[h][:, c, :], T["qdT"][0][:], T["Sb"][0][:], start=True, stop=False)
                        kb.op("pe", [T["attnT"][1], T["vnew"][1]], [pWr[h][c]], "matmul", pW[h][:, c, :], T["attnT"][0][:], T["vnew"][0][:], start=False, stop=True)
                        kd = T["kd%d" % c]
                        kb.op("pe", [kd[1], T["vnew"][1]], [pWr[h][3]], "matmul", pW[h][:, 3, :], kd[0][:], T["vnew"][0][:], start=True, stop=True)

                    def fc(h):
                        T = H[h]
                        A([pWr[h][c]], [o_r], o_sb[rows, h, :], pW[h][rows, c, :], AF.Copy)
                        V([T["S"][1], gtr, pWr[h][3]], [T["S"][1]], "scalar_tensor_tensor", T["S"][0][:], T["S"][0][:], gt[:, 44 + c * 4 + h:45 + c * 4 + h], pW[h][:, 3, :], ALU.mult, ALU.add)
                    return [fa, fb, fc]

                def _final():
                    V([o_r], [osq_r], "tensor_tensor", osq[:], o_sb[:], o_sb[:], ALU.mult)
                    V([osq_r], [ost_r], "tensor_reduce", ost[:, 0:4], osq[:], AX.X, ALU.add)
                    A([ost_r], [ost_r], ost[:, 4:8], ost[:, 0:4], AF.Sqrt, bias=EPS, scale=1.0 / 128)
                    V([ost_r], [ost_r], "reciprocal", ost[:, 8:12], ost[:, 4:8])
                    V([o_r, ost_r], [osq_r], "tensor_tensor", osq[:], o_sb[:], ost[:, 8:12].unsqueeze(2).to_broadcast([128, 4, 128]), ALU.mult)
                    V([osq_r, rowv_r], [osq_r], "tensor_tensor", osq[:], osq[:], rowv[:, R_GG:R_GG + 128].unsqueeze(1).to_broadcast([128, 4, 128]), ALU.mult)
                    V([osq_r, qr_], [omg_r], "tensor_tensor", omg[:], osq[:], q_[:, 1536:2048].rearrange("p (h c) -> p h c", h=4), ALU.mult)
                    pt, ptr = pTb[ti % 2]
                    for h in range(4):
                        kb.op("pe", [omg_r, idb_r], [ptr], "transpose", pt[:, (3 if h % 2 == 0 else 7) - (h // 2) * 0 - (0), :] if False else pt[:, [2, 3, 6, 7][h], :], omg[:, h, :], idb[:])
                    ot, otr = oT[b]
                    V([ptr], [otr], "tensor_copy", ot[:, 0:2, :], pt[:, 2:4, :])
                    V([ptr], [otr], "tensor_copy", ot[:, 2:4, :], pt[:, 6:8, :])
                    kb.store("sp", omixT_d.ap()[0:512, t0:t0 + 128].rearrange("(h p) t -> p h t", p=128), ot[:], otr, omix_r)

                pre = []
                stages = [s1, s1b, s2, s3, s3b, s4] + chain(1) + chain(2) + chain(3) + chain(4) + chain(5) + [s10]
                for step in range(len(stages) + 3):
                    for h in range(4):
                        k = step - h
                        if 0 <= k < len(stages):
                            pre.append(lambda st=stages[k], h=h: st(h))
                scans = []
                for c in range(2):
                    subs = scan(c)
                    L = []
                    for step in range(len(subs) + 3):
                        for h in range(4):
                            k = step - h
                            if 0 <= k < len(subs):
                                L.append(lambda f=subs[k], h=h: f(h))
                    scans.append(L)

                return pre, scans, _final, gsteps

            GT2, GT2_r = kb.sb("GT2", [128, D], F32, pb)
            kb.load("sp", GT2[:], mod_d.ap()[5120:6144].partition_broadcast(128), GT2_r, reads=[mod_r])
            stw = [kb.sb("cvw%d" % i, [128, 4096], F32, pb) for i in range(2)]
            stb = [kb.sb("cvb%d" % i, [128, 4096], BF16, pb) for i in range(2)]
            NCH = 16384 // 512
            conv_steps = []
            for ci in range(NCH):
                for which in range(2):
                    def f_cv(ci=ci, which=which):
                        s_, sr = stw[which]
                        b_, br = stb[which]
                        src = (eu_d, ev_d)[which].ap()[ci * 512:(ci + 1) * 512, :].rearrange("(p r) c -> p r c", r=4)
                        dst = uvb_d.ap()[ci * 512:(ci + 1) * 512, which * 1024:(which + 1) * 1024].rearrange("(p r) c -> p r c", r=4)
                        kb.load("pool", s_[:].rearrange("p (r c) -> p r c", r=4), src, sr)
                        if which == 0:
                            kb.op("act", [sr], [br], "activation", b_[:], s_[:], AF.Copy)
                        else:
                            kb.op("dve", [sr, GT2_r], [br], "tensor_tensor", b_[:].rearrange("p (r c) -> p r c", r=4), s_[:].rearrange("p (r c) -> p r c", r=4), GT2[:].unsqueeze(1).to_broadcast([128, 4, 1024]), ALU.mult)
                        kb.store("pool", dst, b_[:].rearrange("p (r c) -> p r c", r=4), br, uvb_r)
                    conv_steps.append(f_cv)
            cvi = [0]

            def emit_conv(n):
                while n > 0 and cvi[0] < len(conv_steps):
                    conv_steps[cvi[0]]()
                    cvi[0] += 1
                    n -= 1
            GT_ = {}

            def gtile(ti):
                if ti >= NT:
                    return [], None, None, []
                if ti not in GT_:
                    GT_[ti] = gdn_tile(ti)
                return GT_[ti]

            def merge2(A_, B_):
                out, ia, ib = [], 0, 0
                na, nb = len(A_), len(B_)
                for k in range(na + nb):
                    if ib >= nb or (ia < na and (ia + 1) * (nb + 1) <= (ib + 1) * (na + 1)):
                        out.append(A_[ia]); ia += 1
                    else:
                        out.append(B_[ib]); ib += 1
                return out
            for f in gtile(0)[3] + gtile(1)[3] + gtile(0)[0]:
                f()
            cur = gtile(0)
            for ti in range(NT):
                emit_conv(1)
                nxt = gtile(ti + 1)
                npre = merge2(nxt[0], gtile(ti + 2)[3])
                h1 = len(npre) // 2
                for f in cur[1][0]:
                    f()
                for f in npre[:h1]:
                    f()
                for f in cur[1][1]:
                    f()
                for f in npre[h1:]:
                    f()
                cur[2]()
                cur = nxt
                GT_.pop(ti, None)
            emit_conv(len(conv_steps))
            kb.barrier()

        with ExitStack() as pd:
            wout, wout_r = kb.sb("wout", [128, 8, 1024], BF16, pd)
            wpq, wpq_r = kb.sb("wpq", [128, 8, 2048], BF16, pd)
            skT, skT_r = kb.sb("skTb", [128, 16, 128], BF16, pd)
            A2b, A2b_r = kb.sb("A2b", [128, D], F32, pd)
            B2b, B2b_r = kb.sb("B2b", [128, D], F32, pd)
            FNG, FNG_r = kb.sb("FNG", [128, D], F32, pd)
            bc = lambda a, n: mod_d.ap()[a:a + n].partition_broadcast(128)
            kb.load("sp", A2b[:], bc(4096, 1024), A2b_r, reads=[mod_r])
            kb.load("sp", B2b[:], bc(3072, 1024), B2b_r, reads=[mod_r])
            kb.load("sp", FNG[:], lnf_d.ap().partition_broadcast(128), FNG_r)
            kb.op("dve", [A2b_r, FNG_r], [A2b_r], "scalar_tensor_tensor", A2b[:], A2b[:], 1.0, FNG[:], ALU.add, ALU.mult)
            kb.load("sp", FNG[:], fng_d.ap().partition_broadcast(128), FNG_r)
            with ExitStack() as pw:
                GT1, GT1_r = kb.sb("GT1", [128, D], F32, pw)
                kb.load("sp", GT1[:], bc(2048, 1024), GT1_r, reads=[mod_r])
                stw = [kb.sb("stw%d" % i, [128, 4096], F32, pw) for i in range(2)]
                stb = [kb.sb("stb%d" % i, [128, 4096], BF16, pw) for i in range(2)]
                for kt in range(8):
                    s_, sr = stw[kt % 2]
                    kb.load("sp", s_[:, 0:1024], wout_d.ap()[kt * 128:(kt + 1) * 128, :], sr)
                    kb.op("dve", [sr, GT1_r], [wout_r], "tensor_tensor", wout[:, kt, :], s_[:, 0:1024], GT1[:], ALU.mult)
                    s2, sr2 = stw[(kt + 1) % 2]
                    kb.load("sp", s2[:, 0:2048], wpq_d.ap()[kt * 128:(kt + 1) * 128, :], sr2)
                    kb.op("act", [sr2], [wpq_r], "activation", wpq[:, kt, :], s2[:, 0:2048], AF.Copy)
                s_, sr = stw[0]
                kb.load("sp", s_[:, 0:2048], skT_d.ap(), sr)
                kb.op("dve", [sr], [skT_r], "tensor_copy", skT[:].rearrange("p a b -> p (a b)"), s_[:, 0:2048])
                kb.barrier()
            GS = 4
            NG = 128 // GS
            NBUF = 3
            uvg = [(kb.sb("uvg%d" % i, [128, GS, 2048], BF16, pd)[0], [Res("uvg%d_%d" % (i, j)) for j in range(GS)]) for i in range(NBUF)]
            dg = [kb.sb("dgp%d" % i, [128, 128], BF16, pd) for i in range(4)]
            two = lambda n, shp, dt: [kb.sb("%s%d" % (n, i), shp, dt, pd) for i in range(2)]
            om = [kb.sb("om0", [128, 8, 128], BF16, pd)] * 2
            xt = [kb.sb("xtD0", [128, D], F32, pd)] * 2
            x1s = [kb.sb("x1_%d" % i, [128, D], F32, pd) for i in range(3)]
            h2s = [kb.sb("h2_%d" % i, [128, D], BF16, pd) for i in range(3)]
            eids = two("eid", [128, 128], I32)
            gates = two("gate", [128, 8, 16], F32)
            ob = xt
            junkb, junkb_r = kb.sb("junkDb", [128, D], BF16, pd)
            junkv, junkv_r = kb.sb("junkDv", [128, D], BF16, pd)
            h2T, h2T_r = kb.sb("h2T", [128, 8, 128], BF16, pd)
            qTb, qTb_r = kb.sb("qTb", [128, 16, 128], BF16, pd)
            scs = two("scD", [128, 16, 128], F32)
            iu1, iu1_r = kb.sb("iu1", [128, 16, 16], U32, pd)
            iu2, iu2_r = kb.sb("iu2", [128, 8, 16], U32, pd)
            wk, wk_r = kb.sb("wkD", [128, 256], F32, pd)
            stp, stp_r = kb.sb("stop", [128, 16, 16], F32, pd)
            itp, itp_r = kb.sb("itop", [128, 16, 16], F32, pd)
            best, best_r = kb.sb("best", [128, 8, 16], F32, pd)
            posf, posf_r = kb.sb("posf", [128, 8, 16], F32, pd)
            ab, ab_r = kb.sb("abD", [128, 4, 8, 16], F32, pd)
            isel, isel_r = kb.sb("isel", [128, 2, 8, 16], F32, pd)
            eidf, eidf_r = kb.sb("eidf", [128, 128], F32, pd)
            gs, gs_r = kb.sb("gsD", [128, 16], F32, pd)
            dots, dots_r = kb.sb("dots", [128, 128], F32, pd)
            ges = [kb.sb("geD%d" % i, [128, 4, GS], F32, pd) for i in range(3)]
            hgs = [kb.sb("hgD%d" % i, [128, GS], F32, pd) for i in range(3)]
            coef, coef_r = kb.sb("coef", [128, 128], F32, pd)
            stD, stD_r = kb.sb("stD", [128, 8], F32, pd)
            stE, stE_r = kb.sb("stE", [128, 8], F32, pd)
            pM, pM_r = kb.ps("pMD", [128, 2, 512], F32, pd)
            pX, pX_r = kb.ps("pXD", [128, 512], F32, pd)
            pTd, pTd_r = kb.ps("pTD", [128, 8, 128], BF16, pd)
            pQ, pQ_r = kb.ps("pQD", [128, 4, 128], F32, pd)
            pS, pS_r = pQ, pQ_r
            STps = [kb.ps("STp%d" % i, [128, 4, 128], F32, pd) for i in range(2)]
            Op, Op_r = kb.ps("Op", [128, 129], F32, pd)
            nk, nk_r = kb.sb("nk", [128, 8], F32, pd)
            cnb, cnb_r = kb.sb("cnb", [128, 128], BF16, pd)
            Qn2 = [kb.sb("Qn%d" % i, [128, 128], BF16, pd) for i in range(2)]
            Qr2 = [kb.sb("Qr%d" % i, [65, 128], BF16, pd) for i in range(2)]
            Kg = [kb.sb("Kg%d" % i, [128, 512], BF16, pd) for i in range(4)]
            Krg = [kb.sb("Krg%d" % i, [65, 512], BF16, pd) for i in range(4)]
            Vg = [kb.sb("Vg%d" % i, [128, 4, 129], BF16, pd) for i in range(4)]
            PTs = [kb.sb("PT%d" % i, [128, 4, 128], BF16, pd) for i in range(2)]
            rc, rc_r = kb.sb("rc", [128, 2], F32, pd)
            onb, onb_r = kb.sb("onb", [128, 128], BF16, pd)
            kb.op("dve", [kmax_r], [nk_r], "tensor_scalar", nk[:, 0:4], kmax[:, 0:4], kmax[:, 4:5], None, ALU.add)
            kb.op("act", [nk_r], [nk_r], "activation", nk[:, 4:8], nk[:, 0:4], AF.Sqrt, scale=1.0609)
            kb.op("dve", [nk_r], [nk_r], "tensor_scalar", nk[:, 4:8], nk[:, 4:8], -1.0, None, ALU.mult)
            kb.op("dve", [cst_r], [cnb_r], "tensor_copy", cnb[:], cst[:, C_CN:C_CN + 128])
            mla_cnt = [0]
            V = lambda r, w, m, *a, **k: kb.op("dve", r, w, m, *a, **k)
            A = lambda r, w, *a, **k: kb.op("act", r, w, "activation", *a, **k)
            iota16 = cst[:, C_IOTA:C_IOTA + 16]

            def front(ti):
                t0 = ti * 128
                b = ti % 2
                o_, omr = om[b]
                x_, xr = xt[b]
                x1, x1_r = x1s[ti % 3]
                h2, h2_r = h2s[ti % 3]
                eid, eid_r = eids[b]
                gate, gate_r = gates[b]
                sc, sc_r = scs[b]
                cand, cand_r = sc[:].rearrange("p (h t) k -> p h (t k)", t=2), sc_r
                eq, eq_r = cand[:].rearrange("p h (a b) -> p h a b", a=16), sc_r
                steps = []
                add = steps.append

                qi = ti
                units = []
                for h in range(4):
                    units.append(("q", h, 0))
                    for g0 in range(0, qi + 1, 4):
                        units.append(("g", h, g0))
                    units.append(("fin", h, 0))
                gidx = {}
                for u in units:
                    if u[0] == "g":
                        gidx[u] = mla_cnt[0]
                        mla_cnt[0] += 1

                def u_load(u):
                    kind, h, g0 = u
                    if kind == "q":
                        qn, qnr = Qn2[h % 2]
                        qr, qrr = Qr2[h % 2]
                        kb.load("sp", qn[:], qnT_d.ap()[h, :, t0:t0 + 128], qnr, reads=[qnT_r])
                        kb.load("sp", qr[:], qrT_d.ap()[h, :, t0:t0 + 128], qrr, reads=[qrT_r])
                    elif kind == "g":
                        c = gidx[u] % 4
                        n = min(g0 + 4, qi + 1) - g0
                        kg, kgr = Kg[c]
                        krg, krgr = Krg[c]
                        vg_, vgr = Vg[c]
                        kb.load("sp", kg[:, 0:n * 128], knT_d.ap()[h, :, g0 * 128:(g0 + n) * 128], kgr, reads=[knT_r])
                        kb.load("sp", krg[:, 0:n * 128], krT_d.ap()[:, g0 * 128:(g0 + n) * 128], krgr, reads=[krT_r])
                        kb.load("sp", vg_[:, 0:n, :], v_d.ap()[h, g0 * 128:(g0 + n) * 128, :].rearrange("(n p) c -> p n c", p=128), vgr, reads=[v_r])

                def u_comp(u):
                    kind, h, g0 = u
                    qn, qnr = Qn2[h % 2]
                    qr, qrr = Qr2[h % 2]
                    if kind == "q":
                        V([qrr, nk_r], [qrr], "tensor_scalar", qr[64:65, :], qr[64:65, :], nk[64:65, 4 + h:5 + h], None, ALU.mult)
                    elif kind == "g":
                        js = list(range(g0, min(g0 + 4, qi + 1)))
                        n = len(js)
                        c = gidx[u] % 4
                        kg, kgr = Kg[c]
                        krg, krgr = Krg[c]
                        vg_, vgr = Vg[c]
                        pt, ptr = PTs[gidx[u] % 2]
                        STp, STp_r = STps[gidx[u] % 2]
                        for jj, j in enumerate(js):
                            kb.op("pe", [kgr, qnr], [STp_r], "matmul", STp[:, jj, :], kg[:, jj * 128:(jj + 1) * 128], qn[:], start=True, stop=False)
                            kb.op("pe", [krgr, qrr], [STp_r], "matmul", STp[:, jj, :], krg[:, jj * 128:(jj + 1) * 128], qr[:], start=False, stop=(j != qi))
                            if j == qi:
                                kb.op("pe", [idb_r, cnb_r], [STp_r], "matmul", STp[:, jj, :], idb[:], cnb[:], start=False, stop=True)
                        A([STp_r], [ptr], pt[:, 0:n, :], STp[:, 0:n, :], AF.Exp)
                        for jj, j in enumerate(js):
                            kb.op("pe", [ptr, vgr], [Op_r], "matmul", Op[:], pt[:, jj, :], vg_[:, jj, :], start=(j == 0), stop=(j == qi))
                    else:
                        V([Op_r], [rc_r], "reciprocal", rc[:, 0:1], Op[:, 128:129])
                        A([Op_r, rc_r], [onb_r], onb[:], Op[:, 0:128], AF.Copy, scale=rc[:, 0:1])
                        kb.op("pe", [onb_r, idb_r], [pTd_r], "transpose", pTd[:, 0, :], onb[:], idb[:])
                        A([pTd_r], [omr], o_[:, 4 + h, :], pTd[:, 0, :], AF.Copy)

                LOOK = 2
                for k, u in enumerate(units):
                    def f_u(k=k, u=u):
                        if k == 0:
                            for kk in range(min(LOOK, len(units))):
                                u_load(units[kk])
                        if k + LOOK < len(units):
                            u_load(units[k + LOOK])
                        u_comp(u)
                    add(f_u)

                def f_load():
                    kb.load("sp", o_[:, 0:4, :], omixT_d.ap()[0:512, t0:t0 + 128].rearrange("(kt p) t -> p kt t", p=128), omr, reads=[omix_r])
                    kb.load("sp", x_[:], x_d.ap()[t0:t0 + 128, :], xr)
                add(f_load)

                def f_mix():
                    for half in range(2):
                        for kt in range(8):
                            kb.op("pe", [omr, wout_r], [pX_r], "matmul", pX[:], o_[:, kt, :], wout[:, kt, half * 512:(half + 1) * 512], start=(kt == 0), stop=(kt == 7))
                        V([xr, pX_r], [x1_r], "tensor_tensor", x1[:, half * 512:(half + 1) * 512], x_[:, half * 512:(half + 1) * 512], pX[:], ALU.add)
                add(f_mix)

                def f_norm():
                    A([x1_r], [xr, stD_r], x_[:], x1[:], AF.Square, accum_out=stD[:, 0:1])
                    A([stD_r], [stD_r], stD[:, 1:2], stD[:, 0:1], AF.Sqrt, bias=EPS, scale=1.0 / D)
                    V([stD_r], [stD_r], "reciprocal", stD[:, 2:3], stD[:, 1:2])
                    V([x1_r, stD_r, A2b_r], [xr], "scalar_tensor_tensor", x_[:], x1[:], stD[:, 2:3], A2b[:], ALU.mult, ALU.mult)
                add(f_norm)

                def f_h2():
                    V([xr, B2b_r], [h2_r], "tensor_tensor", h2[:], x_[:], B2b[:], ALU.add)
                    for kt in range(8):
                        kb.op("pe", [h2_r, idb_r], [pTd_r], "transpose", pTd[:, kt, :], h2[:, kt * 128:(kt + 1) * 128], idb[:])
                    A([pTd_r], [h2T_r], h2T[:], pTd[:], AF.Copy)
                add(f_h2)
                for g4 in range(4):
                    def f_q(g4=g4):
                        for j in range(4):
                            hp = g4 * 4 + j
                            for kt in range(8):
                                kb.op("pe", [h2T_r, wpq_r], [pQ_r], "matmul", pQ[:, j, :], wpq[:, kt, hp * 128:(hp + 1) * 128], h2T[:, kt, :], start=(kt == 0), stop=(kt == 7))
                        A([pQ_r], [qTb_r], qTb[:, g4 * 4:(g4 + 1) * 4, :], pQ[:], AF.Copy)
                    add(f_q)
                for g4 in range(4):
                    def f_s(g4=g4):
                        for j in range(4):
                            hp = g4 * 4 + j
                            kb.op("pe", [qTb_r, skT_r], [pS_r], "matmul", pS[:, j, :], qTb[:, hp, :], skT[:, hp, :], start=True, stop=True)
                        A([pS_r], [sc_r], sc[:, g4 * 4:(g4 + 1) * 4, :], pS[:], AF.Copy)
                    add(f_s)

                split = len(steps)

                def top16(src, n, vout, vres, iu, iures):
                    V([sc_r], [vres], "max", vout[:, 0:8], src)
                    V([sc_r, vres], [iures], "max_index", iu[:, 0:8], vout[:, 0:8], src)
                    V([sc_r, vres], [wk_r], "match_replace", wk[:, 0:n], vout[:, 0:8], src, -1e30)
                    V([wk_r], [vres], "max", vout[:, 8:16], wk[:, 0:n])
                    V([wk_r, vres], [iures], "max_index", iu[:, 8:16], vout[:, 8:16], wk[:, 0:n])
                for hp in range(16):
                    add(lambda hp=hp: top16(sc[:, hp, :], 128, stp[:, hp, :], stp_r, iu1[:, hp, :], iu1_r))
                add(lambda: V([iu1_r], [itp_r], "tensor_copy", itp[:], iu1[:]))
                st4 = stp[:].rearrange("p (h t) k -> p h t k", t=2)
                it4 = itp[:].rearrange("p (h t) k -> p h t k", t=2)
                add(lambda: V([stp_r], [cand_r], "tensor_tensor", cand[:].rearrange("p h (a b) -> p h a b", a=16), st4[:, :, 0, :].unsqueeze(3).to_broadcast([128, 8, 16, 16]), st4[:, :, 1, :].unsqueeze(2).to_broadcast([128, 8, 16, 16]), ALU.add))
                for h in range(8):
                    add(lambda h=h: top16(cand[:, h, :], 256, best[:, h, :], best_r, iu2[:, h, :], iu2_r))
                add(lambda: V([iu2_r], [posf_r], "tensor_copy", posf[:], iu2[:]))

                def f_idx():
                    V([posf_r], [ab_r], "tensor_scalar", ab[:, 2], posf[:], 1.0 / 16, -0.46875, ALU.mult, ALU.add)
                    V([ab_r], [ab_r], "tensor_scalar", ab[:, 3], ab[:, 2], 12582912.0, None, ALU.add)
                    V([ab_r], [ab_r], "tensor_scalar", ab[:, 0], ab[:, 3], -12582912.0, None, ALU.add)
                    V([ab_r, posf_r], [ab_r], "scalar_tensor_tensor", ab[:, 1], ab[:, 0], -16.0, posf[:], ALU.mult, ALU.add)
                add(f_idx)
                for t in range(2):
                    def f_sel(t=t):
                        V([ab_r, cst_r], [eq_r], "tensor_tensor", eq[:], ab[:, t].unsqueeze(3).to_broadcast([128, 8, 16, 16]), iota16.unsqueeze(1).unsqueeze(1).to_broadcast([128, 8, 16, 16]), ALU.is_equal)
                        V([eq_r, itp_r], [eq_r], "tensor_tensor", eq[:], eq[:], it4[:, :, t, :].unsqueeze(2).to_broadcast([128, 8, 16, 16]), ALU.mult)
                        V([eq_r], [isel_r], "tensor_reduce", isel[:, t], eq[:], AX.X, ALU.add)
                    add(f_sel)

                def f_gate():
                    V([isel_r], [eidf_r], "scalar_tensor_tensor", eidf[:].rearrange("p (h k) -> p h k", h=8), isel[:, 0], 128.0, isel[:, 1], ALU.mult, ALU.add)
                    V([eidf_r], [eid_r], "tensor_copy", eid[:], eidf[:])
                    V([best_r], [gate_r], "tensor_tensor", gate[:], best[:], best[:, :, 0:1].to_broadcast([128, 8, 16]), ALU.subtract)
                    A([gate_r], [gate_r], gate[:], gate[:], AF.Exp)
                    V([gate_r], [gs_r], "tensor_reduce", gs[:, 0:8], gate[:], AX.X, ALU.add)
                    V([gs_r], [gs_r], "reciprocal", gs[:, 8:16], gs[:, 0:8])
                    V([gate_r, gs_r], [gate_r], "tensor_tensor", gate[:], gate[:], gs[:, 8:16].unsqueeze(2).to_broadcast([128, 8, 16]), ALU.mult)
                add(f_gate)
                return steps[:split], steps[split:]

            def back(ti, fsteps, deferred=None):
                t0 = ti * 128
                b = ti % 2
                x1, x1_r = x1s[ti % 3]
                h2, h2_r = h2s[ti % 3]
                eid, eid_r = eids[b]
                gate, gate_r = gates[b]
                gflat = gate[:].rearrange("p h k -> p (h k)")
                V([], [dots_r], "memset", dots[:], 0.0)
                nf = len(fsteps)
                fi = 0

                def bufof(g):
                    return uvg[(ti * NG + g) % NBUF]

                def st_gather(g):
                    buf, bufr = bufof(g)
                    for j in range(GS):
                        hk = g * GS + j
                        kb.dma("pool", [eid_r, uvb_r], [bufr[j]], bufr[j], False, "indirect_dma_start", out=buf[:, j, :], out_offset=None, in_=uvb_d.ap(), in_offset=bass.IndirectOffsetOnAxis(ap=eid[:, hk:hk + 1], axis=0))

                def st_dots(g):
                    buf, bufr = bufof(g)
                    ge, ge_r = ges[g % 3]
                    for j in range(GS):
                        hk = g * GS + j
                        V([bufr[j], h2_r], [junkv_r, dots_r], "scalar_tensor_tensor", junkv[:], buf[:, j, 0:1024], 1.0, h2[:], ALU.mult, ALU.mult, accum_out=dots[:, hk:hk + 1])

                def st_pre(g):
                    ge, ge_r = ges[g % 3]
                    dsl = dots[:, g * GS:(g + 1) * GS]
                    V([dots_r], [ge_r], "tensor_tensor", ge[:, 0], dsl, dsl, ALU.mult)
                    V([ge_r], [ge_r], "tensor_scalar", ge[:, 0], ge[:, 0], 0.044715, 1.0, ALU.mult, ALU.add)
                    V([ge_r, dots_r], [ge_r], "tensor_tensor", ge[:, 1], ge[:, 0], dsl, ALU.mult)
                    A([ge_r], [ge_r], ge[:, 2], ge[:, 1], AF.Tanh, scale=0.7978845608028654)
                    hg, hg_r = hgs[g % 3]
                    V([dots_r, gate_r], [hg_r], "scalar_tensor_tensor", hg[:], dsl, 0.5, gflat[:, g * GS:(g + 1) * GS], ALU.mult, ALU.mult)

                def st_fin(g):
                    buf, bufr = bufof(g)
                    ge, ge_r = ges[g % 3]
                    dsl = dots[:, g * GS:(g + 1) * GS]
                    hg, hg_r = hgs[g % 3]
                    V([ge_r, hg_r], [coef_r], "scalar_tensor_tensor", coef[:, g * GS:(g + 1) * GS], ge[:, 2], 1.0, hg[:], ALU.add, ALU.mult)
                    for j in range(GS):
                        hk = g * GS + j
                        d_, dr = dg[hk % 4]
                        A([idb_r, coef_r], [dr], d_[:], idb[:], AF.Copy, scale=coef[:, hk:hk + 1])
                        for half in range(2):
                            kb.op("pe", [dr, bufr[j]], [pM_r], "matmul", pM[:, half, :], d_[:], buf[:, j, 1024 + half * 512:1024 + (half + 1) * 512], start=(hk == 0), stop=(hk == 127))

                for s_ in range(NG + 2):
                    if s_ < NG:
                        st_gather(s_)
                    if 0 <= s_ - 1 < NG:
                        st_dots(s_ - 1)
                        st_pre(s_ - 1)
                    if s_ == 2 and deferred is not None:
                        deferred()
                    if 0 <= s_ - 2 < NG:
                        st_fin(s_ - 2)
                    tgt = min(nf, ((s_ + 1) * nf) // NG)
                    while fi < tgt:
                        fsteps[fi]()
                        fi += 1
                while fi < nf:
                    fsteps[fi]()
                    fi += 1
                def fin_tile():
                    V([x1_r, pM_r], [x1_r], "tensor_tensor", x1[:].rearrange("p (a b) -> p a b", a=2), x1[:].rearrange("p (a b) -> p a b", a=2), pM[:], ALU.add)
                    A([x1_r], [junkb_r, stE_r], junkb[:], x1[:], AF.Square, accum_out=stE[:, 4:5])
                    A([stE_r], [stE_r], stE[:, 5:6], stE[:, 4:5], AF.Sqrt, bias=EPS, scale=1.0 / D)
                    V([stE_r], [stE_r], "reciprocal", stE[:, 6:7], stE[:, 5:6])
                    ot, otr = ob[b]
                    V([x1_r, stE_r, FNG_r], [otr], "scalar_tensor_tensor", ot[:], x1[:], stE[:, 6:7], FNG[:], ALU.mult, ALU.mult)
                    kb.store("sp", out_d.ap()[t0:t0 + 128, :], ot[:], otr)
                return fin_tile

            def merge(A_, B_):
                out, ia, ib = [], 0, 0
                na, nb = len(A_), len(B_)
                for k in range(na + nb):
                    if ib >= nb or (ia < na and (ia + 1) * (nb + 1) <= (ib + 1) * (na + 1)):
                        out.append(A_[ia]); ia += 1
                    else:
                        out.append(B_[ib]); ib += 1
                return out

            FR = {}

            def fr(ti):
                if ti >= NT:
                    return [], []
                if ti not in FR:
                    FR[ti] = front(ti)
                return FR[ti]
            for f in fr(0)[0] + fr(0)[1] + fr(1)[0]:
                f()
            pend = None
            for ti in range(NT):
                pend = back(ti, merge(fr(ti + 1)[1], fr(ti + 2)[0]), pend)
                FR.pop(ti, None)
            pend()
        kb.barrier()
        kb.finish()
    return nc, kb


def _host_inputs(inp, b, S):
    f = lambda a: np.ascontiguousarray(np.asarray(a, np.float32))
    fm = lambda v: f(np.asarray(v).reshape(-1, 128).T)
    vecs = np.concatenate([fm(inp["c"][b]), fm(inp["ln_mix_g"][0]), fm(inp["ln_ffn_g"][0]), fm(inp["q_norm_g"][0]), fm(inp["kv_norm_g"][0]),
                           f(np.asarray(inp["conv_w"][0]).T.reshape(12, 128, 4).transpose(1, 0, 2).reshape(128, 48))], axis=1)
    rowv = np.concatenate([f(inp["a_log"][0]), f(inp["dt_bias"][0]), f(inp["gdn_norm_g"][0])])
    skT = f(np.asarray(inp["sub_keys"][0]).transpose(3, 1, 0, 2).reshape(128, 2048))
    return {"x": f(inp["x"][b, :S]), "pos": np.ascontiguousarray(np.asarray(inp["positions"][b, :S], np.int32)), "consts": make_consts(),
            "vecs": f(vecs), "rowv": f(rowv), "b_ada": f(inp["b_ada"][0]).reshape(1, 6144), "fng": f(inp["final_norm_g"]),
            "lnf": f(inp["ln_ffn_g"][0]),
            "w_in": f(inp["w_in"][0]), "w_uq": f(inp["w_uq"][0]), "w_ukv": f(inp["w_ukv"][0]), "w_out": f(inp["w_out"][0]),
            "w_pq": f(inp["w_pq"][0]), "skT": skT, "expert_u": f(inp["expert_u"][0]), "expert_v": f(inp["expert_v"][0]), "w_ada": f(inp["w_ada"][0])}


def kernel(**inputs):
    x = np.asarray(inputs["x"])
    B, S, _ = x.shape
    if S not in _CACHE:
        _CACHE[S] = build(S, dbg=False)[0]
    nc = _CACHE[S]
    in_maps = [_host_inputs(inputs, b, S) for b in range(B)]
    res = run_bass_kernel_spmd(nc, in_maps, core_ids=list(range(B)))
    return np.stack([np.asarray(r["out"], np.float32) for r in res.results], axis=0)
```

```python
import math
import numpy as np
from contextlib import ExitStack
import concourse.bass as bass
import concourse.mybir as mybir
from concourse.bass_utils import run_bass_kernel_spmd

F32 = mybir.dt.float32
BF16 = mybir.dt.bfloat16
I32 = mybir.dt.int32
U32 = mybir.dt.uint32
AF = mybir.ActivationFunctionType
ALU = mybir.AluOpType
AX = mybir.AxisListType

SEM_CH = 30000
EPS = 1e-6
D = 1024
IN_DIM = 2760
NEG = -30000.0


class Res:
    __slots__ = ("name", "w", "r", "acc", "n_in", "n_out", "excl")

    def __init__(self, name, acc=False, excl=False):
        self.name = name
        self.excl = excl
        self.w = {}
        self.r = {}
        self.acc = acc
        self.n_in = 0
        self.n_out = 0


class KB:
    ENGS = ("pe", "act", "dve", "pool", "sp")

    def __init__(self, nc, es):
        self.nc = nc
        self.es = es
        self.lists = {e: [] for e in self.ENGS}
        self.count = {e: 0 for e in self.ENGS}
        self.seen = {e: {} for e in self.ENGS}
        self.sems = {}
        self.dma_keys = {}
        self.nsem = 0
        self.ninst = 0

    def sb(self, name, shape, dt, es=None):
        t = (es or self.es).enter_context(self.nc.sbuf_tensor("sb_" + name, list(shape), dt))
        return t, Res(name)

    def ps(self, name, shape, dt, es=None):
        t = (es or self.es).enter_context(self.nc.psum_tensor("ps_" + name, list(shape), dt))
        return t, Res(name, excl=True)

    def _sem(self, key):
        s = self.sems.get(key)
        if s is None:
            self.nsem += 1
            s = self.es.enter_context(self.nc.semaphore("s%d" % self.nsem))
            self.sems[key] = s
        return s

    def _engsem(self, eng, g):
        ch = (g - 1) // SEM_CH
        return self._sem(("eng", eng, ch)), (g - 1) % SEM_CH + 1

    def _deps(self, eng, reads, writes):
        deps = {}
        for r in reads:
            for k, v in r.w.items():
                if deps.get(k, 0) < v:
                    deps[k] = v
        for w in writes:
            if w.acc:
                continue
            for d in (w.w, w.r):
                for k, v in d.items():
                    if deps.get(k, 0) < v:
                        deps[k] = v
        waits = []
        seen = self.seen[eng]
        for k, v in deps.items():
            if eng == "pe" and k == ("e", "pe"):
                continue
            if seen.get(k, 0) >= v:
                continue
            seen[k] = v
            waits.append((k, v))
        return waits

    def _emit_waits(self, eng, waits):
        L = self.lists[eng]
        for k, v in waits:
            if k[0] == "e":
                L.append(("we", k[1], v))
            else:
                L.append(("w", self._sem(k), v))
            self.ninst += 1

    def _mark(self, key, val, reads, writes):
        for r in reads:
            r.r[key] = val
        for w in writes:
            if w.acc:
                w.w[key] = val
            else:
                w.w = {key: val}
                w.r = {}

    def op(self, eng, reads, writes, meth, *args, **kw):
        if any(r.excl for r in reads):
            writes = list(writes) + [r for r in reads if r.excl]
            reads = [r for r in reads if not r.excl]
        waits = self._deps(eng, reads, writes)
        self._emit_waits(eng, waits)
        self.count[eng] += 1
        g = self.count[eng]
        self.lists[eng].append(("ie", meth, args, kw, eng, g))
        self.ninst += 1
        self._mark(("e", eng), g, reads, writes)

    def dma(self, eng, reads, writes, slot, store, meth, *args, **kw):
        waits = self._deps(eng, reads, writes)
        self._emit_waits(eng, waits)
        if store:
            key = ("do", id(slot))
            slot.n_out += 16
            val = slot.n_out
        else:
            key = ("di", id(slot))
            slot.n_in += 16
            val = slot.n_in
        sem = self._sem(key)
        self.dma_keys[key] = val
        self.lists[eng].append(("i", meth, args, kw, sem, 16))
        self.ninst += 1
        self._mark(key, val, reads, writes)

    def load(self, eng, dst_ap, src_ap, slot, reads=()):
        self.dma(eng, list(reads), [slot], slot, False, "dma_start", out=dst_ap, in_=src_ap)

    def store(self, eng, dst_ap, src_ap, slot, dres=None):
        self.dma(eng, [slot], [dres] if dres is not None else [], slot, True, "dma_start", out=dst_ap, in_=src_ap)

    def barrier(self):
        toks = {}
        for e in self.ENGS:
            if self.count[e] > 0:
                toks[("e", e)] = self.count[e]
        toks.update(self.dma_keys)
        for e in self.ENGS:
            waits = []
            for k, v in toks.items():
                if k == ("e", e) and e == "pe":
                    continue
                if self.seen[e].get(k, 0) >= v:
                    continue
                self.seen[e][k] = v
                waits.append((k, v))
            self._emit_waits(e, waits)

    def finish(self):
        nc = self.nc
        lists = self.lists

        waited = {e: set() for e in self.ENGS}
        for L in lists.values():
            for it in L:
                if it[0] == "we":
                    waited[it[1]].add(it[2])
        rank = {e: {g: i + 1 for i, g in enumerate(sorted(waited[e]))} for e in self.ENGS}

        def replay(engine, L):
            for it in L:
                if it[0] == "w":
                    engine.wait_ge(it[1], it[2])
                elif it[0] == "we":
                    sem, lv = self._engsem(it[1], rank[it[1]][it[2]])
                    engine.wait_ge(sem, lv)
                elif it[0] == "ie":
                    ins = getattr(engine, it[1])(*it[2], **it[3])
                    r = rank[it[4]].get(it[5])
                    if r is not None:
                        sem, lv = self._engsem(it[4], r)
                        ins.then_inc(sem, 1)
                else:
                    ins = getattr(engine, it[1])(*it[2], **it[3])
                    ins.then_inc(it[4], it[5])

        with nc.Block() as block:
            @block.tensor
            def _(e):
                replay(e, lists["pe"])

            @block.scalar
            def _(e):
                replay(e, lists["act"])

            @block.vector
            def _(e):
                replay(e, lists["dve"])

            @block.gpsimd
            def _(e):
                replay(e, lists["pool"])

            @block.sync
            def _(e):
                replay(e, lists["sp"])


C_ID, C_U, C_SL0, C_SL1, C_LS, C_PM, C_NA, C_CN, C_ONE = [i * 128 for i in range(9)]
C_IND0 = 9 * 128
C_IND1 = C_IND0 + 1
C_INVF = C_IND0 + 2
C_IOTA = C_IND0 + 3
NCONST = C_IOTA + 16


def make_consts():
    c = np.zeros((128, NCONST), np.float32)
    i = np.arange(128)
    ch = i // 64
    c[:, C_ID:C_ID + 128] = np.eye(128)
    c[:, C_U:C_U + 128] = ((ch[:, None] == ch[None, :]) & (i[:, None] <= i[None, :]))
    c[63, C_SL0:C_SL0 + 128] = 1.0
    c[127, C_SL1:C_SL1 + 128] = 1.0
    c[:, C_LS:C_LS + 128] = (i[:, None] == (ch[None, :] * 64 + 63))
    same = ch[:, None] == ch[None, :]
    c[:, C_PM:C_PM + 128] = np.where(same & (i[None, :] < i[:, None]), 0.0, -NEG)
    c[:, C_NA:C_NA + 128] = np.where(same & (i[:, None] <= i[None, :]), 0.0, NEG)
    c[:, C_CN:C_CN + 128] = np.where(i[:, None] <= i[None, :], 0.0, NEG)
    c[:, C_ONE:C_ONE + 128] = 1.0
    c[:, C_IND0] = (i < 64)
    c[:, C_IND1] = (i >= 64)
    half = 32
    c[:, C_INVF] = (10000.0 ** (-(np.arange(128) % half).astype(np.float64) / half)).astype(np.float32)
    c[:, C_IOTA:C_IOTA + 16] = np.arange(16)[None, :]
    return c


V_C, V_LNM, V_LNF, V_QG, V_KVG, V_CW = 0, 8, 16, 24, 27, 29
NVEC = V_CW + 48
R_ALOG, R_DTB, R_GG = 0, 4, 8
NROW = 136

_CACHE = {}
GDN_NST = 99
GDN_GATES = True


def build(S, dbg=False):
    NT = S // 128
    nc = bass.Bass("TRN2", target_bir_lowering=False)
    okind = "ExternalOutput" if dbg else "Internal"

    def din(name, shape, dt=F32):
        return nc.dram_tensor(name, list(shape), dt, kind="ExternalInput")

    def dsc(name, shape, dt):
        return nc.dram_tensor(name, list(shape), dt, kind=okind)

    x_d = din("x", [S, D]); pos_d = din("pos", [S], I32)
    consts_d = din("consts", [128, NCONST]); vecs_d = din("vecs", [128, NVEC]); rowv_d = din("rowv", [NROW])
    bada_d = din("b_ada", [1, 6144]); fng_d = din("fng", [D]); lnf_d = din("lnf", [D])
    win_d = din("w_in", [D, IN_DIM]); wuq_d = din("w_uq", [384, 768]); wukv_d = din("w_ukv", [256, 1024])
    wout_d = din("w_out", [D, D]); wpq_d = din("w_pq", [D, 2048]); skT_d = din("skT", [128, 2048])
    eu_d = din("expert_u", [16384, D]); ev_d = din("expert_v", [16384, D]); wada_d = din("w_ada", [D, 6144])
    out_d = nc.dram_tensor("out", [S, D], F32, kind="ExternalOutput")
    mod_d = dsc("mod_s", [6144], F32)
    qkvz_d = dsc("qkvz_s", [S, 2048], BF16)
    gab_d = dsc("gab_s", [S, 8], F32)
    qnT_d = dsc("qnT_s", [4, 128, S], BF16)
    qrT_d = dsc("qrT_s", [4, 65, S], BF16)
    knT_d = dsc("knT_s", [4, 128, S], BF16)
    krT_d = dsc("krT_s", [65, S], BF16)
    v_d = dsc("v_s", [4, S, 129], BF16)
    omixT_d = dsc("omixT_s", [D, S], BF16)
    uvb_d = nc.dram_tensor("uvb_s", [16384, 2 * D], BF16)

    with ExitStack() as es:
        kb = KB(nc, es)
        R = lambda n: Res(n, acc=True)
        mod_r, qkvz_r, gab_r, qnT_r, qrT_r, knT_r, krT_r, v_r, omix_r, uvb_r = [R(n) for n in
            ("mod", "qkvz", "gab", "qnT", "qrT", "knT", "krT", "v", "omix", "uvb")]
        cst, cst_r = kb.sb("cst", [128, NCONST], F32)
        vecs, vecs_r = kb.sb("vecs", [128, NVEC], F32)
        rowv, rowv_r = kb.sb("rowv", [128, NROW], F32)
        modT, modT_r = kb.sb("modT", [128, 48], F32)
        AB1, AB1_r = kb.sb("AB1", [128, 32], F32)
        idb, idb_r = kb.sb("idb", [128, 128], BF16)
        oneb, oneb_r = kb.sb("oneb", [128, 128], BF16)
        kmax, kmax_r = kb.sb("kmax", [128, 8], F32)
        kb.load("sp", cst[:], consts_d.ap(), cst_r)
        kb.load("sp", vecs[:], vecs_d.ap(), vecs_r)
        kb.load("sp", rowv[:], rowv_d.ap().partition_broadcast(128), rowv_r)
        kb.op("dve", [cst_r], [idb_r], "tensor_copy", idb[:], cst[:, C_ID:C_ID + 128])
        kb.op("dve", [cst_r], [oneb_r], "tensor_copy", oneb[:], cst[:, C_ONE:C_ONE + 128])
        kb.op("dve", [], [kmax_r], "memset", kmax[:], 0.0)
        ident = cst[:, C_ID:C_ID + 128]
        ones = cst[:, C_ONE:C_ONE + 128]

        with ExitStack() as p0:
            cact, cact_r = kb.sb("cact", [128, 8], F32, p0)
            wst = [kb.sb("wst%d" % i, [128, 6144], F32, p0) for i in range(2)]
            mrow, mrow_r = kb.sb("mrow", [1, 6144], F32, p0)
            brow, brow_r = kb.sb("brow", [1, 6144], F32, p0)
            pm = [kb.ps("pm%d" % i, [1, 512], F32, p0) for i in range(4)]
            kb.op("act", [vecs_r], [cact_r], "activation", cact[:], vecs[:, V_C:V_C + 8], AF.Silu)
            kb.load("sp", brow[:], bada_d.ap(), brow_r)
            for half in range(3):
                for kt in range(8):
                    w, wr = wst[kt % 2]
                    kb.load("sp" if kt % 2 == 0 else "pool", w[:, 0:2048], wada_d.ap()[kt * 128:(kt + 1) * 128, half * 2048:(half + 1) * 2048], wr)
                    for j in range(4):
                        kb.op("pe", [cact_r, wr], [pm[j][1]], "matmul", pm[j][0][:], cact[:, kt:kt + 1], w[:, j * 512:(j + 1) * 512], start=(kt == 0), stop=(kt == 7))
                for j in range(4):
                    c0 = half * 2048 + j * 512
                    kb.op("dve", [pm[j][1], brow_r], [mrow_r], "tensor_tensor", mrow[:, c0:c0 + 512], pm[j][0][:], brow[:, c0:c0 + 512], ALU.add)
            kb.store("sp", mod_d.ap().rearrange("(o n) -> o n", o=1), mrow[:], mrow_r, mod_r)
            kb.barrier()
            kb.dma("sp", [mod_r], [modT_r], modT_r, False, "dma_start", out=modT[:], in_=mod_d.ap().rearrange("(j p) -> p j", p=128), allow_slow_non_contiguous=True)
            kb.op("dve", [modT_r, vecs_r], [AB1_r], "scalar_tensor_tensor", AB1[:, 0:8], modT[:, 8:16], 1.0, vecs[:, V_LNM:V_LNM + 8], ALU.add, ALU.mult)
            kb.op("dve", [modT_r], [AB1_r], "tensor_copy", AB1[:, 8:16], modT[:, 0:8])
            kb.op("dve", [modT_r, vecs_r], [AB1_r], "scalar_tensor_tensor", AB1[:, 16:24], modT[:, 32:40], 1.0, vecs[:, V_LNF:V_LNF + 8], ALU.add, ALU.mult)
            kb.op("dve", [modT_r], [AB1_r], "tensor_copy", AB1[:, 24:32], modT[:, 24:32])
            kb.barrier()

        with ExitStack() as pa:
            win, win_r = kb.sb("win", [128, 8, 2824], BF16, pa)
            wuq, wuq_r = kb.sb("wuq", [128, 3, 1024], BF16, pa)
            wukv, wukv_r = kb.sb("wukv", [128, 2, 1024], BF16, pa)
            stg = [kb.sb("stg%d" % i, [128, 2760], F32, pa) for i in range(2)]
            for kt in range(8):
                s_, sr = stg[kt % 2]
                kb.load("sp", s_[:], win_d.ap()[kt * 128:(kt + 1) * 128, :], sr)
                kb.op("act" if kt % 2 else "dve", [sr], [win_r], "activation" if kt % 2 else "tensor_copy", *((win[:, kt, 0:2760], s_[:], AF.Copy) if kt % 2 else (win[:, kt, 0:2760], s_[:])))
                kb.op("dve", [sr], [win_r], "tensor_scalar", win[:, kt, 2760:2792], s_[:, 2728:2760], -1.0, None, ALU.mult)
                kb.op("dve", [sr], [win_r], "tensor_copy", win[:, kt, 2792:2824], s_[:, 2696:2728])
            for kt in range(3):
                s_, sr = stg[kt % 2]
                kb.load("sp", s_[:, 0:768], wuq_d.ap()[kt * 128:(kt + 1) * 128, :], sr)
                kb.op("dve", [sr], [wuq_r], "tensor_copy", wuq[:, kt, 0:768], s_[:, 0:768])
                for h in range(4):
                    b0 = h * 192 + 128
                    kb.op("dve", [sr], [wuq_r], "tensor_scalar", wuq[:, kt, 768 + h * 64:768 + h * 64 + 32], s_[:, b0 + 32:b0 + 64], -1.0, None, ALU.mult)
                    kb.op("dve", [sr], [wuq_r], "tensor_copy", wuq[:, kt, 768 + h * 64 + 32:768 + h * 64 + 64], s_[:, b0:b0 + 32])
            for kt in range(2):
                s_, sr = stg[(kt + 1) % 2]
                kb.load("sp", s_[:, 0:1024], wukv_d.ap()[kt * 128:(kt + 1) * 128, :], sr)
                kb.op("dve", [sr], [wukv_r], "tensor_copy", wukv[:, kt, :], s_[:, 0:1024])

            xt = [kb.sb("xt%d" % i, [128, D], F32, pa) for i in range(2)]
            junk, junk_r = kb.sb("junkA", [128, D], F32, pa)
            st1, st1_r = kb.sb("st1", [128, 16], F32, pa)
            xn, xn_r = kb.sb("xnA", [128, D], BF16, pa)
            hT, hT_r = kb.sb("hTA", [128, 8, 128], BF16, pa)
            cin, cin_r = kb.sb("cin", [128, 12, 131], F32, pa)
            cv, cv_r = kb.sb("cv", [128, 12, 128], F32, pa)
            cs, cs_r = kb.sb("cs", [128, 12, 128], BF16, pa)
            stA = [kb.sb("stA%d" % i, [128, 2048], BF16, pa) for i in range(2)]
            gst = [kb.sb("gst%d" % i, [128, 8], F32, pa) for i in range(2)]
            cn, cn_r = kb.sb("cn", [128, 640], BF16, pa)
            cnT, cnT_r = kb.sb("cnT", [128, 5, 128], BF16, pa)
            qst = [kb.sb("qst%d" % i, [128, 4, 128], BF16, pa) for i in range(2)]
            kst = [kb.sb("kst%d" % i, [128, 4, 128], BF16, pa) for i in range(2)]
            rst = [kb.sb("rst%d" % i, [65, 4, 128], BF16, pa) for i in range(2)]
            krst = [kb.sb("krst%d" % i, [65, 128], BF16, pa) for i in range(2)]
            vst = [kb.sb("vst%d" % i, [128, 4, 129], BF16, pa) for i in range(2)]
            sq1, sq1_r = kb.sb("sq1", [128, 4, 128], BF16, pa)
            sq2, sq2_r = kb.sb("sq2", [64, 4, 128], BF16, pa)
            posi, posi_r = kb.sb("posi", [64, 128], I32, pa)
            tr, tr_r = kb.sb("trig", [64, 6, 128], F32, pa)
            t12, t12_r = kb.sb("t12", [64, 2, 4, 128], F32, pa)
            tmx, tmx_r = kb.sb("tmx", [128, 4], F32, pa)
            pT, pT_r = kb.ps("pTA", [128, 8, 128], BF16, pa)
            pF = [kb.ps("pFA%d" % i, [128, 4, 128], F32, pa) for i in range(2)]
            pK, pK_r = kb.ps("pKA", [64, 2, 128], F32, pa)
            pZ, pZ_r = kb.ps("pZA", [128, 512], F32, pa)
            pC2, pC2_r = kb.ps("pC2A", [128, 392], F32, pa)
            pC3, pC3_r = kb.ps("pC3A", [128, 256], F32, pa)
            pR, pR_r = kb.ps("pRA", [64, 4, 128], F32, pa)
            kb.op("pool", [], [cin_r], "memset", cin[:], 0.0)
            for i in range(2):
                kb.op("pool", [], [vst[i][1]], "memset", vst[i][0][:], 1.0)
                kb.op("pool", [], [krst[i][1]], "memset", krst[i][0][:], 1.0)
                kb.op("pool", [], [rst[i][1]], "memset", rst[i][0][:], 0.0)
            cwv = vecs[:, V_CW:V_CW + 48]
            QS = 192.0 ** -0.5
            for ti in range(NT):
                t0 = ti * 128
                b = ti % 2
                x_, xr = xt[b]
                if ti == 0:
                    kb.load("sp", x_[:], x_d.ap()[t0:t0 + 128, :], xr)
                if ti + 1 < NT:
                    kb.load("sp", xt[(ti + 1) % 2][0][:], x_d.ap()[t0 + 128:t0 + 256, :], xt[(ti + 1) % 2][1])
                kb.load("pool", posi[:], pos_d.ap()[t0:t0 + 128].partition_broadcast(64), posi_r)
                kb.op("act", [xr], [junk_r, st1_r], "activation", junk[:], x_[:], AF.Square, accum_out=st1[:, 0:1])
                kb.op("act", [st1_r], [st1_r], "activation", st1[:, 1:2], st1[:, 0:1], AF.Sqrt, bias=EPS, scale=1.0 / D)
                kb.op("dve", [st1_r], [st1_r], "reciprocal", st1[:, 2:3], st1[:, 1:2])
                kb.op("act", [xr, st1_r], [xn_r], "activation", xn[:], x_[:], AF.Copy, scale=st1[:, 2:3])
                for kt in range(8):
                    kb.op("pe", [xn_r, idb_r], [pT_r], "transpose", pT[:, kt, :], xn[:, kt * 128:(kt + 1) * 128], idb[:])
                kb.op("dve", [pT_r, AB1_r], [hT_r], "tensor_tensor", hT[:], pT[:], AB1[:, 0:8].unsqueeze(2).to_broadcast([128, 8, 128]), ALU.mult)
                kb.op("dve", [hT_r, AB1_r], [hT_r], "tensor_tensor", hT[:], hT[:], AB1[:, 8:16].unsqueeze(2).to_broadcast([128, 8, 128]), ALU.add)
                for grp in range(3):
                    pf, pfr = pF[grp % 2]
                    for j in range(4):
                        ct = grp * 4 + j
                        for kt in range(8):
                            kb.op("pe", [hT_r, win_r], [pfr], "matmul", pf[:, j, :], win[:, kt, ct * 128:(ct + 1) * 128], hT[:, kt, :], start=(kt == 0), stop=(kt == 7))
                    kb.op("act", [pfr], [cin_r], "activation", cin[:, grp * 4:(grp + 1) * 4, 3:131], pf[:], AF.Copy)
                for j in range(2):
                    for kt in range(8):
                        kb.op("pe", [hT_r, win_r], [pK_r], "matmul", pK[:, j, :], win[:, kt, 2696 + 64 * j + (0 if j == 0 else 0):2696 + 64 * j + 64], hT[:, kt, :], start=(kt == 0), stop=(kt == 7))
                for kt in range(8):
                    kb.op("pe", [hT_r, win_r], [pZ_r], "matmul", pZ[:], hT[:, kt, :], win[:, kt, 1536:2048], start=(kt == 0), stop=(kt == 7))
                for kt in range(8):
                    kb.op("pe", [hT_r, win_r], [pC2_r], "matmul", pC2[:], hT[:, kt, :], win[:, kt, 2048:2440], start=(kt == 0), stop=(kt == 7))
                for kt in range(8):
                    kb.op("pe", [hT_r, win_r], [pC3_r], "matmul", pC3[:], hT[:, kt, :], win[:, kt, 2440:2696], start=(kt == 0), stop=(kt == 7))
                for ct in range(12):
                    kb.op("dve", [cin_r, vecs_r], [cv_r], "tensor_scalar", cv[:, ct, :], cin[:, ct, 0:128], cwv[:, ct * 4:ct * 4 + 1], None, ALU.mult)
                    for i in range(1, 4):
                        kb.op("dve", [cin_r, vecs_r, cv_r], [cv_r], "scalar_tensor_tensor", cv[:, ct, :], cin[:, ct, i:i + 128], cwv[:, ct * 4 + i:ct * 4 + i + 1], cv[:, ct, :], ALU.mult, ALU.add)
                kb.op("pool", [cin_r], [cin_r], "tensor_copy", cin[:, :, 0:3], cin[:, :, 128:131])
                kb.op("act", [cv_r], [cs_r], "activation", cs[:], cv[:], AF.Silu)
                sa, sar = stA[b]
                for grp in range(2):
                    n = 8 if grp == 0 else 4
                    for j in range(n):
                        ct = grp * 8 + j
                        kb.op("pe", [cs_r, idb_r], [pT_r], "transpose", pT[:, j, :], cs[:, ct, :], idb[:])
                    kb.op("dve", [pT_r], [sar], "tensor_copy", sa[:, grp * 1024:grp * 1024 + n * 128], pT[:, 0:n, :])
                kb.op("act", [pZ_r], [sar], "activation", sa[:, 1536:2048], pZ[:], AF.Silu)
                kb.store("sp", qkvz_d.ap()[t0:t0 + 128, :], sa[:], sar, qkvz_r)
                g_, gr_ = gst[b]
                kb.op("dve", [pC2_r], [gr_], "tensor_copy", g_[:], pC2[:, 0:8])
                kb.store("sp", gab_d.ap()[t0:t0 + 128, :], g_[:], gr_, gab_r)
                kb.op("act", [pC2_r], [junk_r, st1_r], "activation", junk[:, 0:384], pC2[:, 8:392], AF.Square, accum_out=st1[:, 4:5])
                kb.op("act", [pC3_r], [junk_r, st1_r], "activation", junk[:, 384:640], pC3[:], AF.Square, accum_out=st1[:, 5:6])
                kb.op("act", [st1_r], [st1_r], "activation", st1[:, 6:7], st1[:, 4:5], AF.Sqrt, bias=EPS, scale=1.0 / 384)
                kb.op("act", [st1_r], [st1_r], "activation", st1[:, 7:8], st1[:, 5:6], AF.Sqrt, bias=EPS, scale=1.0 / 256)
                kb.op("dve", [st1_r], [st1_r], "reciprocal", st1[:, 8:10], st1[:, 6:8])
                kb.op("act", [pC2_r, st1_r], [cn_r], "activation", cn[:, 0:384], pC2[:, 8:392], AF.Copy, scale=st1[:, 8:9])
                kb.op("act", [pC3_r, st1_r], [cn_r], "activation", cn[:, 384:640], pC3[:], AF.Copy, scale=st1[:, 9:10])
                for j in range(5):
                    kb.op("pe", [cn_r, idb_r], [pT_r], "transpose", pT[:, j, :], cn[:, j * 128:(j + 1) * 128], idb[:])
                kb.op("dve", [pT_r, vecs_r], [cnT_r], "tensor_tensor", cnT[:], pT[:, 0:5, :], vecs[:, V_QG:V_QG + 5].unsqueeze(2).to_broadcast([128, 5, 128]), ALU.mult)
                kb.op("dve", [posi_r], [tr_r], "tensor_copy", tr[:, 0, :], posi[:])
                kb.op("dve", [tr_r, cst_r], [tr_r], "tensor_scalar", tr[:, 0, :], tr[:, 0, :], cst[0:64, C_INVF:C_INVF + 1], None, ALU.mult)
                for which, off in ((4, 0.0), (5, math.pi / 2)):
                    kb.op("dve", [tr_r], [tr_r], "tensor_scalar", tr[:, 1, :], tr[:, 0, :], off, 1.0 / (2 * math.pi), ALU.add, ALU.mult)
                    kb.op("dve", [tr_r], [tr_r], "tensor_scalar", tr[:, 2, :], tr[:, 1, :], 12582912.0, None, ALU.add)
                    kb.op("dve", [tr_r], [tr_r], "tensor_scalar", tr[:, 2, :], tr[:, 2, :], -12582912.0, None, ALU.add)
                    kb.op("dve", [tr_r], [tr_r], "tensor_tensor", tr[:, 3, :], tr[:, 1, :], tr[:, 2, :], ALU.subtract)
                    kb.op("dve", [tr_r], [tr_r], "tensor_scalar", tr[:, 3, :], tr[:, 3, :], -0.4999, 0.4999, ALU.max, ALU.min)
                    kb.op("act", [tr_r], [tr_r], "activation", tr[:, which, :], tr[:, 3, :], AF.Sin, scale=2 * math.pi)
                sinT = tr[:, 4, :]
                cosT = tr[:, 5, :]
                pq, pqr = pF[0]
                for h in range(4):
                    for kt in range(3):
                        kb.op("pe", [cnT_r, wuq_r], [pqr], "matmul", pq[:, h, :], wuq[:, kt, h * 192:h * 192 + 128], cnT[:, kt, :], start=(kt == 0), stop=(kt == 2))
                for h in range(4):
                    for kt in range(3):
                        kb.op("pe", [cnT_r, wuq_r], [pR_r], "matmul", pR[:, h, :], wuq[:, kt, h * 192 + 128:h * 192 + 192], cnT[:, kt, :], start=(kt == 0), stop=(kt == 2))
                    for kt in range(3):
                        kb.op("pe", [cnT_r, wuq_r], [pZ_r], "matmul", pZ[0:64, h * 128:(h + 1) * 128], wuq[:, kt, 768 + h * 64:768 + h * 64 + 64], cnT[:, kt, :], start=(kt == 0), stop=(kt == 2))
                q_, qr_ = qst[b]
                kb.op("act", [pqr], [qr_], "activation", q_[:], pq[:], AF.Copy, scale=QS)
                kb.op("act", [pqr], [sq1_r], "activation", sq1[:], pq[:], AF.Square, scale=QS)
                kb.store("pool", qnT_d.ap()[:, :, t0:t0 + 128].rearrange("h p t -> p h t"), q_[:], qr_, qnT_r)
                r_, rr_ = rst[b]
                kb.op("dve", [pR_r, tr_r], [t12_r], "tensor_tensor", t12[:, 0], pR[:], cosT.unsqueeze(1).to_broadcast([64, 4, 128]), ALU.mult)
                kb.op("dve", [pZ_r, tr_r], [t12_r], "tensor_tensor", t12[:, 1], pZ[0:64, :].rearrange("p (h c) -> p h c", h=4), sinT.unsqueeze(1).to_broadcast([64, 4, 128]), ALU.mult)
                kb.op("dve", [t12_r], [t12_r], "tensor_tensor", t12[:, 0], t12[:, 0], t12[:, 1], ALU.add)
                kb.op("act", [t12_r], [rr_], "activation", r_[0:64], t12[:, 0], AF.Copy, scale=QS)
                kb.op("act", [t12_r], [sq2_r], "activation", sq2[:], t12[:, 0], AF.Square, scale=QS)
                pn, pnr = pF[1]
                kb.op("pe", [sq1_r, oneb_r], [pnr], "matmul", pn[:].rearrange("p a b -> p (a b)"), oneb[:], sq1[:].rearrange("p a b -> p (a b)"), start=True, stop=False)
                kb.op("pe", [sq2_r, oneb_r], [pnr], "matmul", pn[:].rearrange("p a b -> p (a b)"), oneb[0:64, :], sq2[:].rearrange("p a b -> p (a b)"), start=False, stop=True)
                kb.op("act", [pnr], [rr_], "activation", r_[64:65], pn[64:65], AF.Sqrt, scale=1.0609)
                kb.store("pool", qrT_d.ap()[:, :, t0:t0 + 128].rearrange("h p t -> p h t"), r_[:], rr_, qrT_r)
                pk2, pk2r = pF[0]
                for h in range(4):
                    for kt in range(2):
                        kb.op("pe", [cnT_r, wukv_r], [pk2r], "matmul", pk2[:, h, :], wukv[:, kt, h * 256:h * 256 + 128], cnT[:, 3 + kt, :], start=(kt == 0), stop=(kt == 1))
                k_, kr_ = kst[b]
                kb.op("act", [pk2r], [kr_], "activation", k_[:], pk2[:], AF.Copy)
                kb.op("act", [pk2r], [sq1_r], "activation", sq1[:], pk2[:], AF.Square)
                kb.store("pool", knT_d.ap()[:, :, t0:t0 + 128].rearrange("h p t -> p h t"), k_[:], kr_, knT_r)
                kr2, kr2r = krst[b]
                kb.op("dve", [pK_r, tr_r], [t12_r], "tensor_tensor", t12[:, 0, 0], pK[:, 0, :], cosT, ALU.mult)
                kb.op("dve", [pK_r, tr_r], [t12_r], "tensor_tensor", t12[:, 1, 0], pK[:, 1, :], sinT, ALU.mult)
                kb.op("dve", [t12_r], [t12_r], "tensor_tensor", t12[:, 0, 0], t12[:, 0, 0], t12[:, 1, 0], ALU.add)
                kb.op("act", [t12_r], [kr2r], "activation", kr2[0:64], t12[:, 0, 0], AF.Copy)
                kb.op("act", [t12_r], [sq2_r], "activation", sq2[:, 0, :], t12[:, 0, 0], AF.Square)
                kb.store("pool", krT_d.ap()[:, t0:t0 + 128], kr2[:], kr2r, krT_r)
                pn2, pn2r = pF[1]
                kb.op("pe", [sq1_r, oneb_r], [pn2r], "matmul", pn2[:].rearrange("p a b -> p (a b)"), oneb[:], sq1[:].rearrange("p a b -> p (a b)"), start=True, stop=True)
                kb.op("pe", [sq2_r, oneb_r], [pC3_r], "matmul", pC3[:, 0:128], oneb[0:64, :], sq2[:, 0, :], start=True, stop=True)
                kb.op("dve", [pn2r], [tmx_r], "tensor_reduce", tmx[:], pn2[:], AX.X, ALU.max)
                kb.op("dve", [tmx_r, kmax_r], [kmax_r], "tensor_tensor", kmax[:, 0:4], kmax[:, 0:4], tmx[:], ALU.max)
                kb.op("dve", [pC3_r], [tmx_r], "tensor_reduce", tmx[:, 0:1], pC3[:, 0:128], AX.X, ALU.max)
                kb.op("dve", [tmx_r, kmax_r], [kmax_r], "tensor_tensor", kmax[:, 4:5], kmax[:, 4:5], tmx[:, 0:1], ALU.max)
                for kt in range(2):
                    kb.op("pe", [cnT_r, wukv_r], [pZ_r], "matmul", pZ[:].rearrange("p (h c) -> p h c", h=4), cnT[:, 3 + kt, :], wukv[:, kt, :].rearrange("p (h c) -> p h c", h=4)[:, :, 128:256], start=(kt == 0), stop=(kt == 1))
                v_, vr_ = vst[b]
                kb.op("dve", [pZ_r], [vr_], "tensor_copy", v_[:, :, 0:128], pZ[:].rearrange("p (h c) -> p h c", h=4))
                kb.store("pool", v_d.ap()[:, t0:t0 + 128, :].rearrange("h p c -> p h c"), v_[:], vr_, v_r)
            kb.barrier()

        with ExitStack() as pb:
            f32t = lambda n, shp=(128, 128): kb.sb(n, list(shp), F32, pb)
            bft = lambda n, shp=(128, 128): kb.sb(n, list(shp), BF16, pb)
            qz = [bft("qz%d" % i, (128, 2048)) for i in range(3)]
            gabt = [f32t("gabt%d" % i, (128, 8)) for i in range(3)]
            gts = [f32t("gt%d" % i, (128, 64)) for i in range(3)]
            nega, nega_r = f32t("nega", (128, 4))
            o_sb, o_r = f32t("o_sb", (128, 4, 128))
            osq, osq_r = f32t("osq", (128, 4, 128))
            ost, ost_r = f32t("ost", (128, 12))
            omg, omg_r = bft("omg", (128, 4, 128))
            oT = [bft("oT%d" % i, (128, 4, 128)) for i in range(2)]
            pTb = [kb.ps("pTbB%d" % i, [128, 8, 128], BF16, pb) for i in range(2)]
            pG, pG_r = kb.ps("pGB", [128, 512], F32, pb)
            pW = [kb.ps("pWB%d" % h, [128, 4, 128], F32, pb)[0] for h in range(4)]
            pWr = [[Res("pW%d" % h, excl=True)] * 4 for h in range(4)]
            HH = [[], []]
            for h in range(4):
                for par in range(2):
                    Td = {}
                    for n in ("stq",):
                        Td[n] = f32t("%s%d_%d" % (n, h, par), (128, 16))
                    for n in ("kbg", "vb", "dg", "decM", "decAT", "Ma", "Mb", "Na", "Nb", "Pa", "Pb", "MI", "u"):
                        Td[n] = f32t("%s%d_%d" % (n, h, par))
                    for n in ("junk", "kn", "qs", "kd0", "kd1", "dq", "kT", "qT", "qdT", "attnT", "wT"):
                        Td[n] = bft("%s%d_%d" % (n, h, par))
                    if par == 0:
                        Td["S"] = f32t("S%d" % h)
                        Td["vnew"] = bft("vnew%d" % h)
                        Td["Sb"] = bft("Sb%d" % h)
                        kb.op("pool", [], [Td["vnew"][1]], "memset", Td["vnew"][0][:], 0.0)
                        kb.op("pool", [], [Td["S"][1]], "memset", Td["S"][0][:], 0.0)
                    else:
                        for n in ("S", "vnew", "Sb"):
                            Td[n] = HH[0][h][n]
                    HH[par].append(Td)
            kb.op("act", [rowv_r], [nega_r], "activation", nega[:], rowv[:, R_ALOG:R_ALOG + 4], AF.Exp)
            kb.op("dve", [nega_r], [nega_r], "tensor_scalar", nega[:], nega[:], -1.0, None, ALU.mult)
            identf = cst[:, C_ID:C_ID + 128]
            def gdn_tile(ti):
                t0 = ti * 128
                b = ti % 2
                q_, qr_ = qz[ti % 3]
                ga_, gar_ = gabt[ti % 3]
                gt, gtr = gts[ti % 3]
                H = HH[b]
                V = lambda r, w, m, *a, **k: kb.op("dve", r, w, m, *a, **k)
                A = lambda r, w, *a, **k: kb.op("act", r, w, "activation", *a, **k)

                gsteps = []

                def g0():
                    kb.load("sp", q_[:], qkvz_d.ap()[t0:t0 + 128, :], qr_, reads=[qkvz_r])
                    kb.load("sp", ga_[:], gab_d.ap()[t0:t0 + 128, :], gar_, reads=[gab_r])
                    V([gar_, rowv_r], [gtr], "tensor_tensor", gt[:, 0:4], ga_[:, 0:4], rowv[:, R_DTB:R_DTB + 4], ALU.add)
                    V([gtr], [gtr], "tensor_scalar", gt[:, 4:8], gt[:, 0:4], -1.0, None, ALU.mult)
                    V([gtr], [gtr], "tensor_tensor", gt[:, 4:8], gt[:, 4:8], gt[:, 0:4], ALU.max)
                gsteps.append(g0)

                def g1():
                    A([gtr], [gtr], gt[:, 8:12], gt[:, 4:8], AF.Exp, scale=-1.0)
                gsteps.append(g1)

                def g2():
                    V([gtr], [gtr], "tensor_scalar", gt[:, 12:16], gt[:, 8:12], 2.0, None, ALU.add)
                    V([gtr], [gtr], "reciprocal", gt[:, 12:16], gt[:, 12:16])
                    V([gtr], [gtr], "tensor_tensor", gt[:, 12:16], gt[:, 12:16], gt[:, 8:12], ALU.mult)
                    V([gtr], [gtr], "tensor_tensor", gt[:, 52:56], gt[:, 12:16], gt[:, 12:16], ALU.mult)
                    V([gtr], [gtr], "tensor_scalar", gt[:, 56:60], gt[:, 52:56], 1.0 / 11, 1.0 / 9, ALU.mult, ALU.add)
                    for cf in (1.0 / 7, 1.0 / 5, 1.0 / 3, 1.0):
                        V([gtr], [gtr], "tensor_tensor", gt[:, 56:60], gt[:, 56:60], gt[:, 52:56], ALU.mult)
                        V([gtr], [gtr], "tensor_scalar", gt[:, 56:60], gt[:, 56:60], cf, None, ALU.add)
                    V([gtr], [gtr], "scalar_tensor_tensor", gt[:, 12:16], gt[:, 12:16], 2.0, gt[:, 56:60], ALU.mult, ALU.mult)
                    V([gtr], [gtr], "tensor_scalar", gt[:, 16:20], gt[:, 0:4], 0.0, None, ALU.max)
                    V([gtr], [gtr], "tensor_tensor", gt[:, 16:20], gt[:, 16:20], gt[:, 12:16], ALU.add)
                    V([gtr, nega_r], [gtr], "tensor_tensor", gt[:, 20:24], gt[:, 16:20], nega[:], ALU.mult)
                gsteps.append(g2)

                def g3():
                    A([gar_], [gtr], gt[:, 24:28], ga_[:, 4:8], AF.Sigmoid)
                gsteps.append(g3)

                def g4():
                    kb.op("pe", [gtr, cst_r], [pG_r], "matmul", pG[:, 0:4], cst[:, C_U:C_U + 128], gt[:, 20:24], start=True, stop=True)
                gsteps.append(g4)

                def g5():
                    V([pG_r], [gtr], "tensor_copy", gt[:, 28:32], pG[:, 0:4])
                    V([gtr], [gtr], "tensor_scalar", gt[:, 32:36], gt[:, 28:32], -1.0, None, ALU.mult)
                gsteps.append(g5)

                def g6():
                    A([gtr], [gtr], gt[:, 36:40], gt[:, 28:32], AF.Exp)
                gsteps.append(g6)

                def g7():
                    kb.op("pe", [gtr, cst_r], [pG_r], "matmul", pG[:, 4:8], cst[:, C_LS:C_LS + 128], gt[:, 28:32], start=True, stop=True)
                    kb.op("pe", [gtr, cst_r], [pG_r], "matmul", pG[:, 8:12], cst[:, C_SL0:C_SL0 + 128], gt[:, 28:32], start=True, stop=True)
                    kb.op("pe", [gtr, cst_r], [pG_r], "matmul", pG[:, 12:16], cst[:, C_SL1:C_SL1 + 128], gt[:, 28:32], start=True, stop=True)
                gsteps.append(g7)

                def g8():
                    V([pG_r, gtr], [gtr], "tensor_tensor", gt[:, 52:56], pG[:, 4:8], gt[:, 28:32], ALU.subtract)
                gsteps.append(g8)

                def g9():
                    A([gtr], [gtr], gt[:, 40:44], gt[:, 52:56], AF.Exp)
                    A([pG_r], [gtr], gt[:, 44:52], pG[:, 8:16], AF.Exp)
                gsteps.append(g9)

                def g10():
                    V([gtr], [gtr], "tensor_tensor", gt[:, 56:60], gt[:, 24:28], gt[:, 36:40], ALU.mult)
                gsteps.append(g10)


                def s1(h):
                    T = H[h]
                    stq, sr = T["stq"]
                    qh = q_[:, h * 128:(h + 1) * 128]; kh = q_[:, 512 + h * 128:512 + (h + 1) * 128]; vh = q_[:, 1024 + h * 128:1024 + (h + 1) * 128]
                    A([qr_], [T["junk"][1], sr], T["junk"][0][:], qh, AF.Square, accum_out=stq[:, 0:1])
                    A([qr_], [T["junk"][1], sr], T["junk"][0][:], kh, AF.Square, accum_out=stq[:, 1:2])
                    A([sr], [sr], stq[:, 2:4], stq[:, 0:2], AF.Sqrt, bias=EPS)
                    V([sr], [sr], "reciprocal", stq[:, 4:6], stq[:, 2:4])
                    V([sr], [sr], "tensor_scalar", stq[:, 6:7], stq[:, 4:5], 128.0 ** -0.5, None, ALU.mult)
                    V([sr, gtr], [sr], "tensor_tensor", stq[:, 7:8], stq[:, 5:6], gt[:, 40 + h:41 + h], ALU.mult)
                    V([sr, cst_r], [sr], "tensor_scalar", stq[:, 8:10], cst[:, C_IND0:C_IND0 + 2], stq[:, 7:8], None, ALU.mult)
                    V([sr, gtr], [sr], "tensor_tensor", stq[:, 10:11], stq[:, 5:6], gt[:, 56 + h:57 + h], ALU.mult)

                def s1b(h):
                    T = H[h]
                    stq, sr = T["stq"]
                    qh = q_[:, h * 128:(h + 1) * 128]; kh = q_[:, 512 + h * 128:512 + (h + 1) * 128]; vh = q_[:, 1024 + h * 128:1024 + (h + 1) * 128]
                    A([qr_, sr], [T["kn"][1]], T["kn"][0][:], kh, AF.Copy, scale=stq[:, 5:6])
                    A([qr_, sr], [T["qs"][1]], T["qs"][0][:], qh, AF.Copy, scale=stq[:, 6:7])
                    V([qr_, sr], [T["kd0"][1]], "tensor_scalar", T["kd0"][0][:], kh, stq[:, 8:9], None, ALU.mult)
                    V([qr_, sr], [T["kd1"][1]], "tensor_scalar", T["kd1"][0][:], kh, stq[:, 9:10], None, ALU.mult)
                    V([qr_, sr], [T["kbg"][1]], "tensor_scalar", T["kbg"][0][:], kh, stq[:, 10:11], None, ALU.mult)
                    V([qr_, gtr], [T["vb"][1]], "tensor_scalar", T["vb"][0][:], vh, gt[:, 24 + h:25 + h], None, ALU.mult)
                    V([cst_r, gtr], [T["dg"][1]], "tensor_scalar", T["dg"][0][:], identf, gt[:, 28 + h:29 + h], None, ALU.mult)
                    V([idb_r, gtr], [T["dq"][1]], "tensor_scalar", T["dq"][0][:], idb[:], gt[:, 36 + h:37 + h], None, ALU.mult)

                def s2(h):
                    T = H[h]
                    pt, ptr = pTb[h // 2]
                    s0 = (h % 2) * 4
                    kb.op("pe", [T["kn"][1], idb_r], [ptr], "transpose", pt[:, s0, :], T["kn"][0][:], idb[:])
                    kb.op("pe", [T["qs"][1], idb_r], [ptr], "transpose", pt[:, s0 + 1, :], T["qs"][0][:], idb[:])
                    kb.op("pe", [T["qs"][1], T["dq"][1]], [pWr[h][3]], "matmul", pW[h][:, 3, :], T["qs"][0][:], T["dq"][0][:], start=True, stop=True)
                    V([ptr], [T["kT"][1]], "tensor_copy", T["kT"][0][:], pt[:, s0, :])
                    V([ptr], [T["qT"][1]], "tensor_copy", T["qT"][0][:], pt[:, s0 + 1, :])
                    A([pWr[h][3]], [T["qdT"][1]], T["qdT"][0][:], pW[h][:, 3, :], AF.Copy)

                def s3(h):
                    T = H[h]
                    kb.op("pe", [T["kT"][1]], [pWr[h][0]], "matmul", pW[h][:, 0, :], T["kT"][0][:], T["kT"][0][:], start=True, stop=True)
                    kb.op("pe", [T["kT"][1], T["qT"][1]], [pWr[h][1]], "matmul", pW[h][:, 1, :], T["kT"][0][:], T["qT"][0][:], start=True, stop=True)
                    kb.op("pe", [T["dg"][1], cst_r], [pWr[h][2]], "matmul", pW[h][:, 2, :], ones, T["dg"][0][:], start=True, stop=False)
                    kb.op("pe", [cst_r], [pWr[h][2]], "matmul", pW[h][:, 2, :], identf, cst[:, C_PM:C_PM + 128], start=False, stop=True)
                    kb.op("pe", [T["dg"][1], cst_r], [pWr[h][3]], "matmul", pW[h][:, 3, :], ones, T["dg"][0][:], start=True, stop=False)
                    kb.op("pe", [cst_r], [pWr[h][3]], "matmul", pW[h][:, 3, :], identf, cst[:, C_NA:C_NA + 128], start=False, stop=True)

                def s3b(h):
                    T = H[h]
                    A([pWr[h][2], gtr], [T["decM"][1]], T["decM"][0][:], pW[h][:, 2, :], AF.Exp, bias=gt[:, 28 + h:29 + h], scale=-1.0)
                    A([pWr[h][3], gtr], [T["decAT"][1]], T["decAT"][0][:], pW[h][:, 3, :], AF.Exp, bias=gt[:, 32 + h:33 + h], scale=1.0)
                    V([pWr[h][0], gtr, T["decM"][1]], [T["Ma"][1]], "scalar_tensor_tensor", T["Ma"][0][:], pW[h][:, 0, :], gt[:, 24 + h:25 + h], T["decM"][0][:], ALU.mult, ALU.mult)
                    V([pWr[h][1], T["decAT"][1]], [T["attnT"][1]], "tensor_tensor", T["attnT"][0][:], pW[h][:, 1, :], T["decAT"][0][:], ALU.mult)

                def s4(h):
                    T = H[h]
                    kb.op("pe", [T["Ma"][1], cst_r], [pWr[h][0]], "transpose", pW[h][:, 0, :], T["Ma"][0][:], identf)
                    A([pWr[h][0]], [T["Na"][1]], T["Na"][0][:], pW[h][:, 0, :], AF.Copy)
                    V([pWr[h][0], cst_r], [T["Pa"][1]], "scalar_tensor_tensor", T["Pa"][0][:], pW[h][:, 0, :], -1.0, identf, ALU.mult, ALU.add)

                def chain(L):
                    def f(h):
                        T = H[h]
                        cur, nxt = ("a", "b") if L % 2 == 1 else ("b", "a")
                        Mc, Nc, Pc = T["M" + cur], T["N" + cur], T["P" + cur]
                        Mn, Nn, Pn = T["M" + nxt], T["N" + nxt], T["P" + nxt]
                        kb.op("pe", [Mc[1], Nc[1]], [pWr[h][0]], "matmul", pW[h][:, 0, :], Nc[0][:], Mc[0][:], start=True, stop=True)
                        if L < 5:
                            kb.op("pe", [Mc[1], Nc[1]], [pWr[h][1]], "matmul", pW[h][:, 1, :], Mc[0][:], Nc[0][:], start=True, stop=True)
                        V([pWr[h][0], cst_r], [T["MI"][1]], "tensor_tensor", T["MI"][0][:], pW[h][:, 0, :], identf, ALU.add)
                        if L < 5:
                            A([pWr[h][0]], [Mn[1]], Mn[0][:], pW[h][:, 0, :], AF.Copy)
                            A([pWr[h][1]], [Nn[1]], Nn[0][:], pW[h][:, 1, :], AF.Copy)

                    def f2(h):
                        T = H[h]
                        cur, nxt = ("a", "b") if L % 2 == 1 else ("b", "a")
                        Pc, Pn = T["P" + cur], T["P" + nxt]
                        kb.op("pe", [T["MI"][1], Pc[1]], [pWr[h][2]], "matmul", pW[h][:, 2, :], T["MI"][0][:], Pc[0][:], start=True, stop=True)
                        V([pWr[h][2]], [Pn[1]], "tensor_copy", Pn[0][:], pW[h][:, 2, :])
                    return [f, f2]

                def s10(h):
                    T = H[h]
                    TT = T["Pb"]
                    kb.op("pe", [TT[1], T["vb"][1]], [pWr[h][0]], "matmul", pW[h][:, 0, :], TT[0][:], T["vb"][0][:], start=True, stop=True)
                    kb.op("pe", [TT[1], T["kbg"][1]], [pWr[h][1]], "matmul", pW[h][:, 1, :], T["kbg"][0][:], TT[0][:], start=True, stop=True)
                    A([pWr[h][0]], [T["u"][1]], T["u"][0][:], pW[h][:, 0, :], AF.Copy)
                    V([pWr[h][1]], [T["wT"][1]], "tensor_copy", T["wT"][0][:], pW[h][:, 1, :])

                def scan(c):
                    rows = slice(c * 64, (c + 1) * 64)

                    def fa(h):
                        T = H[h]
                        A([T["S"][1]], [T["Sb"][1]], T["Sb"][0][:], T["S"][0][:], AF.Copy)
                        kb.op("pe", [T["wT"][1], T["Sb"][1]], [pWr[h][2]], "matmul", pW[h][:, 2, :], T["wT"][0][:], T["Sb"][0][:], start=True, stop=True)

                    def fb(h):
                        T = H[h]
                        V([T["u"][1], pWr[h][2]], [T["vnew"][1]], "tensor_tensor", T["vnew"][0][rows, :], T["u"][0][rows, :], pW[h][rows, 2, :], ALU.subtract)
                        kb.op("pe", [T["qdT"][1], T["Sb"][1]], [pWr[h][c]], "matmul", pW[h][:, c, :], T["qdT"][0][:], T["Sb"][0][:], start=True, stop=False)
                        kb.op("pe", [T["attnT"][1], T["vnew"][1]], [pWr[h][c]], "matmul", pW[h][:, c, :], T["attnT"][0][:], T["vnew"][0][:], start=False, stop=True)
                        kd = T["kd%d" % c]
                        kb.op("pe", [kd[1], T["vnew"][1]], [pWr[h][3]], "matmul", pW[h][:, 3, :], kd[0][:], T["vnew"][0][:], start=True, stop=True)

                    def fc(h):
                        T = H[h]
                        A([pWr[h][c]], [o_r], o_sb[rows, h, :], pW[h][rows, c, :], AF.Copy)
                        V([T["S"][1], gtr, pWr[h][3]], [T["S"][1]], "scalar_tensor_tensor", T["S"][0][:], T["S"][0][:], gt[:, 44 + c * 4 + h:45 + c * 4 + h], pW[h][:, 3, :], ALU.mult, ALU.add)
                    return [fa, fb, fc]

                def _final():
                    V([o_r], [osq_r], "tensor_tensor", osq[:], o_sb[:], o_sb[:], ALU.mult)
                    V([osq_r], [ost_r], "tensor_reduce", ost[:, 0:4], osq[:], AX.X, ALU.add)
                    A([ost_r], [ost_r], ost[:, 4:8], ost[:, 0:4], AF.Sqrt, bias=EPS, scale=1.0 / 128)
                    V([ost_r], [ost_r], "reciprocal", ost[:, 8:12], ost[:, 4:8])
                    V([o_r, ost_r], [osq_r], "tensor_tensor", osq[:], o_sb[:], ost[:, 8:12].unsqueeze(2).to_broadcast([128, 4, 128]), ALU.mult)
                    V([osq_r, rowv_r], [osq_r], "tensor_tensor", osq[:], osq[:], rowv[:, R_GG:R_GG + 128].unsqueeze(1).to_broadcast([128, 4, 128]), ALU.mult)
                    V([osq_r, qr_], [omg_r], "tensor_tensor", omg[:], osq[:], q_[:, 1536:2048].rearrange("p (h c) -> p h c", h=4), ALU.mult)
                    pt, ptr = pTb[ti % 2]
                    for h in range(4):
                        kb.op("pe", [omg_r, idb_r], [ptr], "transpose", pt[:, (3 if h % 2 == 0 else 7) - (h // 2) * 0 - (0), :] if False else pt[:, [2, 3, 6, 7][h], :], omg[:, h, :], idb[:])
                    ot, otr = oT[b]
                    V([ptr], [otr], "tensor_copy", ot[:, 0:2, :], pt[:, 2:4, :])
                    V([ptr], [otr], "tensor_copy", ot[:, 2:4, :], pt[:, 6:8, :])
                    kb.store("sp", omixT_d.ap()[0:512, t0:t0 + 128].rearrange("(h p) t -> p h t", p=128), ot[:], otr, omix_r)

                pre = []
                stages = [s1, s1b, s2, s3, s3b, s4] + chain(1) + chain(2) + chain(3) + chain(4) + chain(5) + [s10]
                for step in range(len(stages) + 3):
                    for h in range(4):
                        k = step - h
                        if 0 <= k < len(stages):
                            pre.append(lambda st=stages[k], h=h: st(h))
                scans = []
                for c in range(2):
                    subs = scan(c)
                    L = []
                    for step in range(len(subs) + 3):
                        for h in range(4):
                            k = step - h
                            if 0 <= k < len(subs):
                                L.append(lambda f=subs[k], h=h: f(h))
                    scans.append(L)

                return pre, scans, _final, gsteps

            GT2, GT2_r = kb.sb("GT2", [128, D], F32, pb)
            kb.load("sp", GT2[:], mod_d.ap()[5120:6144].partition_broadcast(128), GT2_r, reads=[mod_r])
            stw = [kb.sb("cvw%d" % i, [128, 4096], F32, pb) for i in range(2)]
            stb = [kb.sb("cvb%d" % i, [128, 4096], BF16, pb) for i in range(2)]
            NCH = 16384 // 512
            conv_steps = []
            for ci in range(NCH):
                for which in range(2):
                    def f_cv(ci=ci, which=which):
                        s_, sr = stw[which]
                        b_, br = stb[which]
                        src = (eu_d, ev_d)[which].ap()[ci * 512:(ci + 1) * 512, :].rearrange("(p r) c -> p r c", r=4)
                        dst = uvb_d.ap()[ci * 512:(ci + 1) * 512, which * 1024:(which + 1) * 1024].rearrange("(p r) c -> p r c", r=4)
                        kb.load("pool", s_[:].rearrange("p (r c) -> p r c", r=4), src, sr)
                        if which == 0:
                            kb.op("act", [sr], [br], "activation", b_[:], s_[:], AF.Copy)
                        else:
                            kb.op("dve", [sr, GT2_r], [br], "tensor_tensor", b_[:].rearrange("p (r c) -> p r c", r=4), s_[:].rearrange("p (r c) -> p r c", r=4), GT2[:].unsqueeze(1).to_broadcast([128, 4, 1024]), ALU.mult)
                        kb.store("pool", dst, b_[:].rearrange("p (r c) -> p r c", r=4), br, uvb_r)
                    conv_steps.append(f_cv)
            cvi = [0]

            def emit_conv(n):
                while n > 0 and cvi[0] < len(conv_steps):
                    conv_steps[cvi[0]]()
                    cvi[0] += 1
                    n -= 1
            GT_ = {}

            def gtile(ti):
                if ti >= NT:
                    return [], None, None, []
                if ti not in GT_:
                    GT_[ti] = gdn_tile(ti)
                return GT_[ti]

            def merge2(A_, B_):
                out, ia, ib = [], 0, 0
                na, nb = len(A_), len(B_)
                for k in range(na + nb):
                    if ib >= nb or (ia < na and (ia + 1) * (nb + 1) <= (ib + 1) * (na + 1)):
                        out.append(A_[ia]); ia += 1
                    else:
                        out.append(B_[ib]); ib += 1
                return out
            for f in gtile(0)[3] + gtile(1)[3] + gtile(0)[0]:
                f()
            cur = gtile(0)
            for ti in range(NT):
                emit_conv(1)
                nxt = gtile(ti + 1)
                npre = merge2(nxt[0], gtile(ti + 2)[3])
                h1 = len(npre) // 2
                for f in cur[1][0]:
                    f()
                for f in npre[:h1]:
                    f()
                for f in cur[1][1]:
                    f()
                for f in npre[h1:]:
                    f()
                cur[2]()
                cur = nxt
                GT_.pop(ti, None)
            emit_conv(len(conv_steps))
            kb.barrier()

        with ExitStack() as pd:
            wout, wout_r = kb.sb("wout", [128, 8, 1024], BF16, pd)
            wpq, wpq_r = kb.sb("wpq", [128, 8, 2048], BF16, pd)
            skT, skT_r = kb.sb("skTb", [128, 16, 128], BF16, pd)
            A2b, A2b_r = kb.sb("A2b", [128, D], F32, pd)
            B2b, B2b_r = kb.sb("B2b", [128, D], F32, pd)
            FNG, FNG_r = kb.sb("FNG", [128, D], F32, pd)
            bc = lambda a, n: mod_d.ap()[a:a + n].partition_broadcast(128)
            kb.load("sp", A2b[:], bc(4096, 1024), A2b_r, reads=[mod_r])
            kb.load("sp", B2b[:], bc(3072, 1024), B2b_r, reads=[mod_r])
            kb.load("sp", FNG[:], lnf_d.ap().partition_broadcast(128), FNG_r)
            kb.op("dve", [A2b_r, FNG_r], [A2b_r], "scalar_tensor_tensor", A2b[:], A2b[:], 1.0, FNG[:], ALU.add, ALU.mult)
            kb.load("sp", FNG[:], fng_d.ap().partition_broadcast(128), FNG_r)
            with ExitStack() as pw:
                GT1, GT1_r = kb.sb("GT1", [128, D], F32, pw)
                kb.load("sp", GT1[:], bc(2048, 1024), GT1_r, reads=[mod_r])
                stw = [kb.sb("stw%d" % i, [128, 4096], F32, pw) for i in range(2)]
                stb = [kb.sb("stb%d" % i, [128, 4096], BF16, pw) for i in range(2)]
                for kt in range(8):
                    s_, sr = stw[kt % 2]
                    kb.load("sp", s_[:, 0:1024], wout_d.ap()[kt * 128:(kt + 1) * 128, :], sr)
                    kb.op("dve", [sr, GT1_r], [wout_r], "tensor_tensor", wout[:, kt, :], s_[:, 0:1024], GT1[:], ALU.mult)
                    s2, sr2 = stw[(kt + 1) % 2]
                    kb.load("sp", s2[:, 0:2048], wpq_d.ap()[kt * 128:(kt + 1) * 128, :], sr2)
                    kb.op("act", [sr2], [wpq_r], "activation", wpq[:, kt, :], s2[:, 0:2048], AF.Copy)
                s_, sr = stw[0]
                kb.load("sp", s_[:, 0:2048], skT_d.ap(), sr)
                kb.op("dve", [sr], [skT_r], "tensor_copy", skT[:].rearrange("p a b -> p (a b)"), s_[:, 0:2048])
                kb.barrier()
            GS = 4
            NG = 128 // GS
            NBUF = 3
            uvg = [(kb.sb("uvg%d" % i, [128, GS, 2048], BF16, pd)[0], [Res("uvg%d_%d" % (i, j)) for j in range(GS)]) for i in range(NBUF)]
            dg = [kb.sb("dgp%d" % i, [128, 128], BF16, pd) for i in range(4)]
            two = lambda n, shp, dt: [kb.sb("%s%d" % (n, i), shp, dt, pd) for i in range(2)]
            om = [kb.sb("om0", [128, 8, 128], BF16, pd)] * 2
            xt = [kb.sb("xtD0", [128, D], F32, pd)] * 2
            x1s = [kb.sb("x1_%d" % i, [128, D], F32, pd) for i in range(3)]
            h2s = [kb.sb("h2_%d" % i, [128, D], BF16, pd) for i in range(3)]
            eids = two("eid", [128, 128], I32)
            gates = two("gate", [128, 8, 16], F32)
            ob = xt
            junkb, junkb_r = kb.sb("junkDb", [128, D], BF16, pd)
            junkv, junkv_r = kb.sb("junkDv", [128, D], BF16, pd)
            h2T, h2T_r = kb.sb("h2T", [128, 8, 128], BF16, pd)
            qTb, qTb_r = kb.sb("qTb", [128, 16, 128], BF16, pd)
            scs = two("scD", [128, 16, 128], F32)
            iu1, iu1_r = kb.sb("iu1", [128, 16, 16], U32, pd)
            iu2, iu2_r = kb.sb("iu2", [128, 8, 16], U32, pd)
            wk, wk_r = kb.sb("wkD", [128, 256], F32, pd)
            stp, stp_r = kb.sb("stop", [128, 16, 16], F32, pd)
            itp, itp_r = kb.sb("itop", [128, 16, 16], F32, pd)
            best, best_r = kb.sb("best", [128, 8, 16], F32, pd)
            posf, posf_r = kb.sb("posf", [128, 8, 16], F32, pd)
            ab, ab_r = kb.sb("abD", [128, 4, 8, 16], F32, pd)
            isel, isel_r = kb.sb("isel", [128, 2, 8, 16], F32, pd)
            eidf, eidf_r = kb.sb("eidf", [128, 128], F32, pd)
            gs, gs_r = kb.sb("gsD", [128, 16], F32, pd)
            dots, dots_r = kb.sb("dots", [128, 128], F32, pd)
            ges = [kb.sb("geD%d" % i, [128, 4, GS], F32, pd) for i in range(3)]
            hgs = [kb.sb("hgD%d" % i, [128, GS], F32, pd) for i in range(3)]
            coef, coef_r = kb.sb("coef", [128, 128], F32, pd)
            stD, stD_r = kb.sb("stD", [128, 8], F32, pd)
            stE, stE_r = kb.sb("stE", [128, 8], F32, pd)
            pM, pM_r = kb.ps("pMD", [128, 2, 512], F32, pd)
            pX, pX_r = kb.ps("pXD", [128, 512], F32, pd)
            pTd, pTd_r = kb.ps("pTD", [128, 8, 128], BF16, pd)
            pQ, pQ_r = kb.ps("pQD", [128, 4, 128], F32, pd)
            pS, pS_r = pQ, pQ_r
            STps = [kb.ps("STp%d" % i, [128, 4, 128], F32, pd) for i in range(2)]
            Op, Op_r = kb.ps("Op", [128, 129], F32, pd)
            nk, nk_r = kb.sb("nk", [128, 8], F32, pd)
            cnb, cnb_r = kb.sb("cnb", [128, 128], BF16, pd)
            Qn2 = [kb.sb("Qn%d" % i, [128, 128], BF16, pd) for i in range(2)]
            Qr2 = [kb.sb("Qr%d" % i, [65, 128], BF16, pd) for i in range(2)]
            Kg = [kb.sb("Kg%d" % i, [128, 512], BF16, pd) for i in range(4)]
            Krg = [kb.sb("Krg%d" % i, [65, 512], BF16, pd) for i in range(4)]
            Vg = [kb.sb("Vg%d" % i, [128, 4, 129], BF16, pd) for i in range(4)]
            PTs = [kb.sb("PT%d" % i, [128, 4, 128], BF16, pd) for i in range(2)]
            rc, rc_r = kb.sb("rc", [128, 2], F32, pd)
            onb, onb_r = kb.sb("onb", [128, 128], BF16, pd)
            kb.op("dve", [kmax_r], [nk_r], "tensor_scalar", nk[:, 0:4], kmax[:, 0:4], kmax[:, 4:5], None, ALU.add)
            kb.op("act", [nk_r], [nk_r], "activation", nk[:, 4:8], nk[:, 0:4], AF.Sqrt, scale=1.0609)
            kb.op("dve", [nk_r], [nk_r], "tensor_scalar", nk[:, 4:8], nk[:, 4:8], -1.0, None, ALU.mult)
            kb.op("dve", [cst_r], [cnb_r], "tensor_copy", cnb[:], cst[:, C_CN:C_CN + 128])
            mla_cnt = [0]
            V = lambda r, w, m, *a, **k: kb.op("dve", r, w, m, *a, **k)
            A = lambda r, w, *a, **k: kb.op("act", r, w, "activation", *a, **k)
            iota16 = cst[:, C_IOTA:C_IOTA + 16]

            def front(ti):
                t0 = ti * 128
                b = ti % 2
                o_, omr = om[b]
                x_, xr = xt[b]
                x1, x1_r = x1s[ti % 3]
                h2, h2_r = h2s[ti % 3]
                eid, eid_r = eids[b]
                gate, gate_r = gates[b]
                sc, sc_r = scs[b]
                cand, cand_r = sc[:].rearrange("p (h t) k -> p h (t k)", t=2), sc_r
                eq, eq_r = cand[:].rearrange("p h (a b) -> p h a b", a=16), sc_r
                steps = []
                add = steps.append

                qi = ti
                units = []
                for h in range(4):
                    units.append(("q", h, 0))
                    for g0 in range(0, qi + 1, 4):
                        units.append(("g", h, g0))
                    units.append(("fin", h, 0))
                gidx = {}
                for u in units:
                    if u[0] == "g":
                        gidx[u] = mla_cnt[0]
                        mla_cnt[0] += 1

                def u_load(u):
                    kind, h, g0 = u
                    if kind == "q":
                        qn, qnr = Qn2[h % 2]
                        qr, qrr = Qr2[h % 2]
                        kb.load("sp", qn[:], qnT_d.ap()[h, :, t0:t0 + 128], qnr, reads=[qnT_r])
                        kb.load("sp", qr[:], qrT_d.ap()[h, :, t0:t0 + 128], qrr, reads=[qrT_r])
                    elif kind == "g":
                        c = gidx[u] % 4
                        n = min(g0 + 4, qi + 1) - g0
                        kg, kgr = Kg[c]
                        krg, krgr = Krg[c]
                        vg_, vgr = Vg[c]
                        kb.load("sp", kg[:, 0:n * 128], knT_d.ap()[h, :, g0 * 128:(g0 + n) * 128], kgr, reads=[knT_r])
                        kb.load("sp", krg[:, 0:n * 128], krT_d.ap()[:, g0 * 128:(g0 + n) * 128], krgr, reads=[krT_r])
                        kb.load("sp", vg_[:, 0:n, :], v_d.ap()[h, g0 * 128:(g0 + n) * 128, :].rearrange("(n p) c -> p n c", p=128), vgr, reads=[v_r])

                def u_comp(u):
                    kind, h, g0 = u
                    qn, qnr = Qn2[h % 2]
                    qr, qrr = Qr2[h % 2]
                    if kind == "q":
                        V([qrr, nk_r], [qrr], "tensor_scalar", qr[64:65, :], qr[64:65, :], nk[64:65, 4 + h:5 + h], None, ALU.mult)
                    elif kind == "g":
                        js = list(range(g0, min(g0 + 4, qi + 1)))
                        n = len(js)
                        c = gidx[u] % 4
                        kg, kgr = Kg[c]
                        krg, krgr = Krg[c]
                        vg_, vgr = Vg[c]
                        pt, ptr = PTs[gidx[u] % 2]
                        STp, STp_r = STps[gidx[u] % 2]
                        for jj, j in enumerate(js):
                            kb.op("pe", [kgr, qnr], [STp_r], "matmul", STp[:, jj, :], kg[:, jj * 128:(jj + 1) * 128], qn[:], start=True, stop=False)
                            kb.op("pe", [krgr, qrr], [STp_r], "matmul", STp[:, jj, :], krg[:, jj * 128:(jj + 1) * 128], qr[:], start=False, stop=(j != qi))
                            if j == qi:
                                kb.op("pe", [idb_r, cnb_r], [STp_r], "matmul", STp[:, jj, :], idb[:], cnb[:], start=False, stop=True)
                        A([STp_r], [ptr], pt[:, 0:n, :], STp[:, 0:n, :], AF.Exp)
                        for jj, j in enumerate(js):
                            kb.op("pe", [ptr, vgr], [Op_r], "matmul", Op[:], pt[:, jj, :], vg_[:, jj, :], start=(j == 0), stop=(j == qi))
                    else:
                        V([Op_r], [rc_r], "reciprocal", rc[:, 0:1], Op[:, 128:129])
                        A([Op_r, rc_r], [onb_r], onb[:], Op[:, 0:128], AF.Copy, scale=rc[:, 0:1])
                        kb.op("pe", [onb_r, idb_r], [pTd_r], "transpose", pTd[:, 0, :], onb[:], idb[:])
                        A([pTd_r], [omr], o_[:, 4 + h, :], pTd[:, 0, :], AF.Copy)

                LOOK = 3
                for k, u in enumerate(units):
                    def f_u(k=k, u=u):
                        if k == 0:
                            for kk in range(min(LOOK, len(units))):
                                u_load(units[kk])
                        if k + LOOK < len(units):
                            u_load(units[k + LOOK])
                        u_comp(u)
                    add(f_u)

                def f_load():
                    kb.load("sp", o_[:, 0:4, :], omixT_d.ap()[0:512, t0:t0 + 128].rearrange("(kt p) t -> p kt t", p=128), omr, reads=[omix_r])
                    kb.load("sp", x_[:], x_d.ap()[t0:t0 + 128, :], xr)
                add(f_load)

                def f_mix():
                    for half in range(2):
                        for kt in range(8):
                            kb.op("pe", [omr, wout_r], [pX_r], "matmul", pX[:], o_[:, kt, :], wout[:, kt, half * 512:(half + 1) * 512], start=(kt == 0), stop=(kt == 7))
                        V([xr, pX_r], [x1_r], "tensor_tensor", x1[:, half * 512:(half + 1) * 512], x_[:, half * 512:(half + 1) * 512], pX[:], ALU.add)
                add(f_mix)

                def f_norm():
                    A([x1_r], [xr, stD_r], x_[:], x1[:], AF.Square, accum_out=stD[:, 0:1])
                    A([stD_r], [stD_r], stD[:, 1:2], stD[:, 0:1], AF.Sqrt, bias=EPS, scale=1.0 / D)
                    V([stD_r], [stD_r], "reciprocal", stD[:, 2:3], stD[:, 1:2])
                    V([x1_r, stD_r, A2b_r], [xr], "scalar_tensor_tensor", x_[:], x1[:], stD[:, 2:3], A2b[:], ALU.mult, ALU.mult)
                add(f_norm)

                def f_h2():
                    V([xr, B2b_r], [h2_r], "tensor_tensor", h2[:], x_[:], B2b[:], ALU.add)
                    for kt in range(8):
                        kb.op("pe", [h2_r, idb_r], [pTd_r], "transpose", pTd[:, kt, :], h2[:, kt * 128:(kt + 1) * 128], idb[:])
                    A([pTd_r], [h2T_r], h2T[:], pTd[:], AF.Copy)
                add(f_h2)
                for g4 in range(4):
                    def f_q(g4=g4):
                        for j in range(4):
                            hp = g4 * 4 + j
                            for kt in range(8):
                                kb.op("pe", [h2T_r, wpq_r], [pQ_r], "matmul", pQ[:, j, :], wpq[:, kt, hp * 128:(hp + 1) * 128], h2T[:, kt, :], start=(kt == 0), stop=(kt == 7))
                        A([pQ_r], [qTb_r], qTb[:, g4 * 4:(g4 + 1) * 4, :], pQ[:], AF.Copy)
                    add(f_q)
                for g4 in range(4):
                    def f_s(g4=g4):
                        for j in range(4):
                            hp = g4 * 4 + j
                            kb.op("pe", [qTb_r, skT_r], [pS_r], "matmul", pS[:, j, :], qTb[:, hp, :], skT[:, hp, :], start=True, stop=True)
                        A([pS_r], [sc_r], sc[:, g4 * 4:(g4 + 1) * 4, :], pS[:], AF.Copy)
                    add(f_s)

                split = len(steps)

                def top16(src, n, vout, vres, iu, iures):
                    V([sc_r], [vres], "max", vout[:, 0:8], src)
                    V([sc_r, vres], [iures], "max_index", iu[:, 0:8], vout[:, 0:8], src)
                    V([sc_r, vres], [wk_r], "match_replace", wk[:, 0:n], vout[:, 0:8], src, -1e30)
                    V([wk_r], [vres], "max", vout[:, 8:16], wk[:, 0:n])
                    V([wk_r, vres], [iures], "max_index", iu[:, 8:16], vout[:, 8:16], wk[:, 0:n])
                for hp in range(16):
                    add(lambda hp=hp: top16(sc[:, hp, :], 128, stp[:, hp, :], stp_r, iu1[:, hp, :], iu1_r))
                add(lambda: V([iu1_r], [itp_r], "tensor_copy", itp[:], iu1[:]))
                st4 = stp[:].rearrange("p (h t) k -> p h t k", t=2)
                it4 = itp[:].rearrange("p (h t) k -> p h t k", t=2)
                add(lambda: V([stp_r], [cand_r], "tensor_tensor", cand[:].rearrange("p h (a b) -> p h a b", a=16), st4[:, :, 0, :].unsqueeze(3).to_broadcast([128, 8, 16, 16]), st4[:, :, 1, :].unsqueeze(2).to_broadcast([128, 8, 16, 16]), ALU.add))
                for h in range(8):
                    add(lambda h=h: top16(cand[:, h, :], 256, best[:, h, :], best_r, iu2[:, h, :], iu2_r))
                add(lambda: V([iu2_r], [posf_r], "tensor_copy", posf[:], iu2[:]))

                def f_idx():
                    V([posf_r], [ab_r], "tensor_scalar", ab[:, 2], posf[:], 1.0 / 16, -0.46875, ALU.mult, ALU.add)
                    V([ab_r], [ab_r], "tensor_scalar", ab[:, 3], ab[:, 2], 12582912.0, None, ALU.add)
                    V([ab_r], [ab_r], "tensor_scalar", ab[:, 0], ab[:, 3], -12582912.0, None, ALU.add)
                    V([ab_r, posf_r], [ab_r], "scalar_tensor_tensor", ab[:, 1], ab[:, 0], -16.0, posf[:], ALU.mult, ALU.add)
                add(f_idx)
                for t in range(2):
                    def f_sel(t=t):
                        V([ab_r, cst_r], [eq_r], "tensor_tensor", eq[:], ab[:, t].unsqueeze(3).to_broadcast([128, 8, 16, 16]), iota16.unsqueeze(1).unsqueeze(1).to_broadcast([128, 8, 16, 16]), ALU.is_equal)
                        V([eq_r, itp_r], [eq_r], "tensor_tensor", eq[:], eq[:], it4[:, :, t, :].unsqueeze(2).to_broadcast([128, 8, 16, 16]), ALU.mult)
                        V([eq_r], [isel_r], "tensor_reduce", isel[:, t], eq[:], AX.X, ALU.add)
                    add(f_sel)

                def f_gate():
                    V([isel_r], [eidf_r], "scalar_tensor_tensor", eidf[:].rearrange("p (h k) -> p h k", h=8), isel[:, 0], 128.0, isel[:, 1], ALU.mult, ALU.add)
                    V([eidf_r], [eid_r], "tensor_copy", eid[:], eidf[:])
                    V([best_r], [gate_r], "tensor_tensor", gate[:], best[:], best[:, :, 0:1].to_broadcast([128, 8, 16]), ALU.subtract)
                    A([gate_r], [gate_r], gate[:], gate[:], AF.Exp)
                    V([gate_r], [gs_r], "tensor_reduce", gs[:, 0:8], gate[:], AX.X, ALU.add)
                    V([gs_r], [gs_r], "reciprocal", gs[:, 8:16], gs[:, 0:8])
                    V([gate_r, gs_r], [gate_r], "tensor_tensor", gate[:], gate[:], gs[:, 8:16].unsqueeze(2).to_broadcast([128, 8, 16]), ALU.mult)
                add(f_gate)
                return steps[:split], steps[split:]

            def back(ti, fsteps, deferred=None):
                t0 = ti * 128
                b = ti % 2
                x1, x1_r = x1s[ti % 3]
                h2, h2_r = h2s[ti % 3]
                eid, eid_r = eids[b]
                gate, gate_r = gates[b]
                gflat = gate[:].rearrange("p h k -> p (h k)")
                V([], [dots_r], "memset", dots[:], 0.0)
                nf = len(fsteps)
                fi = 0

                def bufof(g):
                    return uvg[(ti * NG + g) % NBUF]

                def st_gather(g):
                    buf, bufr = bufof(g)
                    for j in range(GS):
                        hk = g * GS + j
                        kb.dma("pool", [eid_r, uvb_r], [bufr[j]], bufr[j], False, "indirect_dma_start", out=buf[:, j, :], out_offset=None, in_=uvb_d.ap(), in_offset=bass.IndirectOffsetOnAxis(ap=eid[:, hk:hk + 1], axis=0))

                def st_dots(g):
                    buf, bufr = bufof(g)
                    ge, ge_r = ges[g % 3]
                    for j in range(GS):
                        hk = g * GS + j
                        V([bufr[j], h2_r], [junkv_r, dots_r], "scalar_tensor_tensor", junkv[:], buf[:, j, 0:1024], 1.0, h2[:], ALU.mult, ALU.mult, accum_out=dots[:, hk:hk + 1])

                def st_pre(g):
                    ge, ge_r = ges[g % 3]
                    dsl = dots[:, g * GS:(g + 1) * GS]
                    V([dots_r], [ge_r], "tensor_tensor", ge[:, 0], dsl, dsl, ALU.mult)
                    V([ge_r], [ge_r], "tensor_scalar", ge[:, 0], ge[:, 0], 0.044715, 1.0, ALU.mult, ALU.add)
                    V([ge_r, dots_r], [ge_r], "tensor_tensor", ge[:, 1], ge[:, 0], dsl, ALU.mult)
                    A([ge_r], [ge_r], ge[:, 2], ge[:, 1], AF.Tanh, scale=0.7978845608028654)
                    hg, hg_r = hgs[g % 3]
                    V([dots_r, gate_r], [hg_r], "scalar_tensor_tensor", hg[:], dsl, 0.5, gflat[:, g * GS:(g + 1) * GS], ALU.mult, ALU.mult)

                def st_fin(g):
                    buf, bufr = bufof(g)
                    ge, ge_r = ges[g % 3]
                    dsl = dots[:, g * GS:(g + 1) * GS]
                    hg, hg_r = hgs[g % 3]
                    V([ge_r, hg_r], [coef_r], "scalar_tensor_tensor", coef[:, g * GS:(g + 1) * GS], ge[:, 2], 1.0, hg[:], ALU.add, ALU.mult)
                    for j in range(GS):
                        hk = g * GS + j
                        d_, dr = dg[hk % 4]
                        A([idb_r, coef_r], [dr], d_[:], idb[:], AF.Copy, scale=coef[:, hk:hk + 1])
                        for half in range(2):
                            kb.op("pe", [dr, bufr[j]], [pM_r], "matmul", pM[:, half, :], d_[:], buf[:, j, 1024 + half * 512:1024 + (half + 1) * 512], start=(hk == 0), stop=(hk == 127))

                for s_ in range(NG + 2):
                    if s_ < NG:
                        st_gather(s_)
                    if 0 <= s_ - 1 < NG:
                        st_dots(s_ - 1)
                        st_pre(s_ - 1)
                    if s_ == 2 and deferred is not None:
                        deferred()
                    if 0 <= s_ - 2 < NG:
                        st_fin(s_ - 2)
                    tgt = min(nf, ((s_ + 1) * nf) // NG)
                    while fi < tgt:
                        fsteps[fi]()
                        fi += 1
                while fi < nf:
                    fsteps[fi]()
                    fi += 1
                def fin_tile():
                    V([x1_r, pM_r], [x1_r], "tensor_tensor", x1[:].rearrange("p (a b) -> p a b", a=2), x1[:].rearrange("p (a b) -> p a b", a=2), pM[:], ALU.add)
                    A([x1_r], [junkb_r, stE_r], junkb[:], x1[:], AF.Square, accum_out=stE[:, 4:5])
                    A([stE_r], [stE_r], stE[:, 5:6], stE[:, 4:5], AF.Sqrt, bias=EPS, scale=1.0 / D)
                    V([stE_r], [stE_r], "reciprocal", stE[:, 6:7], stE[:, 5:6])
                    ot, otr = ob[b]
                    V([x1_r, stE_r, FNG_r], [otr], "scalar_tensor_tensor", ot[:], x1[:], stE[:, 6:7], FNG[:], ALU.mult, ALU.mult)
                    kb.store("sp", out_d.ap()[t0:t0 + 128, :], ot[:], otr)
                return fin_tile

            def merge(A_, B_):
                out, ia, ib = [], 0, 0
                na, nb = len(A_), len(B_)
                for k in range(na + nb):
                    if ib >= nb or (ia < na and (ia + 1) * (nb + 1) <= (ib + 1) * (na + 1)):
                        out.append(A_[ia]); ia += 1
                    else:
                        out.append(B_[ib]); ib += 1
                return out

            FR = {}

            def fr(ti):
                if ti >= NT:
                    return [], []
                if ti not in FR:
                    FR[ti] = front(ti)
                return FR[ti]
            for f in fr(0)[0] + fr(0)[1] + fr(1)[0]:
                f()
            pend = None
            for ti in range(NT):
                pend = back(ti, merge(fr(ti + 1)[1], fr(ti + 2)[0]), pend)
                FR.pop(ti, None)
            pend()
        kb.barrier()
        kb.finish()
    return nc, kb


def _host_inputs(inp, b, S):
    f = lambda a: np.ascontiguousarray(np.asarray(a, np.float32))
    fm = lambda v: f(np.asarray(v).reshape(-1, 128).T)
    vecs = np.concatenate([fm(inp["c"][b]), fm(inp["ln_mix_g"][0]), fm(inp["ln_ffn_g"][0]), fm(inp["q_norm_g"][0]), fm(inp["kv_norm_g"][0]),
                           f(np.asarray(inp["conv_w"][0]).T.reshape(12, 128, 4).transpose(1, 0, 2).reshape(128, 48))], axis=1)
    rowv = np.concatenate([f(inp["a_log"][0]), f(inp["dt_bias"][0]), f(inp["gdn_norm_g"][0])])
    skT = f(np.asarray(inp["sub_keys"][0]).transpose(3, 1, 0, 2).reshape(128, 2048))
    return {"x": f(inp["x"][b, :S]), "pos": np.ascontiguousarray(np.asarray(inp["positions"][b, :S], np.int32)), "consts": make_consts(),
            "vecs": f(vecs), "rowv": f(rowv), "b_ada": f(inp["b_ada"][0]).reshape(1, 6144), "fng": f(inp["final_norm_g"]),
            "lnf": f(inp["ln_ffn_g"][0]),
            "w_in": f(inp["w_in"][0]), "w_uq": f(inp["w_uq"][0]), "w_ukv": f(inp["w_ukv"][0]), "w_out": f(inp["w_out"][0]),
            "w_pq": f(inp["w_pq"][0]), "skT": skT, "expert_u": f(inp["expert_u"][0]), "expert_v": f(inp["expert_v"][0]), "w_ada": f(inp["w_ada"][0])}


def kernel(**inputs):
    x = np.asarray(inputs["x"])
    B, S, _ = x.shape
    if S not in _CACHE:
        _CACHE[S] = build(S, dbg=False)[0]
    nc = _CACHE[S]
    in_maps = [_host_inputs(inputs, b, S) for b in range(B)]
    res = run_bass_kernel_spmd(nc, in_maps, core_ids=list(range(B)))
    return np.stack([np.asarray(r["out"], np.float32) for r in res.results], axis=0)
```
